# Optimizing a Trainium2 kernel written in Bass

```python
import math
import jax, jax.numpy as jnp
from jax import lax
import numpy as np

D_MODEL = 2048
BATCH = 16
SEQ = 2048
DEPTH = 4

GRID_W = 64
CTX_LEN = 256
EPS = 1e-6

MLA_HEADS = 8
QK_NOPE = 128
QK_ROPE = 64
V_HEAD = 128
QK_HEAD = QK_NOPE + QK_ROPE
Q_LORA = 512
KV_LORA = 256
MLA_WIDTH = MLA_HEADS * V_HEAD
ROPE_THETA = 10000.0
ROPE_AXIS_DIM = QK_ROPE // 2
ROPE_FREQS = ROPE_AXIS_DIM // 2
ATTN_SCALE = 1.0 / math.sqrt(QK_HEAD)
Q_BLOCK = 128

SGU_GROUPS = 8
SGU_CHUNK = 128
SGU_GROUP_CH = 128
SGU_WIDTH = SGU_GROUPS * SGU_GROUP_CH

POOL_WINDOWS = (2, 4, 8, 16)
POOL_GROUP_CH = 256
POOL_WIDTH = len(POOL_WINDOWS) * POOL_GROUP_CH

N_BRANCH = 3
IN_SPLITS = (Q_LORA, KV_LORA, QK_ROPE, 2 * SGU_WIDTH, POOL_WIDTH, N_BRANCH * D_MODEL)
IN_COLS = sum(IN_SPLITS)

N_GROUPS = 4
EXPERTS_PER_GROUP = 8
N_EXPERTS = N_GROUPS * EXPERTS_PER_GROUP
TOP_K = 2
EXPERT_HIDDEN = 512

kernel_name = "hybrid_mla_sgu_pool_hmoe_dit"


def rmsnorm(x, g=None):
    xf = x.astype(jnp.float32)
    y = xf * lax.rsqrt(jnp.mean(xf * xf, axis=-1, keepdims=True) + EPS)
    if g is not None:
        y = y * g.astype(jnp.float32)
    return y.astype(x.dtype)


def modulate(h, shift, scale):
    return h * (1 + scale) + shift


def axial_rope_tables(rows):
    row = jnp.repeat(jnp.arange(rows, dtype=jnp.float32), GRID_W)
    col = jnp.tile(jnp.arange(GRID_W, dtype=jnp.float32), rows)
    inv = ROPE_THETA ** (-jnp.arange(0, ROPE_AXIS_DIM, 2, dtype=jnp.float32) / ROPE_AXIS_DIM)
    ang = jnp.stack([row[:, None] * inv, col[:, None] * inv], axis=1)
    return jnp.cos(ang), jnp.sin(ang)


def apply_axial_rope(x, cos, sin):
    xr = x.reshape(x.shape[:-1] + (2, 2, ROPE_FREQS))
    x1, x2 = xr[..., 0, :], xr[..., 1, :]
    cos = cos[:, None].astype(x.dtype)
    sin = sin[:, None].astype(x.dtype)
    out = jnp.stack([x1 * cos - x2 * sin, x1 * sin + x2 * cos], axis=-2)
    return out.reshape(x.shape)


def split_cols(p):
    return jnp.split(p, [int(o) for o in np.cumsum(IN_SPLITS)[:-1]], axis=-1)


def mla_q(p_q, lp, rope):
    B, n, _ = p_q.shape
    q = (rmsnorm(p_q, lp["g_cq"]) @ lp["w_uq"]).reshape(B, n, MLA_HEADS, QK_HEAD)
    q = rmsnorm(q, lp["g_q"])
    if rope is not None:
        q = jnp.concatenate([q[..., :QK_NOPE], apply_axial_rope(q[..., QK_NOPE:], *rope)], axis=-1)
    return q


def mla_kv(p_kv, p_kr, lp, rope):
    B, n, _ = p_kv.shape
    kv = (rmsnorm(p_kv, lp["g_ckv"]) @ lp["w_ukv"]).reshape(B, n, MLA_HEADS, QK_NOPE + V_HEAD)
    k_nope, v = kv[..., :QK_NOPE], kv[..., QK_NOPE:]
    k_rope = jnp.broadcast_to(p_kr[:, :, None, :], (B, n, MLA_HEADS, QK_ROPE))
    k = rmsnorm(jnp.concatenate([k_nope, k_rope], axis=-1), lp["g_k"])
    if rope is not None:
        k = jnp.concatenate([k[..., :QK_NOPE], apply_axial_rope(k[..., QK_NOPE:], *rope)], axis=-1)
    return k, v


def attend(q, k, v):
    s = jnp.einsum('bqhd,bkhd->bhqk', q, k).astype(jnp.float32) * ATTN_SCALE
    p = jax.nn.softmax(s, axis=-1).astype(v.dtype)
    o = jnp.einsum('bhqk,bkhd->bqhd', p, v)
    return o.reshape(o.shape[0], o.shape[1], -1)


def latent_attention(q, k, v, kc, vc):
    B, n, H, dq = q.shape
    k_all = jnp.concatenate([k, kc], axis=1)
    v_all = jnp.concatenate([v, vc], axis=1)
    qb = q.reshape(B, n // Q_BLOCK, Q_BLOCK, H, dq).swapaxes(0, 1)
    o = lax.map(lambda qi: attend(qi, k_all, v_all), qb)
    return o.swapaxes(0, 1).reshape(B, n, MLA_WIDTH)


def spatial_gating(p, lp):
    B, n, _ = p.shape
    u, v = jnp.split(jax.nn.gelu(p), 2, axis=-1)
    v = rmsnorm(v, lp["g_sgu"])
    v = v.reshape(B, n // SGU_CHUNK, SGU_CHUNK, SGU_GROUPS, SGU_GROUP_CH)
    v = jnp.einsum('gqp,bnpgc->bnqgc', lp["w_sgu"], v) + lp["b_sgu"].T[:, :, None]
    return u * v.reshape(B, n, SGU_WIDTH)


def multiscale_pool(p, lp):
    B, n, _ = p.shape
    pf = p.astype(jnp.float32)
    cs = jnp.pad(jnp.cumsum(pf, axis=1), ((0, 0), (1, 0), (0, 0)))
    t = jnp.arange(n)
    outs = []
    for gi, w in enumerate(POOL_WINDOWS):
        lo = jnp.clip(t - w // 2, 0, n)
        hi = jnp.clip(t + w // 2, 0, n)
        sl = slice(gi * POOL_GROUP_CH, (gi + 1) * POOL_GROUP_CH)
        mean = (cs[:, hi, sl] - cs[:, lo, sl]) / (hi - lo).astype(jnp.float32)[:, None]
        outs.append(mean - pf[:, :, sl])
    m = jnp.stack(outs, axis=2).astype(p.dtype)
    y = jnp.einsum('bngc,gcd->bngd', m, lp["w_pool"]).reshape(B, n, POOL_WIDTH)
    return y * lp["s_pool"]


def merge_branches(a, b, cp, p_gate, lp):
    ga, gb, gc = jnp.split(jax.nn.sigmoid(p_gate), N_BRANCH, axis=-1)
    y = ga * (a @ lp["w_ao"]) + gb * (b @ lp["w_bo"]) + gc * (cp @ lp["w_co"])
    return y @ lp["w_out"]


def mixer_sublayer(h, hc, lp, rope, ctx_out):
    p_q, p_kv, p_kr, p_sgu, p_pool, p_gate = split_cols(h @ lp["w_in"])
    q = mla_q(p_q, lp, rope)
    k, v = mla_kv(p_kv, p_kr, lp, rope)
    if ctx_out:
        pc_q, pc_kv, pc_kr, pc_sgu, pc_pool, pc_gate = split_cols(hc @ lp["w_in"])
    else:
        pc_kv, pc_kr = jnp.split(hc @ lp["w_in"][:, Q_LORA:Q_LORA + KV_LORA + QK_ROPE], [KV_LORA], axis=-1)
    kc, vc = mla_kv(pc_kv, pc_kr, lp, None)
    a = latent_attention(q, k, v, kc, vc)
    y = merge_branches(a, spatial_gating(p_sgu, lp), multiscale_pool(p_pool, lp), p_gate, lp)
    yc = None
    if ctx_out:
        qc = mla_q(pc_q, lp, None)
        yc = merge_branches(attend(qc, kc, vc), spatial_gating(pc_sgu, lp),
                            multiscale_pool(pc_pool, lp), pc_gate, lp)
    return y, yc


def hier_moe(h, lp):
    lg = (h @ lp["w_rg"]).astype(jnp.float32) + lp["b_rg"]
    g_w, g_i = lax.top_k(jax.nn.softmax(lg, axis=-1), 1)
    le = jnp.einsum('td,gde->tge', h, lp["w_re"]).astype(jnp.float32) + lp["b_re"]
    le = jnp.take_along_axis(le, g_i[:, :, None], axis=1)[:, 0]
    e_w, e_i = lax.top_k(jax.nn.softmax(le, axis=-1), TOP_K)
    e_w = e_w / jnp.sum(e_w, axis=-1, keepdims=True)
    combine = g_w * e_w
    eid = g_i * EXPERTS_PER_GROUP + e_i
    gates = jnp.einsum('tk,tke->te', combine,
                       jax.nn.one_hot(eid, N_EXPERTS, dtype=jnp.float32)).astype(h.dtype)
    y = jnp.zeros_like(h)
    for e in range(N_EXPERTS):
        hid = jax.nn.silu(h @ lp["w_e_gate"][e]) * (h @ lp["w_e_up"][e])
        y = y + gates[:, e:e + 1] * (hid @ lp["w_e_down"][e])
    return y


def setup_inputs(seed: int = 0) -> dict:
    key = jax.random.key(seed)
    ks = jax.random.split(key, 29)
    f32 = jnp.float32
    L, D = DEPTH, D_MODEL

    def nrm(k, shape, scale):
        return jax.random.normal(k, shape, f32) * scale

    def gain(k, shape, s=0.05):
        return 1.0 + s * jax.random.normal(k, shape, f32)

    return {
        "x": nrm(ks[0], (BATCH, SEQ, D), 1.0),
        "c": nrm(ks[1], (BATCH, D), 1.0),
        "ctx": nrm(ks[2], (BATCH, CTX_LEN, D), 1.0),
        "c_ctx": nrm(ks[3], (D,), 1.0),
        "w_ada": nrm(ks[4], (L, D, 6 * D), 0.5 * D ** -0.5),
        "b_ada": nrm(ks[5], (L, 6 * D), 0.02),
        "w_in": nrm(ks[6], (L, D, IN_COLS), D ** -0.5),
        "g_cq": gain(ks[7], (L, Q_LORA)),
        "g_ckv": gain(ks[8], (L, KV_LORA)),
        "w_uq": nrm(ks[9], (L, Q_LORA, MLA_HEADS * QK_HEAD), Q_LORA ** -0.5),
        "w_ukv": nrm(ks[10], (L, KV_LORA, MLA_HEADS * (QK_NOPE + V_HEAD)), KV_LORA ** -0.5),
        "g_q": gain(ks[11], (L, QK_HEAD)),
        "g_k": gain(ks[12], (L, QK_HEAD)),
        "w_sgu": nrm(ks[13], (L, SGU_GROUPS, SGU_CHUNK, SGU_CHUNK), 0.5 * SGU_CHUNK ** -0.5),
        "b_sgu": gain(ks[14], (L, SGU_GROUPS, SGU_CHUNK), 0.1),
        "g_sgu": gain(ks[15], (L, SGU_WIDTH)),
        "w_pool": nrm(ks[16], (L, len(POOL_WINDOWS), POOL_GROUP_CH, POOL_GROUP_CH), POOL_GROUP_CH ** -0.5),
        "s_pool": gain(ks[17], (L, POOL_WIDTH)),
        "w_ao": nrm(ks[18], (L, MLA_WIDTH, D), MLA_WIDTH ** -0.5),
        "w_bo": nrm(ks[19], (L, SGU_WIDTH, D), SGU_WIDTH ** -0.5),
        "w_co": nrm(ks[20], (L, POOL_WIDTH, D), POOL_WIDTH ** -0.5),
        "w_out": nrm(ks[21], (L, D, D), D ** -0.5),
        "w_rg": nrm(ks[22], (L, D, N_GROUPS), D ** -0.5),
        "b_rg": nrm(ks[23], (L, N_GROUPS), 0.01),
        "w_re": nrm(ks[24], (L, N_GROUPS, D, EXPERTS_PER_GROUP), D ** -0.5),
        "b_re": nrm(ks[25], (L, N_GROUPS, EXPERTS_PER_GROUP), 0.01),
        "w_e_gate": nrm(ks[26], (L, N_EXPERTS, D, EXPERT_HIDDEN), D ** -0.5),
        "w_e_up": nrm(ks[27], (L, N_EXPERTS, D, EXPERT_HIDDEN), D ** -0.5),
        "w_e_down": nrm(ks[28], (L, N_EXPERTS, EXPERT_HIDDEN, D), EXPERT_HIDDEN ** -0.5),
    }


def reference(x, c, ctx, c_ctx, w_ada, b_ada, w_in, g_cq, g_ckv, w_uq, w_ukv, g_q, g_k,
              w_sgu, b_sgu, g_sgu, w_pool, s_pool, w_ao, w_bo, w_co, w_out,
              w_rg, b_rg, w_re, b_re, w_e_gate, w_e_up, w_e_down):
    B, n, _ = x.shape
    rows = n // GRID_W
    rope = axial_rope_tables(rows)
    xc = ctx
    for i in range(DEPTH):
        ctx_out = i < DEPTH - 1
        lp = {"w_in": w_in[i], "g_cq": g_cq[i], "g_ckv": g_ckv[i], "w_uq": w_uq[i], "w_ukv": w_ukv[i],
              "g_q": g_q[i], "g_k": g_k[i], "w_sgu": w_sgu[i], "b_sgu": b_sgu[i], "g_sgu": g_sgu[i],
              "w_pool": w_pool[i], "s_pool": s_pool[i], "w_ao": w_ao[i], "w_bo": w_bo[i], "w_co": w_co[i],
              "w_out": w_out[i], "w_rg": w_rg[i], "b_rg": b_rg[i], "w_re": w_re[i], "b_re": b_re[i],
              "w_e_gate": w_e_gate[i], "w_e_up": w_e_up[i], "w_e_down": w_e_down[i]}
        mod = jax.nn.silu(c) @ w_ada[i] + b_ada[i]
        mod_c = jax.nn.silu(c_ctx) @ w_ada[i] + b_ada[i]
        sh1, sc1, g1, sh2, sc2, g2 = jnp.split(mod[:, None, :], 6, axis=-1)
        csh1, csc1, cg1, csh2, csc2, cg2 = jnp.split(mod_c, 6, axis=-1)

        h = modulate(rmsnorm(x), sh1, sc1)
        hc = modulate(rmsnorm(xc), csh1, csc1)
        y, yc = mixer_sublayer(h, hc, lp, rope, ctx_out)
        x = x + g1 * y
        if ctx_out:
            xc = xc + cg1 * yc

        h = modulate(rmsnorm(x), sh2, sc2)
        if ctx_out:
            hc = modulate(rmsnorm(xc), csh2, csc2)
            tok = jnp.concatenate([h.reshape(-1, D_MODEL), hc.reshape(-1, D_MODEL)], axis=0)
            yt = hier_moe(tok, lp)
            n_lat = B * n
            x = x + g2 * yt[:n_lat].reshape(x.shape)
            xc = xc + cg2 * yt[n_lat:].reshape(xc.shape)
        else:
            x = x + g2 * hier_moe(h.reshape(-1, D_MODEL), lp).reshape(x.shape)
    return x
```

```python
import math
from contextlib import ExitStack
import numpy as np
import ml_dtypes
import concourse.bass as bass
import concourse.mybir as mybir
from concourse.bass_utils import run_bass_kernel_spmd

F32 = mybir.dt.float32
BF16 = mybir.dt.bfloat16
AF = mybir.ActivationFunctionType
ALU = mybir.AluOpType
AX = mybir.AxisListType

D = 2048
KC = 16
DEPTH = 4
NLAT = 2048
NCTX = 256
NB = 2
NBT = NLAT + NCTX
NT = NB * NBT
IN_COLS = 10048
EPS = 1e-6
ATTN_SCALE = 1.0 / math.sqrt(192.0)
NEXP = 32
HID = 512
POOL_WINDOWS = (2, 4, 8, 16)
NCORES = 8

ENGS = ("pe", "act", "dve", "pool", "sp")

_PACK_SHAPES = [
    ("w_ada", (2048, 12288)), ("b_ada", (12288,)), ("w_in", (2048, 10048)),
    ("g_cq", (512,)), ("g_ckv", (256,)), ("w_uq", (512, 1536)), ("w_ukv", (256, 2048)),
    ("g_q", (192,)), ("g_k", (192,)), ("w_sguT", (8, 128, 128)), ("b_sgu", (1024,)), ("g_sgu", (1024,)),
    ("w_pool", (4, 256, 256)), ("s_pool", (1024,)),
    ("w_ao", (1024, 2048)), ("w_bo", (1024, 2048)), ("w_co", (1024, 2048)), ("w_out", (2048, 2048)),
    ("w_r", (2048, 36)), ("b_r", (36,)),
    ("w_e_gate", (32, 2048, 512)), ("w_e_up", (32, 2048, 512)), ("w_e_down", (32, 512, 2048)),
]
_CHUNK_OF = {"w_e_gate": 1, "w_e_up": 1, "w_e_down": 2}
CHUNK_ROWS = [32768, 32768, 16384]
NCHUNK = 3
PACK_SPEC = []
_rows = [0, 0, 0]
for _n, _s in _PACK_SHAPES:
    _c = _CHUNK_OF.get(_n, 0)
    PACK_SPEC.append((_n, _s, _c, _rows[_c]))
    _rows[_c] += -(-int(np.prod(_s)) // 2048)
assert all(_rows[i] <= CHUNK_ROWS[i] for i in range(3)), _rows
STOP_AFTER = None


class _Op:
    __slots__ = ("eng", "emit", "deps", "dma", "sig", "ord", "dsem", "dval", "idx", "dmadeps")

    def __init__(self, eng, emit, dma):
        self.eng = eng
        self.emit = emit
        self.dma = dma
        self.deps = []
        self.dmadeps = []
        self.sig = False
        self.ord = 0
        self.dsem = None
        self.dval = 0


class Prog:
    def __init__(self, nc, es, n_dma_sems=80):
        self.nc = nc
        self.esem = {e: es.enter_context(nc.semaphore("S_" + e)) for e in ENGS}
        self.ecount = {e: 0 for e in ENGS}
        self.bar = es.enter_context(nc.semaphore("BAR"))
        self.nbar = 0
        self.dsems = [es.enter_context(nc.semaphore("DQ%d" % i)) for i in range(n_dma_sems)]
        self.dtotal = [0] * n_dma_sems
        self.seen = {e: {} for e in ENGS}
        self._reset_phase()

    def _reset_phase(self):
        self.ops = {e: [] for e in ENGS}
        self.lastw = {}
        self.readers = {}
        self.keymap = {}

    def _track(self, o, r, w):
        deps = []
        for k in r:
            x = self.lastw.get(k)
            if x is not None:
                deps.append(x)
        for k in w:
            x = self.lastw.get(k)
            if x is not None:
                deps.append(x)
            deps.extend(self.readers.get(k, ()))
        seen = set()
        for d in deps:
            if id(d) in seen or d is o:
                continue
            seen.add(id(d))
            if d.dma:
                o.dmadeps.append((d.dsem, self.dtotal[d.dsem]))
            else:
                if d.eng == "pe" and o.eng == "pe" and not o.dma:
                    continue
                d.sig = True
                o.deps.append(d)
        for k in r:
            self.readers.setdefault(k, []).append(o)
        for k in w:
            self.lastw[k] = o
            self.readers[k] = []

    def op(self, eng, emit, r=(), w=()):
        o = _Op(eng, emit, False)
        self._track(o, r, w)
        self.ops[eng].append(o)
        return o

    def dma(self, q, out, in_, r=(), w=(), key=None, **kw):
        if key is None:
            key = w[0] if len(w) else r[0]
        if key not in self.keymap:
            self.keymap[key] = len(self.keymap)
            assert len(self.keymap) <= len(self.dsems), "too many dma keys"
        si = self.keymap[key]
        o = _Op(q, lambda e: e.dma_start(out=out, in_=in_, **kw), True)
        o.dsem = si
        self._track(o, r, w)
        self.dtotal[si] += 16
        o.dval = self.dtotal[si]
        self.ops[q].append(o)
        return o

    def flush(self, name=None):
        nc = self.nc
        last_compute = {}
        for e in ENGS:
            for o in self.ops[e]:
                if not o.dma:
                    last_compute[e] = o
        for e, o in last_compute.items():
            o.sig = True
        for e in ENGS:
            for o in self.ops[e]:
                if (not o.dma) and o.sig:
                    self.ecount[e] += 1
                    o.ord = self.ecount[e]
        self.nbar += 1
        nbar = self.nbar
        used_dsems = sorted(set(self.keymap.values()))

        def run(e, eng):
            seen = self.seen[e]

            def wait(sem, sid, val):
                if seen.get(sid, 0) >= val:
                    return
                seen[sid] = val
                eng.wait_ge(sem, val)

            my_dsems = set()
            for o in self.ops[e]:
                for d in o.deps:
                    wait(self.esem[d.eng], "E" + d.eng, d.ord)
                for (si, val) in o.dmadeps:
                    wait(self.dsems[si], si, val)
                ins = o.emit(eng)
                if o.dma:
                    ins.then_inc(self.dsems[o.dsem], 16)
                    my_dsems.add(o.dsem)
                elif o.sig:
                    ins.then_inc(self.esem[e], 1)
            if e in last_compute:
                wait(self.esem[e], "E" + e, last_compute[e].ord)
            for si in sorted(my_dsems):
                wait(self.dsems[si], si, self.dtotal[si])
            eng.sem_inc(self.bar, 1)
            eng.wait_ge(self.bar, len(ENGS) * nbar)

        with nc.Block() as block:
            block.tensor(lambda eng: run("pe", eng))
            block.scalar(lambda eng: run("act", eng))
            block.vector(lambda eng: run("dve", eng))
            block.gpsimd(lambda eng: run("pool", eng))
            block.sync(lambda eng: run("sp", eng))
        self._reset_phase()


def _rope_tables():
    t = np.arange(NLAT)
    row = (t // 64).astype(np.float32)
    col = (t % 64).astype(np.float32)
    inv = (10000.0 ** (-np.arange(0, 32, 2, dtype=np.float32) / 32.0)).astype(np.float32)
    ang = np.stack([row[:, None] * inv, col[:, None] * inv], axis=1).astype(np.float32)
    return np.cos(ang).astype(np.float32).reshape(NLAT, 32), np.sin(ang).astype(np.float32).reshape(NLAT, 32)


def _pool_matT(n, w):
    t = np.arange(n)
    lo = np.clip(t - w // 2, 0, n)
    hi = np.clip(t + w // 2, 0, n)
    A = np.zeros((n, n), dtype=np.float64)
    for i in range(n):
        A[i, lo[i]:hi[i]] = 1.0 / float(hi[i] - lo[i])
    A -= np.eye(n)
    return A.T.astype(np.float32)


def _band_consts():
    bl = np.zeros((4, 4, 6, 128, 512), dtype=np.float32)
    bc = np.zeros((4, 2, 128, 256), dtype=np.float32)
    for gi, w in enumerate(POOL_WINDOWS):
        MT = _pool_matT(NLAT, w)
        for j in range(4):
            for si in range(6):
                s = 4 * j - 1 + si
                if 0 <= s < 16:
                    bl[gi, j, si] = MT[s * 128:(s + 1) * 128, j * 512:(j + 1) * 512]
        MC = _pool_matT(NCTX, w)
        for s in range(2):
            bc[gi, s] = MC[s * 128:(s + 1) * 128, :]
    return bl.astype(ml_dtypes.bfloat16), bc.astype(ml_dtypes.bfloat16)


def _blocks(include_ctx=True):
    out = []
    for b in range(NB):
        for j in range(4):
            out.append((b * NBT + j * 512, 512, b, False, b, j * 512))
        if include_ctx:
            out.append((b * NBT + NLAT, NCTX, 2, True, b, 0))
    return out


def _bcast_rows(ap2d, nparts=128):
    t = ap2d.partition_broadcast(nparts)
    if len(t.shape) == 3:
        t = t[:, 0, :]
    return t


def build_program(layers, single_layer_inputs, debug=None):
    nc = bass.Bass("TRN2", target_bir_lowering=False)

    def din(name, shape, dt=F32):
        return nc.dram_tensor(name, list(shape), dt, kind="ExternalInput").ap()

    xin = din("xin", [NT, D])
    c3 = din("c3", [3, D])
    nL = len(layers)
    gath = [[nc.dram_tensor("wpk_%d_%d" % (pos, k), [CHUNK_ROWS[k], 2048], F32, kind="ExternalInput") for k in range(NCHUNK)] for pos in range(nL)]

    def layer_views(pos):
        v = {}
        for name, shape, ck, row0 in PACK_SPEC:
            off = row0 * 2048
            ap = []
            stride = 1
            for d_ in reversed(shape):
                ap.insert(0, [stride, d_])
                stride *= d_
            ap.insert(0, [0, 1])
            v[name] = bass.AP(gath[pos][ck], off, ap)
        v["w_eg"] = v["w_e_gate"]; v["w_eu"] = v["w_e_up"]; v["w_ed"] = v["w_e_down"]
        return v
    rope_cos = din("rope_cos", [NLAT, 32])
    rope_sin = din("rope_sin", [NLAT, 32])
    bandL = din("bandL", [4, 4, 6, 128, 512], BF16)
    bandC = din("bandC", [4, 2, 128, 256], BF16)
    ident_in = din("ident", [128, 128], BF16)

    xout = nc.dram_tensor("xout", [NT, D], F32, kind="ExternalOutput").ap()
    dbg_out = None

    def scratch(name, shape, dt):
        if debug and name in debug:
            return nc.dram_tensor(name, list(shape), dt, kind="ExternalOutput").ap()
        return nc.dram_tensor(name, list(shape), dt).ap()

    xs = xout
    mod_d = scratch("mod_d", [3, 6 * D], F32)
    hT_d = scratch("hT_d", [D, NT], BF16)
    qT_d = scratch("qT_d", [8, 192, NT], BF16)
    knT_d = scratch("knT_d", [8, 128, NT], BF16)
    krT_d = scratch("krT_d", [64, NT], BF16)
    v_d = scratch("v_d", [NT, 8, 128], BF16)
    r_d = scratch("r_d", [NT, 8], F32)
    bT_d = scratch("bT_d", [1024, NT], BF16)
    cpT_d = scratch("cpT_d", [1024, NT], BF16)
    aT_d = scratch("aT_d", [1024, NT], BF16)
    yT_d = scratch("yT_d", [D, NT], BF16)
    g_d = scratch("g_d", [NT, 32], F32)
    ymoe_d = scratch("ymoe_d", [NT, D], F32)

    with ExitStack() as gs:
        P = Prog(nc, gs)

        uid = [0]

        def sb(es, name, shape, dt):
            uid[0] += 1
            return es.enter_context(nc.sbuf_tensor("%s_s%d" % (name, uid[0]), list(shape), dt))

        def ps(es, name, shape, dt=F32):
            uid[0] += 1
            return es.enter_context(nc.psum_tensor("%s_p%d" % (name, uid[0]), list(shape), dt))

        for i in range(4):
            r0 = i * (NT // 4)
            P.dma("sp", xs[r0:r0 + NT // 4, :], xin[r0:r0 + NT // 4, :], w=[("xs", i)])
        P.flush()

        base_G = dict(locals())
        for li_pos, li in enumerate(layers):
            last_layer = (li == DEPTH - 1)
            ctx_out = not last_layer
            Gl = dict(base_G)
            Gl.update(layer_views(li_pos))
            _layer(nc, P, sb, ps, 0, ctx_out, Gl, None)
    return nc


def _load_modT(nc, P, es, sb, mod_d, j, name):
    t = sb(es, name, [128, 3, 16], F32)
    for r_ in range(3):
        src = bass.AP(mod_d.tensor, r_ * 6 * D + j * D, [[1, 128], [128, 16]])
        P.dma("sp", t[:, r_, :], src, w=[name], allow_slow_non_contiguous=True)
    return t


def _norm_mod_tile(P, xt, xkey, jk, jkey, st, stkey, xnb, xnkey, psT, pskey, ident, tmpf, tmpkey,
                   scp1, shv, row, hT_dst, hkey, extra_r=()):
    P.op("act", lambda e: e.activation(out=jk[:], in_=xt[:], func=AF.Square, accum_out=st[:, 0:1]),
         r=[xkey], w=[jkey, stkey])
    P.op("act", lambda e: e.activation(out=st[:, 1:2], in_=st[:, 0:1], func=AF.Sqrt, bias=EPS, scale=1.0 / D),
         r=[stkey], w=[stkey])
    P.op("dve", lambda e: e.reciprocal(out=st[:, 2:3], in_=st[:, 1:2]), r=[stkey], w=[stkey])
    P.op("dve", lambda e: e.tensor_scalar(out=xnb[:], in0=xt[:], scalar1=st[:, 2:3], scalar2=None, op0=ALU.mult),
         r=[xkey, stkey], w=[xnkey])
    for c in range(KC):
        P.op("pe", lambda e, c=c: e.transpose(out=psT[:, c, :], in_=xnb[:, c * 128:(c + 1) * 128], identity=ident[:]),
             r=[xnkey, "ident"], w=[pskey])
    P.op("dve", lambda e: e.tensor_tensor(out=tmpf[:], in0=psT[:], in1=scp1[:, row, :].unsqueeze(2).to_broadcast([128, KC, 128]),
                                          op=ALU.mult), r=[pskey, "modsc"] + list(extra_r), w=[tmpkey])
    P.op("dve", lambda e: e.tensor_tensor(out=hT_dst, in0=tmpf[:], in1=shv[:, row, :].unsqueeze(2).to_broadcast([128, KC, 128]),
                                          op=ALU.add), r=[tmpkey, "modsh"], w=[hkey])


def _layer(nc, P, sb, ps, lw, ctx_out, G, dbg_out):
    xs = G["xs"]; c3 = G["c3"]; mod_d = G["mod_d"]
    w_ada = G["w_ada"]; b_ada = G["b_ada"]; w_in = G["w_in"]
    hT_d = G["hT_d"]; qT_d = G["qT_d"]; knT_d = G["knT_d"]; krT_d = G["krT_d"]; v_d = G["v_d"]; r_d = G["r_d"]
    bT_d = G["bT_d"]; cpT_d = G["cpT_d"]; aT_d = G["aT_d"]; yT_d = G["yT_d"]
    ident_in = G["ident_in"]
    blocks_all = _blocks(True)
    blocks_out = _blocks(ctx_out)

    with ExitStack() as es:
        c3T = sb(es, "c3T", [128, 3, KC], F32)
        scT = sb(es, "scT", [128, KC, 3], F32)
        bias3 = sb(es, "bias3", [3, 6 * D], F32)
        modsb = sb(es, "modsb", [3, 6 * D], F32)
        wts = [sb(es, "wada%d" % i, [128, KC, 512], F32) for i in range(2)]
        pms = [ps(es, "pmod%d" % i, [3, 512]) for i in range(2)]
        for r_ in range(3):
            P.dma("sp", c3T[:, r_, :], bass.AP(c3.tensor, r_ * D, [[1, 128], [128, KC]]), w=["c3T"], allow_slow_non_contiguous=True)
        P.dma("sp", bias3[:], _bcast_rows(b_ada[lw:lw + 1, :], 3), w=["bias3"])
        P.op("act", lambda e: e.activation(out=scT[:], in_=c3T[:].rearrange("p r c -> p c r"), func=AF.Silu), r=["c3T"], w=["scT"])
        for jc in range(24):
            wt = wts[jc % 2]; pm = pms[jc % 2]
            wk = ("wada", jc % 2); pk = ("pmod", jc % 2)
            P.dma("sp", wt[:], w_ada[lw, :, jc * 512:(jc + 1) * 512].rearrange("(c p) n -> p c n", p=128), w=[wk])
            for c in range(KC):
                P.op("pe", lambda e, c=c, wt=wt, pm=pm: e.matmul(pm[:], lhsT=scT[:, c, :], rhs=wt[:, c, :], start=(c == 0), stop=(c == KC - 1)),
                     r=["scT", wk], w=[pk])
            P.op("dve", lambda e, pm=pm, jc=jc: e.tensor_tensor(out=modsb[:, jc * 512:(jc + 1) * 512], in0=pm[:], in1=bias3[:, jc * 512:(jc + 1) * 512], op=ALU.add),
                 r=[pk, "bias3"], w=[("modsb", jc)])
        P.dma("sp", mod_d[:, :], modsb[:], r=[("modsb", jc) for jc in range(24)], w=["mod_d"])
        P.flush()

    if dbg_out is not None and "mod" in dbg_out:
        with ExitStack() as es:
            t = sb(es, "dbgmod", [3, 6 * D], F32)
            P.dma("sp", t[:], mod_d[:, :], w=["t"])
            P.dma("sp", dbg_out["mod"][:, :], t[:], r=["t"], w=["o"])
            P.flush()

    phases = [("m1a", _phase_m1a), ("sgu", _phase_sgu), ("pool", _phase_pool), ("attn", _phase_attn),
              ("merge", _phase_merge), ("outproj", _phase_outproj), ("moe", _phase_moe)]
    for name, fn in phases:
        if STOP_AFTER is not None and STOP_AFTER == "adaln":
            break
        fn(nc, P, sb, ps, lw, ctx_out, G)
        if STOP_AFTER is not None and STOP_AFTER == name:
            break


def _phase_m1a(nc, P, sb, ps, lw, ctx_out, G):
    xs = G["xs"]; mod_d = G["mod_d"]; w_in = G["w_in"]
    hT_d = G["hT_d"]; qT_d = G["qT_d"]; knT_d = G["knT_d"]; krT_d = G["krT_d"]; v_d = G["v_d"]; r_d = G["r_d"]
    with ExitStack() as es:
        win_a = sb(es, "win_a", [128, KC, 832], BF16)
        wuq = sb(es, "wuq", [128, 4, 1536], BF16)
        wukv = sb(es, "wukv", [128, 2, 2048], BF16)
        ident = sb(es, "ident", [128, 128], BF16)
        gT6 = sb(es, "gT6", [128, 6], F32)
        gq_bc = sb(es, "gq_bc", [128, 192], F32)
        gk_bc = sb(es, "gk_bc", [128, 192], F32)
        xt = [sb(es, "xt%d" % i, [128, D], F32) for i in range(2)]
        jk = sb(es, "jk", [128, D], BF16)
        st = [sb(es, "st%d" % i, [128, 16], F32) for i in range(2)]
        s8 = [sb(es, "s8_%d" % i, [128, 4, 8], F32) for i in range(2)]
        xnb = [sb(es, "xnb%d" % i, [128, D], BF16) for i in range(2)]
        tmpf = sb(es, "tmpf", [128, KC, 128], F32)
        hT = sb(es, "hT", [128, KC, 512], BF16)
        cn = [sb(es, "cn%d" % i, [128, 768], BF16) for i in range(2)]
        krf = sb(es, "krf", [128, 4, 64], F32)
        cT = sb(es, "cT", [128, 6, 512], BF16)
        sqf = sb(es, "sqf", [128, 1536], F32)
        qg = sb(es, "qg", [128, 8, 192], F32)
        qr = sb(es, "qr", [128, 8, 64], F32)
        rtmp = sb(es, "rtmp", [128, 4, 8, 32], F32)
        qb = sb(es, "qb", [128, 8, 192], BF16)
        qTn_blk = sb(es, "qTn_blk", [128, 8, 512], BF16)
        qTr_blk = sb(es, "qTr_blk", [64, 8, 512], BF16)
        kTn_blk = sb(es, "kTn_blk", [128, 8, 512], BF16)
        krT_blk = sb(es, "krT_blk", [64, 512], BF16)
        knb = sb(es, "knb", [128, 8, 128], BF16)
        vb = [sb(es, "vb%d" % i, [128, 8, 128], BF16) for i in range(2)]
        krg = sb(es, "krg", [128, 64], F32)
        krb = sb(es, "krb", [128, 64], BF16)
        ktmp = sb(es, "ktmp", [128, 4, 32], F32)
        rk_blk = sb(es, "rk_blk", [128, 4, 8], F32)
        cs = [sb(es, "cos%d" % i, [128, 32], F32) for i in range(2)]
        sn = [sb(es, "sin%d" % i, [128, 32], F32) for i in range(2)]
        psT = ps(es, "psT", [128, 2048], BF16)
        psA = ps(es, "psA", [128, 512])
        psB = ps(es, "psB", [128, 512])
        psC = ps(es, "psC", [128, 1024], BF16)
        psQ = ps(es, "psQ", [128, 1536])
        psT3 = psT[:].rearrange("p (c n) -> p c n", c=KC)

        P.dma("pool", win_a[:], w_in[lw, :, 0:832].rearrange("(c p) n -> p c n", p=128), w=["win_a"])
        P.dma("pool", wuq[:], G["w_uq"][lw].rearrange("(c p) n -> p c n", p=128), w=["wuq"])
        P.dma("pool", wukv[:], G["w_ukv"][lw].rearrange("(c p) n -> p c n", p=128), w=["wukv"])
        P.dma("sp", ident[:], G["ident_in"][:, :], w=["ident"])
        P.dma("sp", gT6[:, 0:4], bass.AP(G["g_cq"].tensor, G["g_cq"].offset, [[1, 128], [128, 4]]), w=["gT6a"], allow_slow_non_contiguous=True)
        P.dma("sp", gT6[:, 4:6], bass.AP(G["g_ckv"].tensor, G["g_ckv"].offset, [[1, 128], [128, 2]]), w=["gT6b"], allow_slow_non_contiguous=True)
        P.dma("sp", gq_bc[:], _bcast_rows(G["g_q"][lw:lw + 1, :]), w=["gq_bc"])
        P.dma("sp", gk_bc[:], _bcast_rows(G["g_k"][lw:lw + 1, :]), w=["gk_bc"])
        sc1 = _load_modT(nc, P, es, sb, mod_d, 1, "modsc")
        sh1 = _load_modT(nc, P, es, sb, mod_d, 0, "modsh")
        P.op("dve", lambda e: e.tensor_scalar(out=sc1[:], in0=sc1[:], scalar1=1.0, scalar2=None, op0=ALU.add), r=["modsc"], w=["modsc"])

        gt = 0
        for (tok0, ntok, row, is_ctx, b, pos0) in _blocks(True):
            need_q = ctx_out or (not is_ctx)
            ntile = ntok // 128
            for t in range(ntile):
                sl = gt % 2; gt += 1
                r0 = tok0 + t * 128
                P.dma("pool", xt[sl][:], xs[r0:r0 + 128, :], w=[("xt", sl)])
                _norm_mod_tile(P, xt[sl], ("xt", sl), jk, "jk", st[sl], ("st", sl), xnb[sl], ("xnb", sl), psT3, "psT", ident,
                               tmpf, "tmpf", sc1, sh1, row, hT[:, :, t * 128:(t + 1) * 128], ("hT", t))
            P.dma("sp", hT_d[:, tok0:tok0 + ntok].rearrange("(c p) n -> p c n", p=128), hT[:, :, 0:ntok],
                  r=[("hT", t) for t in range(ntile)], key="st_hT")
            for t in range(ntile):
                sl = t % 2
                stt = st[sl]; sk = ("st", sl)
                for c in range(KC):
                    P.op("pe", lambda e, c=c, t=t: e.matmul(psA[:], lhsT=hT[:, c, t * 128:(t + 1) * 128], rhs=win_a[:, c, 0:512], start=(c == 0), stop=(c == KC - 1)),
                         r=[("hT", t), "win_a"], w=["psA"])
                for c in range(KC):
                    P.op("pe", lambda e, c=c, t=t: e.matmul(psB[:, 0:320], lhsT=hT[:, c, t * 128:(t + 1) * 128], rhs=win_a[:, c, 512:832], start=(c == 0), stop=(c == KC - 1)),
                         r=[("hT", t), "win_a"], w=["psB"])
                P.op("act", lambda e, stt=stt: e.activation(out=jk[:, 0:512], in_=psA[:], func=AF.Square, accum_out=stt[:, 3:4]), r=["psA"], w=["jk", sk])
                P.op("act", lambda e, stt=stt: e.activation(out=jk[:, 512:768], in_=psB[:, 0:256], func=AF.Square, accum_out=stt[:, 6:7]), r=["psB"], w=["jk", sk])
                P.op("act", lambda e, stt=stt: e.activation(out=stt[:, 4:5], in_=stt[:, 3:4], func=AF.Sqrt, bias=EPS, scale=1.0 / 512), r=[sk], w=[sk])
                P.op("act", lambda e, stt=stt: e.activation(out=stt[:, 7:8], in_=stt[:, 6:7], func=AF.Sqrt, bias=EPS, scale=1.0 / 256), r=[sk], w=[sk])
                P.op("dve", lambda e, stt=stt: e.reciprocal(out=stt[:, 5:6], in_=stt[:, 4:5]), r=[sk], w=[sk])
                P.op("dve", lambda e, stt=stt: e.reciprocal(out=stt[:, 8:9], in_=stt[:, 7:8]), r=[sk], w=[sk])
                cnt = cn[sl]; ck = ("cn", sl)
                P.op("dve", lambda e, stt=stt, cnt=cnt: e.tensor_scalar(out=cnt[:, 0:512], in0=psA[:], scalar1=stt[:, 5:6], scalar2=None, op0=ALU.mult), r=["psA", sk], w=[ck])
                P.op("dve", lambda e, stt=stt, cnt=cnt: e.tensor_scalar(out=cnt[:, 512:768], in0=psB[:, 0:256], scalar1=stt[:, 8:9], scalar2=None, op0=ALU.mult), r=["psB", sk, ck], w=[ck])
                P.op("act", lambda e, t=t: e.copy(out=krf[:, t, :], in_=psB[:, 256:320]), r=["psB"], w=[("krf", t)])
                for c in range(6):
                    P.op("pe", lambda e, c=c, cnt=cnt: e.transpose(out=psC[:, c * 128:(c + 1) * 128], in_=cnt[:, c * 128:(c + 1) * 128], identity=ident[:]),
                         r=[ck, "ident"], w=["psC"])
                P.op("dve", lambda e, t=t: e.tensor_tensor(out=cT[:, :, t * 128:(t + 1) * 128], in0=psC[:, 0:768].rearrange("p (c n) -> p c n", c=6),
                                                         in1=gT6[:].unsqueeze(2).to_broadcast([128, 6, 128]), op=ALU.mult),
                     r=["psC", "gT6a", "gT6b"], w=[("cT", t)])
            for t in range(ntile):
                sl = t % 2
                r0 = tok0 + t * 128
                if not is_ctx:
                    P.dma("pool", cs[sl][:], G["rope_cos"][pos0 + t * 128:pos0 + (t + 1) * 128, :], w=[("cos", sl)])
                    P.dma("pool", sn[sl][:], G["rope_sin"][pos0 + t * 128:pos0 + (t + 1) * 128, :], w=[("sin", sl)])
                cosb = cs[sl]; sinb = sn[sl]; ckk = ("cos", sl); skk = ("sin", sl)
                s8t = s8[sl]; s8k = ("s8", sl)
                if need_q:
                    for j in range(3):
                        for c in range(4):
                            P.op("pe", lambda e, c=c, j=j, t=t: e.matmul(psQ[:, j * 512:(j + 1) * 512], lhsT=cT[:, c, t * 128:(t + 1) * 128], rhs=wuq[:, c, j * 512:(j + 1) * 512], start=(c == 0), stop=(c == 3)),
                                 r=[("cT", t), "wuq"], w=["psQ"])
                    psQ3 = psQ[:].rearrange("p (h d) -> p h d", h=8)
                    P.op("act", lambda e: e.activation(out=sqf[:], in_=psQ[:], func=AF.Square), r=["psQ"], w=["sqf"])
                    P.op("dve", lambda e, s8t=s8t: e.tensor_reduce(out=s8t[:, 0, :], in_=sqf[:].rearrange("p (h d) -> p h d", h=8), axis=AX.X, op=ALU.add), r=["sqf"], w=[s8k])
                    P.op("act", lambda e, s8t=s8t: e.activation(out=s8t[:, 1, :], in_=s8t[:, 0, :], func=AF.Sqrt, bias=EPS, scale=1.0 / 192), r=[s8k], w=[s8k])
                    P.op("dve", lambda e, s8t=s8t: e.reciprocal(out=s8t[:, 2, :], in_=s8t[:, 1, :]), r=[s8k], w=[s8k])
                    P.op("dve", lambda e, s8t=s8t, psQ3=psQ3: e.tensor_tensor(out=qg[:], in0=psQ3, in1=s8t[:, 2, :].unsqueeze(2).to_broadcast([128, 8, 192]), op=ALU.mult), r=["psQ", s8k], w=["qg"])
                    P.op("dve", lambda e: e.tensor_tensor(out=qb[:, :, 0:128], in0=qg[:, :, 0:128], in1=gq_bc[:, 0:128].unsqueeze(1).to_broadcast([128, 8, 128]), op=ALU.mult), r=["qg", "gq_bc"], w=["qbn"])
                    if is_ctx:
                        P.op("dve", lambda e: e.tensor_tensor(out=qb[:, :, 128:192], in0=qg[:, :, 128:192], in1=gq_bc[:, 128:192].unsqueeze(1).to_broadcast([128, 8, 64]), op=ALU.mult), r=["qg", "gq_bc"], w=["qbr"])
                    else:
                        P.op("dve", lambda e: e.tensor_tensor(out=qr[:], in0=qg[:, :, 128:192], in1=gq_bc[:, 128:192].unsqueeze(1).to_broadcast([128, 8, 64]), op=ALU.mult), r=["qg", "gq_bc"], w=["qr"])
                        q5 = qr[:].rearrange("p h (a s f) -> p h a s f", a=2, s=2)
                        o5 = qb[:, :, 128:192].rearrange("p h (a s f) -> p h a s f", a=2, s=2)
                        x1 = q5[:, :, :, 0, :]; x2 = q5[:, :, :, 1, :]
                        cb = cosb[:].rearrange("p (a f) -> p a f", a=2).unsqueeze(1).to_broadcast([128, 8, 2, 16])
                        sbb = sinb[:].rearrange("p (a f) -> p a f", a=2).unsqueeze(1).to_broadcast([128, 8, 2, 16])
                        tv = [rtmp[:, i].rearrange("p h (a f) -> p h a f", a=2) for i in range(4)]
                        P.op("dve", lambda e, x1=x1, cb=cb, tv=tv: e.tensor_tensor(out=tv[0], in0=x1, in1=cb, op=ALU.mult), r=["qr", ckk], w=[("rt", 0)])
                        P.op("dve", lambda e, x2=x2, sbb=sbb, tv=tv: e.tensor_tensor(out=tv[1], in0=x2, in1=sbb, op=ALU.mult), r=["qr", skk], w=[("rt", 1)])
                        P.op("dve", lambda e, x1=x1, sbb=sbb, tv=tv: e.tensor_tensor(out=tv[2], in0=x1, in1=sbb, op=ALU.mult), r=["qr", skk], w=[("rt", 2)])
                        P.op("dve", lambda e, x2=x2, cb=cb, tv=tv: e.tensor_tensor(out=tv[3], in0=x2, in1=cb, op=ALU.mult), r=["qr", ckk], w=[("rt", 3)])
                        P.op("dve", lambda e, o5=o5, tv=tv: e.tensor_tensor(out=o5[:, :, :, 0, :], in0=tv[0], in1=tv[1], op=ALU.subtract), r=[("rt", 0), ("rt", 1)], w=["qbr"])
                        P.op("dve", lambda e, o5=o5, tv=tv: e.tensor_tensor(out=o5[:, :, :, 1, :], in0=tv[2], in1=tv[3], op=ALU.add), r=[("rt", 2), ("rt", 3), "qbr"], w=["qbr"])
                    for h in range(8):
                        P.op("pe", lambda e, h=h: e.transpose(out=psT[:, h * 128:(h + 1) * 128], in_=qb[:, h, 0:128], identity=ident[:]), r=["qbn", "ident"], w=["psT"])
                    for h in range(8):
                        P.op("pe", lambda e, h=h: e.transpose(out=psT[0:64, 1024 + h * 128:1024 + (h + 1) * 128], in_=qb[:, h, 128:192], identity=ident[:]), r=["qbr", "ident"], w=["psT"])
                    P.op("act", lambda e, t=t: e.copy(out=qTn_blk[:, :, t * 128:(t + 1) * 128], in_=psT[:, 0:1024].rearrange("p (h n) -> p h n", h=8)), r=["psT"], w=[("qTn", t)])
                    P.op("act", lambda e, t=t: e.copy(out=qTr_blk[:, :, t * 128:(t + 1) * 128], in_=psT[0:64, 1024:2048].rearrange("p (h n) -> p h n", h=8)), r=["psT"], w=[("qTr", t)])
                vbt = vb[sl]; vk = ("vb", sl)
                for hh in range(2):
                    for j in range(2):
                        for c in range(2):
                            P.op("pe", lambda e, c=c, j=j, hh=hh, t=t: e.matmul(psQ[:, j * 512:(j + 1) * 512], lhsT=cT[:, 4 + c, t * 128:(t + 1) * 128],
                                                                              rhs=wukv[:, c, hh * 1024 + j * 512:hh * 1024 + (j + 1) * 512], start=(c == 0), stop=(c == 1)),
                                 r=[("cT", t), "wukv"], w=["psQ"])
                    kv4 = psQ[:, 0:1024].rearrange("p (h d) -> p h d", h=4)
                    P.op("act", lambda e, kv4=kv4: e.activation(out=sqf[:, 0:512].rearrange("p (h d) -> p h d", h=4), in_=kv4[:, :, 0:128], func=AF.Square), r=["psQ"], w=["sqf"])
                    P.op("dve", lambda e, s8t=s8t, hh=hh: e.tensor_reduce(out=s8t[:, 0, hh * 4:(hh + 1) * 4], in_=sqf[:, 0:512].rearrange("p (h d) -> p h d", h=4), axis=AX.X, op=ALU.add), r=["sqf", s8k], w=[s8k])
                    P.op("dve", lambda e, kv4=kv4, hh=hh: e.tensor_tensor(out=knb[:, hh * 4:(hh + 1) * 4, :], in0=kv4[:, :, 0:128], in1=gk_bc[:, 0:128].unsqueeze(1).to_broadcast([128, 4, 128]), op=ALU.mult),
                         r=["psQ", "gk_bc"], w=[("knb", hh)])
                    P.op("act", lambda e, kv4=kv4, hh=hh, vbt=vbt: e.copy(out=vbt[:, hh * 4:(hh + 1) * 4, :], in_=kv4[:, :, 128:256]), r=["psQ"], w=[vk])
                P.dma("sp", v_d[r0:r0 + 128, :, :], vbt[:], r=[vk], key=("st_v", sl))
                stt = st[sl]; sk = ("st", sl)
                P.op("act", lambda e, stt=stt, t=t: e.activation(out=jk[:, 0:64], in_=krf[:, t, :], func=AF.Square, accum_out=stt[:, 9:10]), r=[("krf", t)], w=["jk", sk])
                P.op("dve", lambda e, s8t=s8t, stt=stt: e.tensor_scalar(out=s8t[:, 1, :], in0=s8t[:, 0, :], scalar1=stt[:, 9:10], scalar2=None, op0=ALU.add), r=[s8k, sk], w=[s8k])
                P.op("act", lambda e, s8t=s8t: e.activation(out=s8t[:, 2, :], in_=s8t[:, 1, :], func=AF.Sqrt, bias=EPS, scale=1.0 / 192), r=[s8k], w=[s8k])
                P.op("dve", lambda e, s8t=s8t: e.reciprocal(out=s8t[:, 3, :], in_=s8t[:, 2, :]), r=[s8k], w=[s8k])
                P.op("dve", lambda e, s8t=s8t, t=t: e.tensor_scalar(out=rk_blk[:, t, :], in0=s8t[:, 3, :], scalar1=ATTN_SCALE, scalar2=None, op0=ALU.mult), r=[s8k], w=[("rk", t)])
                for h in range(8):
                    P.op("pe", lambda e, h=h: e.transpose(out=psT[:, h * 128:(h + 1) * 128], in_=knb[:, h, :], identity=ident[:]), r=[("knb", 0), ("knb", 1), "ident"], w=["psT"])
                P.op("act", lambda e, t=t: e.copy(out=kTn_blk[:, :, t * 128:(t + 1) * 128], in_=psT[:, 0:1024].rearrange("p (h n) -> p h n", h=8)), r=["psT"], w=[("kTn", t)])
                if is_ctx:
                    P.op("dve", lambda e, t=t: e.tensor_tensor(out=krb[:], in0=krf[:, t, :], in1=gk_bc[:, 128:192], op=ALU.mult), r=[("krf", t), "gk_bc"], w=["krb"])
                else:
                    P.op("dve", lambda e, t=t: e.tensor_tensor(out=krg[:], in0=krf[:, t, :], in1=gk_bc[:, 128:192], op=ALU.mult), r=[("krf", t), "gk_bc"], w=["krg"])
                    k4 = krg[:].rearrange("p (a s f) -> p a s f", a=2, s=2)
                    ko = krb[:].rearrange("p (a s f) -> p a s f", a=2, s=2)
                    c3_ = cosb[:].rearrange("p (a f) -> p a f", a=2)
                    s3_ = sinb[:].rearrange("p (a f) -> p a f", a=2)
                    kt = [ktmp[:, i, :].rearrange("p (a f) -> p a f", a=2) for i in range(4)]
                    P.op("dve", lambda e, k4=k4, c3_=c3_, kt=kt: e.tensor_tensor(out=kt[0], in0=k4[:, :, 0, :], in1=c3_, op=ALU.mult), r=["krg", ckk], w=[("kt", 0)])
                    P.op("dve", lambda e, k4=k4, s3_=s3_, kt=kt: e.tensor_tensor(out=kt[1], in0=k4[:, :, 1, :], in1=s3_, op=ALU.mult), r=["krg", skk], w=[("kt", 1)])
                    P.op("dve", lambda e, k4=k4, s3_=s3_, kt=kt: e.tensor_tensor(out=kt[2], in0=k4[:, :, 0, :], in1=s3_, op=ALU.mult), r=["krg", skk], w=[("kt", 2)])
                    P.op("dve", lambda e, k4=k4, c3_=c3_, kt=kt: e.tensor_tensor(out=kt[3], in0=k4[:, :, 1, :], in1=c3_, op=ALU.mult), r=["krg", ckk], w=[("kt", 3)])
                    P.op("dve", lambda e, ko=ko, kt=kt: e.tensor_tensor(out=ko[:, :, 0, :], in0=kt[0], in1=kt[1], op=ALU.subtract), r=[("kt", 0), ("kt", 1)], w=["krb"])
                    P.op("dve", lambda e, ko=ko, kt=kt: e.tensor_tensor(out=ko[:, :, 1, :], in0=kt[2], in1=kt[3], op=ALU.add), r=[("kt", 2), ("kt", 3), "krb"], w=["krb"])
                P.op("pe", lambda e: e.transpose(out=psT[0:64, 1024:1152], in_=krb[:], identity=ident[:]), r=["krb", "ident"], w=["psT"])
                P.op("act", lambda e, t=t: e.copy(out=krT_blk[:, t * 128:(t + 1) * 128], in_=psT[0:64, 1024:1152]), r=["psT"], w=[("krT", t)])
            tl = list(range(ntile))
            if need_q:
                P.dma("sp", qT_d[:, 0:128, tok0:tok0 + ntok].rearrange("h p n -> p h n"), qTn_blk[:, :, 0:ntok], r=[("qTn", t) for t in tl], key="st_qTn")
                P.dma("sp", qT_d[:, 128:192, tok0:tok0 + ntok].rearrange("h p n -> p h n"), qTr_blk[:, :, 0:ntok], r=[("qTr", t) for t in tl], key="st_qTr")
            P.dma("sp", knT_d[:, :, tok0:tok0 + ntok].rearrange("h p n -> p h n"), kTn_blk[:, :, 0:ntok], r=[("kTn", t) for t in tl], key="st_kTn")
            P.dma("sp", krT_d[:, tok0:tok0 + ntok], krT_blk[:, 0:ntok], r=[("krT", t) for t in tl], key="st_krT")
            P.dma("sp", r_d[tok0:tok0 + ntok, :].rearrange("(t p) e -> p t e", p=128), rk_blk[:, 0:ntile, :], r=[("rk", t) for t in tl], key="st_rk")
        P.flush()


def _phase_sgu(nc, P, sb, ps, lw, ctx_out, G):
    w_in = G["w_in"]; hT_d = G["hT_d"]; bT_d = G["bT_d"]
    with ExitStack() as es:
        win_b = sb(es, "win_b", [128, KC, 2048], BF16)
        wsT = sb(es, "wsT", [128, 8, 128], BF16)
        bs_bc = sb(es, "bs_bc", [128, 8, 128], F32)
        gs_bc = sb(es, "gs_bc", [128, 1024], F32)
        hT = [sb(es, "hT%d" % i, [128, KC, 512], BF16) for i in range(2)]
        ut = sb(es, "ut", [128, 8, 128], F32)
        gv = [sb(es, "gv%d" % i, [128, 1024], F32) for i in range(2)]
        jk = sb(es, "jk", [128, 1024], BF16)
        st = [sb(es, "st%d" % i, [128, 4], F32) for i in range(2)]
        vnb = [sb(es, "vnb%d" % i, [128, 8, 128], BF16) for i in range(2)]
        tmp = sb(es, "tmp", [128, 8, 128], F32)
        bT = [sb(es, "bT%d" % i, [128, 8, 512], BF16) for i in range(2)]
        psUt = ps(es, "psUt", [128, 8, 128])
        psV = [ps(es, "psV%d" % i, [128, 1024]) for i in range(2)]
        psS = ps(es, "psS", [128, 8, 128])

        for half in range(2):
            P.dma("pool", win_b[:, :, half * 1024:(half + 1) * 1024], w_in[lw, :, 832 + half * 1024:832 + (half + 1) * 1024].rearrange("(c p) n -> p c n", p=128), w=[("win_b", half)])
        P.dma("pool", wsT[:], G["w_sguT"][lw].rearrange("g p q -> p g q"), w=["wsT"])
        P.dma("sp", bs_bc[:].rearrange("p g q -> p (g q)"), _bcast_rows(G["b_sgu"][lw:lw + 1, :]), w=["bs_bc"])
        P.dma("sp", gs_bc[:], _bcast_rows(G["g_sgu"][lw:lw + 1, :]), w=["gs_bc"])

        gt = 0
        for bi, (tok0, ntok, row, is_ctx, b, pos0) in enumerate(_blocks(ctx_out)):
            ntile = ntok // 128
            hs = bi % 2
            hTb = hT[hs]; hk = ("hT", hs)
            bTb = bT[hs]
            P.dma("pool", hTb[:, :, 0:ntok], hT_d[:, tok0:tok0 + ntok].rearrange("(c p) n -> p c n", p=128), w=[hk])
            for t in range(ntile):
                sl = gt % 2; gt += 1
                pv = psV[sl]; pvk = ("psV", sl)
                for j in range(2):
                    for c in range(KC):
                        P.op("pe", lambda e, c=c, j=j, t=t, pv=pv, hTb=hTb: e.matmul(pv[:, j * 512:(j + 1) * 512], lhsT=hTb[:, c, t * 128:(t + 1) * 128], rhs=win_b[:, c, 1024 + j * 512:1024 + (j + 1) * 512], start=(c == 0), stop=(c == KC - 1)),
                             r=[hk, ("win_b", 1)], w=[pvk])
                for g in range(8):
                    for c in range(KC):
                        P.op("pe", lambda e, c=c, g=g, t=t, hTb=hTb: e.matmul(psUt[:, g, :], lhsT=win_b[:, c, g * 128:(g + 1) * 128], rhs=hTb[:, c, t * 128:(t + 1) * 128], start=(c == 0), stop=(c == KC - 1)),
                             r=[hk, ("win_b", 0)], w=["psUt"])
                P.op("act", lambda e: e.activation(out=ut[:], in_=psUt[:], func=AF.Gelu), r=["psUt"], w=["ut"])
                gvt = gv[sl]; gk = ("gv", sl); stt = st[sl]; sk = ("st", sl)
                P.op("act", lambda e, gvt=gvt, pv=pv: e.activation(out=gvt[:], in_=pv[:], func=AF.Gelu), r=[pvk], w=[gk])
                P.op("act", lambda e, gvt=gvt, stt=stt: e.activation(out=jk[:], in_=gvt[:], func=AF.Square, accum_out=stt[:, 0:1]), r=[gk], w=["jk", sk])
                P.op("act", lambda e, stt=stt: e.activation(out=stt[:, 1:2], in_=stt[:, 0:1], func=AF.Sqrt, bias=EPS, scale=1.0 / 1024), r=[sk], w=[sk])
                P.op("dve", lambda e, stt=stt: e.reciprocal(out=stt[:, 2:3], in_=stt[:, 1:2]), r=[sk], w=[sk])
                vt = vnb[sl]; vk = ("vnb", sl)
                P.op("dve", lambda e, gvt=gvt, stt=stt, vt=vt: e.scalar_tensor_tensor(out=vt[:].rearrange("p g c -> p (g c)"), in0=gvt[:], scalar=stt[:, 2:3], in1=gs_bc[:], op0=ALU.mult, op1=ALU.mult),
                     r=[gk, sk, "gs_bc"], w=[vk])
                for g in range(8):
                    P.op("pe", lambda e, g=g, vt=vt: e.matmul(psS[:, g, :], lhsT=vt[:, g, :], rhs=wsT[:, g, :], start=True, stop=True), r=[vk, "wsT"], w=["psS"])
                P.op("dve", lambda e: e.tensor_tensor(out=tmp[:], in0=psS[:], in1=bs_bc[:], op=ALU.add), r=["psS", "bs_bc"], w=["tmp"])
                P.op("dve", lambda e, t=t, bTb=bTb: e.tensor_tensor(out=bTb[:, :, t * 128:(t + 1) * 128], in0=tmp[:], in1=ut[:], op=ALU.mult),
                     r=["tmp", "ut"], w=[("bT", hs, t)])
            P.dma("sp", bT_d[:, tok0:tok0 + ntok].rearrange("(g p) n -> p g n", p=128), bTb[:, :, 0:ntok], r=[("bT", hs, t) for t in range(ntile)], key=("st_bT", hs))
        P.flush()


def _phase_pool(nc, P, sb, ps, lw, ctx_out, G):
    w_in = G["w_in"]; hT_d = G["hT_d"]; cpT_d = G["cpT_d"]
    with ExitStack() as es:
        win_c = sb(es, "win_c", [128, KC, 1024], BF16)
        wpool = sb(es, "wpool", [128, 4, 2, 256], BF16)
        spT = sb(es, "spT", [128, 8], F32)
        hT = [sb(es, "hT%d" % i, [128, KC, 512], BF16) for i in range(2)]
        pp = sb(es, "pp", [128, 18, 1024], BF16)
        band = [sb(es, "band%d" % i, [128, 4, 6, 512], BF16) for i in range(2)]
        mT = sb(es, "mT", [128, 8, 512], BF16)
        cpT = [sb(es, "cpT%d" % i, [128, 8, 512], BF16) for i in range(2)]
        psP = [ps(es, "psP%d" % i, [128, 1024]) for i in range(2)]
        psM = [ps(es, "psM%d" % i, [128, 512]) for i in range(2)]
        psY = [ps(es, "psY%d" % i, [128, 512]) for i in range(2)]

        P.dma("pool", win_c[:], w_in[lw, :, 2880:3904].rearrange("(c p) n -> p c n", p=128), w=["win_c"])
        for gi in range(4):
            P.dma("pool", wpool[:, gi], G["w_pool"][lw, gi].rearrange("(k p) d -> p k d", p=128), w=[("wpool", gi)], key="wpool")
        P.dma("sp", spT[:], bass.AP(G["s_pool"].tensor, G["s_pool"].offset, [[1, 128], [128, 8]]), w=["spT"], allow_slow_non_contiguous=True)

        gt = 0; gb = 0; gm = 0
        for b in range(NB):
            blks = [(b * NBT + j * 512, 512, False, j) for j in range(4)]
            if ctx_out:
                blks.append((b * NBT + NLAT, NCTX, True, 0))
            for (tok0, ntok, is_ctx, j) in blks:
                ntile = ntok // 128
                hs = gb % 2; gb += 1
                hTb = hT[hs]; hk = ("hT", hs)
                P.dma("pool", hTb[:, :, 0:ntok], hT_d[:, tok0:tok0 + ntok].rearrange("(c p) n -> p c n", p=128), w=[hk])
                for t in range(ntile):
                    ti = (16 + t) if is_ctx else (j * 4 + t)
                    sl = gt % 2; gt += 1
                    pv = psP[sl]; pvk = ("psP", sl)
                    for jj in range(2):
                        for c in range(KC):
                            P.op("pe", lambda e, c=c, jj=jj, t=t, pv=pv, hTb=hTb: e.matmul(pv[:, jj * 512:(jj + 1) * 512], lhsT=hTb[:, c, t * 128:(t + 1) * 128], rhs=win_c[:, c, jj * 512:(jj + 1) * 512], start=(c == 0), stop=(c == KC - 1)),
                                 r=[hk, "win_c"], w=[pvk])
                    P.op("act", lambda e, ti=ti, pv=pv: e.copy(out=pp[:, ti, :], in_=pv[:]), r=[pvk], w=[("pp", ti)])
            for bi, (tok0, ntok, is_ctx, j) in enumerate(blks):
                bs_ = gm % 2; gm += 1
                bd = band[bs_]; bk = ("band", bs_)
                cpb = cpT[bs_]
                if is_ctx:
                    srcs = [(16 + s, s) for s in range(2)]
                    for gi in range(4):
                        P.dma("pool", bd[:, gi, 0:2, 0:256], G["bandC"][gi].rearrange("s p n -> p s n"), w=[(bk, gi)], key=bk)
                else:
                    srcs = [(4 * j - 1 + si, si) for si in range(6) if 0 <= 4 * j - 1 + si < 16]
                    for gi in range(4):
                        P.dma("pool", bd[:, gi], G["bandL"][gi, j].rearrange("s p n -> p s n"), w=[(bk, gi)], key=bk)
                for gi in range(4):
                    for cc in range(2):
                        idx = gi * 2 + cc
                        pm = psM[idx % 2]; pmk = ("psM", idx % 2)
                        for n_, (ti, si) in enumerate(srcs):
                            P.op("pe", lambda e, gi=gi, cc=cc, ti=ti, si=si, n_=n_, pm=pm, bd=bd, ntok=ntok, ns=len(srcs): e.matmul(pm[:, 0:ntok], lhsT=pp[:, ti, gi * 256 + cc * 128:gi * 256 + (cc + 1) * 128], rhs=bd[:, gi, si, 0:ntok], start=(n_ == 0), stop=(n_ == ns - 1)),
                                 r=[("pp", ti), (bk, gi)], w=[pmk])
                        P.op("act", lambda e, idx=idx, pm=pm, ntok=ntok: e.copy(out=mT[:, idx, 0:ntok], in_=pm[:, 0:ntok]), r=[pmk], w=[("mT", idx)])
                for gi in range(4):
                    for dc in range(2):
                        idx = gi * 2 + dc
                        py = psY[idx % 2]; pyk = ("psY", idx % 2)
                        for kc in range(2):
                            P.op("pe", lambda e, gi=gi, dc=dc, kc=kc, py=py, ntok=ntok: e.matmul(py[:, 0:ntok], lhsT=wpool[:, gi, kc, dc * 128:(dc + 1) * 128], rhs=mT[:, gi * 2 + kc, 0:ntok], start=(kc == 0), stop=(kc == 1)),
                                 r=[("mT", gi * 2), ("mT", gi * 2 + 1), ("wpool", gi)], w=[pyk])
                        P.op("dve", lambda e, idx=idx, py=py, ntok=ntok, cpb=cpb: e.tensor_scalar(out=cpb[:, idx, 0:ntok], in0=py[:, 0:ntok], scalar1=spT[:, idx:idx + 1], scalar2=None, op0=ALU.mult),
                             r=[pyk, "spT"], w=[("cpT", bs_, idx)])
                P.dma("sp", cpT_d[:, tok0:tok0 + ntok].rearrange("(g p) n -> p g n", p=128), cpb[:, :, 0:ntok], r=[("cpT", bs_, i) for i in range(8)], key=("st_cpT", bs_))
        P.flush()


def _phase_attn(nc, P, sb, ps, lw, ctx_out, G):
    qT_d = G["qT_d"]; knT_d = G["knT_d"]; krT_d = G["krT_d"]; v_d = G["v_d"]; r_d = G["r_d"]; aT_d = G["aT_d"]
    NKC = NBT // 128
    with ExitStack() as es:
        ones = sb(es, "ones", [128, 128], BF16)
        krT = sb(es, "krT", [64, NBT], BF16)
        rk = sb(es, "rk", [128, NKC, 8], F32)
        kTn = [sb(es, "kTn%d" % i, [128, NBT], BF16) for i in range(2)]
        vh = [sb(es, "vh%d" % i, [128, NKC, 128], BF16) for i in range(2)]
        qn = [sb(es, "qn%d" % i, [128, 512], BF16) for i in range(2)]
        qr = [sb(es, "qr%d" % i, [64, 512], BF16) for i in range(2)]
        pT = [sb(es, "pT%d" % i, [128, 512], BF16) for i in range(3)]
        rden = sb(es, "rden", [128, 512], F32)
        aT = [sb(es, "aT%d" % i, [128, 512], BF16) for i in range(2)]
        psS = [ps(es, "psS%d" % i, [128, 512]) for i in range(3)]
        psO = [ps(es, "psO%d" % i, [128, 512]) for i in range(2)]
        psD = [ps(es, "psD%d" % i, [128, 512]) for i in range(2)]
        P.op("dve", lambda e: e.memset(ones[:], 1.0), w=["ones"])
        gq = 0; gp = 0; gh = 0
        for b in range(NB):
            k0 = b * NBT
            P.dma("pool", krT[:], krT_d[:, k0:k0 + NBT], w=["krT"])
            P.dma("pool", rk[:], r_d[k0:k0 + NBT, :].rearrange("(t p) e -> p t e", p=128), w=["rk"])
            for h in range(8):
                hs = gh % 2; gh += 1
                kt = kTn[hs]; kk = ("kTn", hs); vt = vh[hs]; vk = ("vh", hs)
                P.dma("pool", kt[:], knT_d[h, :, k0:k0 + NBT], w=[kk])
                P.dma("pool", vt[:], v_d[k0:k0 + NBT, h, :].rearrange("(t p) d -> p t d", p=128), w=[vk])
                qblocks = [(k0 + j * 512, 512, list(range(NKC))) for j in range(4)]
                if ctx_out:
                    qblocks.append((k0 + NLAT, NCTX, [16, 17]))
                for (q0, nq, kcs) in qblocks:
                    qs = gq % 2; gq += 1
                    qnt = qn[qs]; qrt = qr[qs]; qk = ("q", qs)
                    P.dma("pool", qnt[:, 0:nq], qT_d[h, 0:128, q0:q0 + nq], w=[qk], key=("ldqn", qs))
                    P.dma("pool", qrt[:, 0:nq], qT_d[h, 128:192, q0:q0 + nq], w=[("qr", qs)], key=("ldqr", qs))
                    po = psO[qs]; pd = psD[qs]; pok = ("psO", qs)
                    for n_, kc in enumerate(kcs):
                        ss = gp % 3; gp += 1
                        pss = psS[ss]; psk = ("psS", ss); ptt = pT[ss]; ptk = ("pT", ss)
                        P.op("pe", lambda e, kc=kc, pss=pss, kt=kt, qnt=qnt, nq=nq: e.matmul(pss[:, 0:nq], lhsT=kt[:, kc * 128:(kc + 1) * 128], rhs=qnt[:, 0:nq], start=True, stop=False),
                             r=[kk, qk], w=[psk])
                        P.op("pe", lambda e, kc=kc, pss=pss, qrt=qrt, nq=nq: e.matmul(pss[:, 0:nq], lhsT=krT[:, kc * 128:(kc + 1) * 128], rhs=qrt[:, 0:nq], start=False, stop=True),
                             r=["krT", ("qr", qs)], w=[psk])
                        P.op("act", lambda e, kc=kc, h=h, pss=pss, ptt=ptt, nq=nq: e.activation(out=ptt[:, 0:nq], in_=pss[:, 0:nq], func=AF.Exp, scale=rk[:, kc, h:h + 1]),
                             r=[psk, "rk"], w=[ptk])
                        first = (n_ == 0); lastk = (n_ == len(kcs) - 1)
                        P.op("pe", lambda e, kc=kc, vt=vt, ptt=ptt, po=po, nq=nq, first=first, lastk=lastk: e.matmul(po[:, 0:nq], lhsT=vt[:, kc, :], rhs=ptt[:, 0:nq], start=first, stop=lastk),
                             r=[vk, ptk], w=[pok])
                        P.op("pe", lambda e, ptt=ptt, pd=pd, nq=nq, first=first, lastk=lastk: e.matmul(pd[:, 0:nq], lhsT=ones[:], rhs=ptt[:, 0:nq], start=first, stop=lastk),
                             r=["ones", ptk], w=[pok])
                    at = aT[qs]; ak = ("aT", qs)
                    P.op("dve", lambda e, pd=pd, nq=nq: e.reciprocal(out=rden[:, 0:nq], in_=pd[:, 0:nq]), r=[pok], w=["rden"])
                    P.op("dve", lambda e, po=po, at=at, nq=nq: e.tensor_tensor(out=at[:, 0:nq], in0=po[:, 0:nq], in1=rden[:, 0:nq], op=ALU.mult), r=[pok, "rden"], w=[ak])
                    P.dma("sp", aT_d[h * 128:(h + 1) * 128, q0:q0 + nq], at[:, 0:nq], r=[ak], key=("st_aT", qs))
        P.flush()


def _phase_merge(nc, P, sb, ps, lw, ctx_out, G):
    w_in = G["w_in"]; hT_d = G["hT_d"]; yT_d = G["yT_d"]
    srcs_d = [G["aT_d"], G["bT_d"], G["cpT_d"]]
    wbr_d = [G["w_ao"], G["w_bo"], G["w_co"]]
    GATE0 = 3904
    with ExitStack() as es:
        hT = sb(es, "hT", [128, KC, 512], BF16)
        br = [sb(es, "br%d" % i, [128, 8, 512], BF16) for i in range(3)]
        wg = [sb(es, "wg%d" % i, [128, KC, 3, 256], BF16) for i in range(2)]
        wb = [sb(es, "wb%d" % i, [128, 3, 8, 256], BF16) for i in range(2)]
        sg = [sb(es, "sg%d" % i, [128, 512], F32) for i in range(2)]
        acc = sb(es, "acc", [128, 512], F32)
        t2 = sb(es, "t2", [128, 512], F32)
        yT = [sb(es, "yT%d" % i, [128, 2, 512], BF16) for i in range(2)]
        psG = [ps(es, "psG%d" % i, [128, 512]) for i in range(2)]
        psB = [ps(es, "psB%d" % i, [128, 512]) for i in range(2)]
        gw = 0; gpp = 0
        for (tok0, ntok, row, is_ctx, b, pos0) in _blocks(ctx_out):
            P.dma("sp", hT[:, :, 0:ntok], hT_d[:, tok0:tok0 + ntok].rearrange("(c p) n -> p c n", p=128), w=["hT"])
            for i in range(3):
                P.dma("sp", br[i][:, :, 0:ntok], srcs_d[i][:, tok0:tok0 + ntok].rearrange("(c p) n -> p c n", p=128), w=[("br", i)])
            for dg in range(8):
                ws = gw % 2; gw += 1
                wgt = wg[ws]; wbt = wb[ws]; wgk = ("wg", ws); wbk = ("wb", ws)
                for i in range(3):
                    P.dma("pool", wgt[:, :, i, :], w_in[lw, :, GATE0 + i * D + dg * 256:GATE0 + i * D + (dg + 1) * 256].rearrange("(c p) n -> p c n", p=128), w=[(wgk, i)], key=wgk)
                    P.dma("pool", wbt[:, i, :, :], wbr_d[i][lw, :, dg * 256:(dg + 1) * 256].rearrange("(c p) n -> p c n", p=128), w=[(wbk, i)], key=wbk)
                ytt = yT[ws]; yk = ("yT", ws)
                for dl in range(2):
                    for i in range(3):
                        s_ = gpp % 2; gpp += 1
                        pg = psG[s_]; pb = psB[s_]; pgk = ("psG", s_); pbk = ("psB", s_)
                        sgt = sg[s_]; sgk = ("sg", s_)
                        for c in range(KC):
                            P.op("pe", lambda e, c=c, i=i, dl=dl, pg=pg, wgt=wgt, ntok=ntok: e.matmul(pg[:, 0:ntok], lhsT=wgt[:, c, i, dl * 128:(dl + 1) * 128], rhs=hT[:, c, 0:ntok], start=(c == 0), stop=(c == KC - 1)),
                                 r=["hT", (wgk, i)], w=[pgk])
                        for c in range(8):
                            P.op("pe", lambda e, c=c, i=i, dl=dl, pb=pb, wbt=wbt, ntok=ntok: e.matmul(pb[:, 0:ntok], lhsT=wbt[:, i, c, dl * 128:(dl + 1) * 128], rhs=br[i][:, c, 0:ntok], start=(c == 0), stop=(c == 7)),
                                 r=[("br", i), (wbk, i)], w=[pbk])
                        P.op("act", lambda e, pg=pg, sgt=sgt, ntok=ntok: e.activation(out=sgt[:, 0:ntok], in_=pg[:, 0:ntok], func=AF.Sigmoid), r=[pgk], w=[sgk])
                        if i == 0:
                            P.op("dve", lambda e, pb=pb, sgt=sgt, ntok=ntok: e.tensor_tensor(out=acc[:, 0:ntok], in0=pb[:, 0:ntok], in1=sgt[:, 0:ntok], op=ALU.mult), r=[pbk, sgk], w=["acc"])
                        elif i == 1:
                            P.op("dve", lambda e, pb=pb, sgt=sgt, ntok=ntok: e.tensor_tensor(out=t2[:, 0:ntok], in0=pb[:, 0:ntok], in1=sgt[:, 0:ntok], op=ALU.mult), r=[pbk, sgk], w=["t2"])
                            P.op("dve", lambda e, ntok=ntok: e.tensor_tensor(out=acc[:, 0:ntok], in0=acc[:, 0:ntok], in1=t2[:, 0:ntok], op=ALU.add), r=["acc", "t2"], w=["acc"])
                        else:
                            P.op("dve", lambda e, pb=pb, sgt=sgt, ntok=ntok: e.tensor_tensor(out=t2[:, 0:ntok], in0=pb[:, 0:ntok], in1=sgt[:, 0:ntok], op=ALU.mult), r=[pbk, sgk], w=["t2"])
                            P.op("dve", lambda e, ntok=ntok, ytt=ytt, dl=dl: e.tensor_tensor(out=ytt[:, dl, 0:ntok], in0=acc[:, 0:ntok], in1=t2[:, 0:ntok], op=ALU.add), r=["acc", "t2"], w=[(yk, dl)])
                P.dma("sp", yT_d[dg * 256:(dg + 1) * 256, tok0:tok0 + ntok].rearrange("(c p) n -> p c n", p=128), ytt[:, :, 0:ntok], r=[(yk, 0), (yk, 1)], key=("st_yT", ws))
        P.flush()


def _phase_outproj(nc, P, sb, ps, lw, ctx_out, G):
    xs = G["xs"]; mod_d = G["mod_d"]; yT_d = G["yT_d"]
    with ExitStack() as es:
        wout = sb(es, "wout", [128, KC, D], BF16)
        g1 = sb(es, "g1", [128, 3, D], F32)
        yT = [sb(es, "yT%d" % i, [128, KC, 512], BF16) for i in range(2)]
        xt = [sb(es, "xt%d" % i, [128, D], F32) for i in range(2)]
        tm = [sb(es, "tm%d" % i, [128, D], F32) for i in range(2)]
        psO = [ps(es, "psO%d" % i, [128, 512]) for i in range(8)]
        for hf in range(2):
            P.dma("pool", wout[:, :, hf * 1024:(hf + 1) * 1024], G["w_out"][lw, :, hf * 1024:(hf + 1) * 1024].rearrange("(c p) n -> p c n", p=128), w=[("wout", hf)])
        for r_ in range(3):
            P.dma("sp", g1[:, r_, :], _bcast_rows(mod_d[r_:r_ + 1, 2 * D:3 * D]), w=[("g1", r_)])
        gt = 0
        for bi, (tok0, ntok, row, is_ctx, b, pos0) in enumerate(_blocks(ctx_out)):
            ys = bi % 2
            ytt = yT[ys]; yk = ("yT", ys)
            P.dma("pool", ytt[:, :, 0:ntok], yT_d[:, tok0:tok0 + ntok].rearrange("(c p) n -> p c n", p=128), w=[yk])
            for t in range(ntok // 128):
                sl = gt % 2; gt += 1
                r0 = tok0 + t * 128
                xtt = xt[sl]; xk = ("xt", sl); tmt = tm[sl]; tk = ("tm", sl)
                P.dma("pool", xtt[:], xs[r0:r0 + 128, :], w=[xk])
                for j in range(4):
                    po = psO[sl * 4 + j]; pk = ("psO", sl * 4 + j)
                    for c in range(KC):
                        P.op("pe", lambda e, c=c, j=j, t=t, po=po, ytt=ytt: e.matmul(po[:], lhsT=ytt[:, c, t * 128:(t + 1) * 128], rhs=wout[:, c, j * 512:(j + 1) * 512], start=(c == 0), stop=(c == KC - 1)),
                             r=[yk, ("wout", j // 2)], w=[pk])
                    P.op("dve", lambda e, j=j, po=po, tmt=tmt, row=row: e.tensor_tensor(out=tmt[:, j * 512:(j + 1) * 512], in0=po[:], in1=g1[:, row, j * 512:(j + 1) * 512], op=ALU.mult),
                         r=[pk, ("g1", row)], w=[(tk, j)])
                P.op("pool", lambda e, xtt=xtt, tmt=tmt: e.tensor_tensor(out=xtt[:], in0=xtt[:], in1=tmt[:], op=ALU.add), r=[xk] + [(tk, j) for j in range(4)], w=[xk])
                P.dma("sp", xs[r0:r0 + 128, :], xtt[:], r=[xk], key=("st_x", sl))
        P.flush()


def _tile_row(gti):
    b, lt = divmod(gti, NBT // 128)
    return b if lt < NLAT // 128 else 2


def _phase_moe(nc, P, sb, ps, lw, ctx_out, G):
    xs = G["xs"]; mod_d = G["mod_d"]; hT_d = G["hT_d"]
    g_d = G["g_d"]; ymoe_d = G["ymoe_d"]
    NTILE = NT // 128
    with ExitStack() as es:
        ident = sb(es, "ident", [128, 128], BF16)
        wr = sb(es, "wr", [128, KC, 36], BF16)
        br_bc = sb(es, "br_bc", [128, 36], F32)
        xt = [sb(es, "xt%d" % i, [128, D], F32) for i in range(2)]
        jk = sb(es, "jk", [128, D], BF16)
        st = [sb(es, "st%d" % i, [128, 16], F32) for i in range(2)]
        xnb = [sb(es, "xnb%d" % i, [128, D], BF16) for i in range(2)]
        tmpf = sb(es, "tmpf", [128, KC, 128], F32)
        hT = [sb(es, "hT%d" % i, [128, KC, 128], BF16) for i in range(2)]
        Lg = [sb(es, "Lg%d" % i, [128, 36], F32) for i in range(2)]
        rt = [sb(es, "rt%d" % i, [128, 96], F32) for i in range(2)]
        gat = [sb(es, "gat%d" % i, [128, 4, 8], F32) for i in range(2)]
        psT = ps(es, "psT", [128, 2048], BF16)
        psR = [ps(es, "psR%d" % i, [128, 64]) for i in range(2)]
        psT3 = psT[:].rearrange("p (c n) -> p c n", c=KC)
        P.dma("sp", ident[:], G["ident_in"][:, :], w=["ident"])
        P.dma("pool", wr[:], G["w_r"][lw].rearrange("(c p) n -> p c n", p=128), w=["wr"])
        P.dma("sp", br_bc[:], _bcast_rows(G["b_r"][lw:lw + 1, :]), w=["br_bc"])
        sc2 = _load_modT(nc, P, es, sb, mod_d, 4, "modsc")
        sh2 = _load_modT(nc, P, es, sb, mod_d, 3, "modsh")
        P.op("dve", lambda e: e.tensor_scalar(out=sc2[:], in0=sc2[:], scalar1=1.0, scalar2=None, op0=ALU.add), r=["modsc"], w=["modsc"])
        for gti in range(NTILE):
            row = _tile_row(gti)
            if row == 2 and not ctx_out:
                continue
            sl = gti % 2
            r0 = gti * 128
            P.dma("pool", xt[sl][:], xs[r0:r0 + 128, :], w=[("xt", sl)])
            hTt = hT[sl]; hk = ("hT", sl)
            _norm_mod_tile(P, xt[sl], ("xt", sl), jk, "jk", st[sl], ("st", sl), xnb[sl], ("xnb", sl), psT3, "psT", ident,
                           tmpf, "tmpf", sc2, sh2, row, hTt[:], hk)
            P.dma("sp", hT_d[:, r0:r0 + 128].rearrange("(c p) n -> p c n", p=128), hTt[:], r=[hk], key=("st_hT", sl))
            pr = psR[sl]; prk = ("psR", sl)
            for c in range(KC):
                P.op("pe", lambda e, c=c, pr=pr, hTt=hTt: e.matmul(pr[:, 0:36], lhsT=hTt[:, c, :], rhs=wr[:, c, :], start=(c == 0), stop=(c == KC - 1)), r=[hk, "wr"], w=[prk])
            L = Lg[sl]; lk = ("Lg", sl); R_ = rt[sl]; rk = ("rt", sl); gt_ = gat[sl]; gk = ("gat", sl)
            P.op("dve", lambda e, L=L, pr=pr: e.tensor_tensor(out=L[:], in0=pr[:, 0:36], in1=br_bc[:], op=ALU.add), r=[prk, "br_bc"], w=[lk])
            P.op("dve", lambda e, L=L, R_=R_: e.tensor_reduce(out=R_[:, 0:1], in_=L[:, 0:4], axis=AX.X, op=ALU.max), r=[lk], w=[rk])
            P.op("dve", lambda e, R_=R_: e.tensor_scalar(out=R_[:, 1:2], in0=R_[:, 0:1], scalar1=-1.0, scalar2=None, op0=ALU.mult), r=[rk], w=[rk])
            P.op("dve", lambda e, L=L, R_=R_: e.tensor_scalar(out=R_[:, 4:8], in0=L[:, 0:4], scalar1=R_[:, 0:1], scalar2=None, op0=ALU.is_ge), r=[lk, rk], w=[rk])
            P.op("act", lambda e, L=L, R_=R_: e.activation(out=R_[:, 8:12], in_=L[:, 0:4], func=AF.Exp, bias=R_[:, 1:2], scale=1.0, accum_out=R_[:, 2:3]), r=[lk, rk], w=[rk])
            P.op("dve", lambda e, R_=R_: e.reciprocal(out=R_[:, 3:4], in_=R_[:, 2:3]), r=[rk], w=[rk])
            P.op("dve", lambda e, L=L, R_=R_: e.tensor_tensor(out=R_[:, 12:44].rearrange("p (g x) -> p g x", g=4), in0=L[:, 4:36].rearrange("p (g x) -> p g x", g=4),
                                                             in1=R_[:, 4:8].unsqueeze(2).to_broadcast([128, 4, 8]), op=ALU.mult), r=[lk, rk], w=[rk])
            P.op("dve", lambda e, R_=R_: e.tensor_reduce(out=R_[:, 44:52], in_=R_[:, 12:44].rearrange("p (g x) -> p x g", g=4), axis=AX.X, op=ALU.add), r=[rk], w=[rk])
            P.op("dve", lambda e, R_=R_: e.tensor_reduce(out=R_[:, 52:53], in_=R_[:, 44:52], axis=AX.X, op=ALU.max), r=[rk], w=[rk])
            P.op("dve", lambda e, R_=R_: e.tensor_scalar(out=R_[:, 54:62], in0=R_[:, 44:52], scalar1=R_[:, 52:53], scalar2=None, op0=ALU.is_ge), r=[rk], w=[rk])
            P.op("dve", lambda e, R_=R_: e.scalar_tensor_tensor(out=R_[:, 62:70], in0=R_[:, 54:62], scalar=-1e30, in1=R_[:, 44:52], op0=ALU.mult, op1=ALU.add), r=[rk], w=[rk])
            P.op("dve", lambda e, R_=R_: e.tensor_reduce(out=R_[:, 53:54], in_=R_[:, 62:70], axis=AX.X, op=ALU.max), r=[rk], w=[rk])
            P.op("dve", lambda e, R_=R_: e.tensor_scalar(out=R_[:, 70:78], in0=R_[:, 62:70], scalar1=R_[:, 53:54], scalar2=None, op0=ALU.is_ge), r=[rk], w=[rk])
            P.op("dve", lambda e, R_=R_: e.tensor_tensor(out=R_[:, 78:79], in0=R_[:, 52:53], in1=R_[:, 53:54], op=ALU.subtract), r=[rk], w=[rk])
            P.op("act", lambda e, R_=R_: e.activation(out=R_[:, 79:80], in_=R_[:, 78:79], func=AF.Sigmoid), r=[rk], w=[rk])
            P.op("dve", lambda e, R_=R_: e.tensor_scalar(out=R_[:, 80:81], in0=R_[:, 79:80], scalar1=-1.0, scalar2=1.0, op0=ALU.mult, op1=ALU.add), r=[rk], w=[rk])
            P.op("dve", lambda e, R_=R_: e.tensor_scalar(out=R_[:, 81:83], in0=R_[:, 79:81], scalar1=R_[:, 3:4], scalar2=None, op0=ALU.mult), r=[rk], w=[rk])
            P.op("dve", lambda e, R_=R_: e.tensor_scalar(out=R_[:, 83:91], in0=R_[:, 54:62], scalar1=R_[:, 81:82], scalar2=None, op0=ALU.mult), r=[rk], w=[rk])
            P.op("dve", lambda e, R_=R_: e.scalar_tensor_tensor(out=R_[:, 44:52], in0=R_[:, 70:78], scalar=R_[:, 82:83], in1=R_[:, 83:91], op0=ALU.mult, op1=ALU.add), r=[rk], w=[rk])
            P.op("dve", lambda e, R_=R_, gt_=gt_: e.tensor_tensor(out=gt_[:], in0=R_[:, 4:8].unsqueeze(2).to_broadcast([128, 4, 8]), in1=R_[:, 44:52].unsqueeze(1).to_broadcast([128, 4, 8]), op=ALU.mult),
                 r=[rk], w=[gk])
            P.dma("sp", g_d[r0:r0 + 128, :], gt_[:].rearrange("p g x -> p (g x)"), r=[gk], key=("st_g", sl))
        P.flush()

    TB = 768
    with ExitStack() as es:
        hT = sb(es, "hT", [128, KC, TB], BF16)
        gates = sb(es, "gates", [128, TB // 128, 32], F32)
        yacc = sb(es, "yacc", [128, TB // 128, D], F32)
        wg = [sb(es, "wg%d" % i, [128, KC, HID], BF16) for i in range(2)]
        wu = [sb(es, "wu%d" % i, [128, KC, HID], BF16) for i in range(2)]
        wd = [sb(es, "wd%d" % i, [128, 4, D], BF16) for i in range(2)]
        hid = [sb(es, "hid%d" % i, [128, 4, TB], BF16) for i in range(2)]
        sg = [sb(es, "sg%d" % i, [128, 384], F32) for i in range(2)]
        psG = [ps(es, "psG%d" % i, [128, 512]) for i in range(2)]
        psU = [ps(es, "psU%d" % i, [128, 512]) for i in range(2)]
        psD = [ps(es, "psD%d" % i, [128, 512]) for i in range(4)]
        ge = 0; gs_ = 0
        for blk in range(NT // TB):
            tok0 = blk * TB
            tiles = [t for t in range(TB // 128) if ctx_out or _tile_row(tok0 // 128 + t) != 2]
            P.dma("sp", hT[:], hT_d[:, tok0:tok0 + TB].rearrange("(c p) n -> p c n", p=128), w=["hT"])
            P.dma("sp", gates[:], g_d[tok0:tok0 + TB, :].rearrange("(t p) e -> p t e", p=128), w=["gates"])
            P.op("pool", lambda e: e.memset(yacc[:], 0.0), w=[("yacc", t) for t in range(TB // 128)])
            for ex in range(NEXP):
                ws = ge % 2; ge += 1
                wgt = wg[ws]; wut = wu[ws]; wdt = wd[ws]; hidt = hid[ws]
                P.dma("pool", wgt[:], G["w_eg"][lw, ex].rearrange("(c p) n -> p c n", p=128), w=[("wg", ws)])
                P.dma("pool", wut[:], G["w_eu"][lw, ex].rearrange("(c p) n -> p c n", p=128), w=[("wu", ws)])
                P.dma("pool", wdt[:], G["w_ed"][lw, ex].rearrange("(c p) n -> p c n", p=128), w=[("wd", ws)])
                for hc in range(4):
                    for hf in range(2):
                        s_ = gs_ % 2; gs_ += 1
                        pg = psG[s_]; pu = psU[s_]; pgk = ("psG", s_); puk = ("psU", s_); sgt = sg[s_]; sgk = ("sg", s_)
                        for c in range(KC):
                            P.op("pe", lambda e, c=c, hc=hc, hf=hf, pg=pg, wgt=wgt: e.matmul(pg[:, 0:384], lhsT=wgt[:, c, hc * 128:(hc + 1) * 128], rhs=hT[:, c, hf * 384:(hf + 1) * 384], start=(c == 0), stop=(c == KC - 1)),
                                 r=["hT", ("wg", ws)], w=[pgk])
                        for c in range(KC):
                            P.op("pe", lambda e, c=c, hc=hc, hf=hf, pu=pu, wut=wut: e.matmul(pu[:, 0:384], lhsT=wut[:, c, hc * 128:(hc + 1) * 128], rhs=hT[:, c, hf * 384:(hf + 1) * 384], start=(c == 0), stop=(c == KC - 1)),
                                 r=["hT", ("wu", ws)], w=[puk])
                        P.op("act", lambda e, pg=pg, sgt=sgt: e.activation(out=sgt[:], in_=pg[:, 0:384], func=AF.Silu), r=[pgk], w=[sgk])
                        P.op("dve", lambda e, pu=pu, sgt=sgt, hidt=hidt, hc=hc, hf=hf: e.tensor_tensor(out=hidt[:, hc, hf * 384:(hf + 1) * 384], in0=pu[:, 0:384], in1=sgt[:], op=ALU.mult),
                             r=[puk, sgk], w=[("hid", ws, hc, hf)])
                for t in tiles:
                    hf = (t * 128) // 384
                    hf2 = (t * 128 + 127) // 384
                    for j in range(4):
                        pd = psD[j]; pdk = ("psD", j)
                        for hc in range(4):
                            P.op("pe", lambda e, hc=hc, j=j, t=t, pd=pd, hidt=hidt, wdt=wdt: e.matmul(pd[:], lhsT=hidt[:, hc, t * 128:(t + 1) * 128], rhs=wdt[:, hc, j * 512:(j + 1) * 512], start=(hc == 0), stop=(hc == 3)),
                                 r=[("hid", ws, hc, hf), ("hid", ws, hc, hf2), ("wd", ws)], w=[pdk])
                        P.op("dve", lambda e, j=j, t=t, pd=pd, ex=ex: e.scalar_tensor_tensor(out=yacc[:, t, j * 512:(j + 1) * 512], in0=pd[:], scalar=gates[:, t, ex:ex + 1], in1=yacc[:, t, j * 512:(j + 1) * 512], op0=ALU.mult, op1=ALU.add),
                             r=[pdk, "gates", ("yacc", t)], w=[("yacc", t)])
            for t in tiles:
                r0 = tok0 + t * 128
                P.dma("sp", ymoe_d[r0:r0 + 128, :], yacc[:, t, :], r=[("yacc", t)], key=("st_y", t))
        P.flush()

    with ExitStack() as es:
        g2 = sb(es, "g2", [128, 3, D], F32)
        xt = [sb(es, "xt%d" % i, [128, D], F32) for i in range(3)]
        yt = [sb(es, "yt%d" % i, [128, D], F32) for i in range(3)]
        for r_ in range(3):
            P.dma("sp", g2[:, r_, :], _bcast_rows(mod_d[r_:r_ + 1, 5 * D:6 * D]), w=[("g2", r_)])
        n = 0
        for gti in range(NTILE):
            row = _tile_row(gti)
            if row == 2 and not ctx_out:
                continue
            sl = n % 3; n += 1
            r0 = gti * 128
            xtt = xt[sl]; ytt = yt[sl]; xk = ("xt", sl); yk = ("yt", sl)
            P.dma("pool", xtt[:], xs[r0:r0 + 128, :], w=[xk])
            P.dma("pool", ytt[:], ymoe_d[r0:r0 + 128, :], w=[yk])
            P.op("dve", lambda e, ytt=ytt, row=row: e.tensor_tensor(out=ytt[:], in0=ytt[:], in1=g2[:, row, :], op=ALU.mult), r=[yk, ("g2", row)], w=[yk])
            P.op("dve", lambda e, xtt=xtt, ytt=ytt: e.tensor_tensor(out=xtt[:], in0=xtt[:], in1=ytt[:], op=ALU.add), r=[xk, yk], w=[xk])
            P.dma("sp", xs[r0:r0 + 128, :], xtt[:], r=[xk], key=("st_x", sl))
        P.flush()


_CONSTS = None


def _consts():
    global _CONSTS
    if _CONSTS is None:
        cosv, sinv = _rope_tables()
        bl, bc = _band_consts()
        _CONSTS = {"rope_cos": cosv, "rope_sin": sinv, "bandL": bl, "bandC": bc,
                   "ident": np.eye(128, dtype=np.float32).astype(ml_dtypes.bfloat16)}
    return _CONSTS


def _pack_layer(inp, li):
    w_re = inp["w_re"][li]
    src = {
        "w_sguT": np.transpose(inp["w_sgu"][li], (0, 2, 1)),
        "w_r": np.concatenate([inp["w_rg"][li], np.transpose(w_re, (1, 0, 2)).reshape(D, 32)], axis=-1),
        "b_r": np.concatenate([inp["b_rg"][li], inp["b_re"][li].reshape(32)], axis=-1),
    }
    bufs = [np.zeros((CHUNK_ROWS[k], 2048), dtype=np.float32) for k in range(NCHUNK)]
    for name, shape, ck, row0 in PACK_SPEC:
        a = src[name] if name in src else inp[name][li]
        a = np.asarray(a, dtype=np.float32).reshape(-1)
        assert a.size == int(np.prod(shape)), (name, a.size, shape)
        bufs[ck].reshape(-1)[row0 * 2048:row0 * 2048 + a.size] = a
    return bufs


_PROGS = {}


def _get_prog(layers, single):
    key = (tuple(layers), single)
    if key not in _PROGS:
        _PROGS[key] = build_program(list(layers), single)
    return _PROGS[key]


def _core_tokens(x, ctx, core):
    parts = []
    for b in range(NB):
        parts.append(x[core * NB + b])
        parts.append(ctx[core * NB + b])
    return np.ascontiguousarray(np.concatenate(parts, axis=0))


FUSED = False


def kernel(**inp):
    inp = {k: np.asarray(v) for k, v in inp.items()}
    x = inp["x"].astype(np.float32, copy=False)
    ctx = inp["ctx"].astype(np.float32, copy=False)
    c = inp["c"]; c_ctx = inp["c_ctx"]
    consts = _consts()
    xin = [_core_tokens(x, ctx, core) for core in range(NCORES)]
    c3 = [np.ascontiguousarray(np.stack([c[core * NB], c[core * NB + 1], c_ctx], axis=0)) for core in range(NCORES)]
    groups = [list(range(DEPTH))] if FUSED else [[li] for li in range(DEPTH)]
    for layers in groups:
        packed = [_pack_layer(inp, li) for li in layers]
        nc = _get_prog(layers, True)
        in_maps = []
        for core in range(NCORES):
            m = dict(consts, xin=xin[core], c3=c3[core])
            for pos in range(len(layers)):
                for k in range(NCHUNK):
                    m["wpk_%d_%d" % (pos, k)] = packed[pos][k]
            in_maps.append(m)
        del packed
        res = run_bass_kernel_spmd(nc, in_maps, core_ids=list(range(NCORES)))
        xin = [np.asarray(r["xout"]) for r in res.results]
        del res, in_maps
    out = np.empty((NCORES * NB, NLAT, D), dtype=np.float32)
    for core in range(NCORES):
        for b in range(NB):
            out[core * NB + b] = xin[core][b * NBT:b * NBT + NLAT]
    return out
```

```python
import math
from contextlib import ExitStack
import numpy as np
import ml_dtypes
import concourse.bass as bass
import concourse.mybir as mybir
from concourse.bass_utils import run_bass_kernel_spmd

F32 = mybir.dt.float32
BF16 = mybir.dt.bfloat16
AF = mybir.ActivationFunctionType
ALU = mybir.AluOpType
AX = mybir.AxisListType

D = 2048
KC = 16
DEPTH = 4
NLAT = 2048
NCTX = 256
NB = 2
NBT = NLAT + NCTX
NT = NB * NBT
IN_COLS = 10048
EPS = 1e-6
ATTN_SCALE = 1.0 / math.sqrt(192.0)
NEXP = 32
HID = 512
POOL_WINDOWS = (2, 4, 8, 16)
NCORES = 8

ENGS = ("pe", "act", "dve", "pool", "sp")

_PACK_SHAPES = [
    ("w_ada", (2048, 12288)), ("b_ada", (12288,)), ("w_in", (2048, 10048)),
    ("g_cq", (512,)), ("g_ckv", (256,)), ("w_uq", (512, 1536)), ("w_ukv", (256, 2048)),
    ("g_q", (192,)), ("g_k", (192,)), ("w_sguT", (8, 128, 128)), ("b_sgu", (1024,)), ("g_sgu", (1024,)),
    ("w_pool", (4, 256, 256)), ("s_pool", (1024,)),
    ("w_ao", (1024, 2048)), ("w_bo", (1024, 2048)), ("w_co", (1024, 2048)), ("w_out", (2048, 2048)),
    ("w_r", (2048, 36)), ("b_r", (36,)),
    ("w_e_gate", (32, 2048, 512)), ("w_e_up", (32, 2048, 512)), ("w_e_down", (32, 512, 2048)),
]
_CHUNK_OF = {"w_e_gate": 1, "w_e_up": 1, "w_e_down": 2}
CHUNK_ROWS = [32768, 32768, 16384]
NCHUNK = 3
PACK_SPEC = []
_rows = [0, 0, 0]
for _n, _s in _PACK_SHAPES:
    _c = _CHUNK_OF.get(_n, 0)
    PACK_SPEC.append((_n, _s, _c, _rows[_c]))
    _rows[_c] += -(-int(np.prod(_s)) // 2048)
assert all(_rows[i] <= CHUNK_ROWS[i] for i in range(3)), _rows
STOP_AFTER = None


class _Op:
    __slots__ = ("eng", "emit", "deps", "dma", "sig", "ord", "dsem", "dval", "idx", "dmadeps")

    def __init__(self, eng, emit, dma):
        self.eng = eng
        self.emit = emit
        self.dma = dma
        self.deps = []
        self.dmadeps = []
        self.sig = False
        self.ord = 0
        self.dsem = None
        self.dval = 0


class Prog:
    def __init__(self, nc, es, n_dma_sems=72):
        self.nc = nc
        self.es = es
        self.nepoch = 0
        self.esem = {e: es.enter_context(nc.semaphore("S_" + e)) for e in ENGS}
        self.ecount = {e: 0 for e in ENGS}
        self.bar = es.enter_context(nc.semaphore("BAR"))
        self.nbar = 0
        self.dsems = [es.enter_context(nc.semaphore("DQ%d" % i)) for i in range(n_dma_sems)]
        self.dtotal = [0] * n_dma_sems
        self.seen = {e: {} for e in ENGS}
        self._reset_phase()

    def new_epoch(self):
        self.nepoch += 1
        self.esem = {e: self.es.enter_context(self.nc.semaphore("S_%s_%d" % (e, self.nepoch))) for e in ENGS}
        self.ecount = {e: 0 for e in ENGS}
        for e in ENGS:
            for k in [k for k in self.seen[e] if isinstance(k, str)]:
                del self.seen[e][k]

    def _reset_phase(self):
        self.ops = {e: [] for e in ENGS}
        self.lastw = {}
        self.readers = {}
        self.keymap = {}

    def _track(self, o, r, w):
        deps = []
        for k in r:
            x = self.lastw.get(k)
            if x is not None:
                deps.append(x)
        for k in w:
            x = self.lastw.get(k)
            if x is not None:
                deps.append(x)
            deps.extend(self.readers.get(k, ()))
        seen = set()
        for d in deps:
            if id(d) in seen or d is o:
                continue
            seen.add(id(d))
            if d.dma:
                o.dmadeps.append((d.dsem, self.dtotal[d.dsem]))
            else:
                if d.eng == "pe" and o.eng == "pe" and not o.dma:
                    continue
                d.sig = True
                o.deps.append(d)
        for k in r:
            self.readers.setdefault(k, []).append(o)
        for k in w:
            self.lastw[k] = o
            self.readers[k] = []

    def op(self, eng, emit, r=(), w=()):
        o = _Op(eng, emit, False)
        self._track(o, r, w)
        self.ops[eng].append(o)
        return o

    def dma(self, q, out, in_, r=(), w=(), key=None, **kw):
        if key is None:
            key = w[0] if len(w) else r[0]
        if key not in self.keymap:
            self.keymap[key] = len(self.keymap)
            assert len(self.keymap) <= len(self.dsems), "too many dma keys"
        si = self.keymap[key]
        o = _Op(q, lambda e: e.dma_start(out=out, in_=in_, **kw), True)
        o.dsem = si
        self._track(o, r, w)
        self.dtotal[si] += 16
        o.dval = self.dtotal[si]
        self.ops[q].append(o)
        return o

    def flush(self, name=None):
        nc = self.nc
        last_compute = {}
        for e in ENGS:
            for o in self.ops[e]:
                if not o.dma:
                    last_compute[e] = o
        for e, o in last_compute.items():
            o.sig = True
        for e in ENGS:
            for o in self.ops[e]:
                if (not o.dma) and o.sig:
                    self.ecount[e] += 1
                    o.ord = self.ecount[e]
        self.nbar += 1
        nbar = self.nbar
        used_dsems = sorted(set(self.keymap.values()))

        def run(e, eng):
            seen = self.seen[e]

            def wait(sem, sid, val):
                if seen.get(sid, 0) >= val:
                    return
                seen[sid] = val
                eng.wait_ge(sem, val)

            my_dsems = set()
            for o in self.ops[e]:
                for d in o.deps:
                    wait(self.esem[d.eng], "E" + d.eng, d.ord)
                for (si, val) in o.dmadeps:
                    wait(self.dsems[si], si, val)
                ins = o.emit(eng)
                if o.dma:
                    ins.then_inc(self.dsems[o.dsem], 16)
                    my_dsems.add(o.dsem)
                elif o.sig:
                    ins.then_inc(self.esem[e], 1)
            if e in last_compute:
                wait(self.esem[e], "E" + e, last_compute[e].ord)
            for si in sorted(my_dsems):
                wait(self.dsems[si], si, self.dtotal[si])
            eng.sem_inc(self.bar, 1)
            eng.wait_ge(self.bar, len(ENGS) * nbar)

        with nc.Block() as block:
            block.tensor(lambda eng: run("pe", eng))
            block.scalar(lambda eng: run("act", eng))
            block.vector(lambda eng: run("dve", eng))
            block.gpsimd(lambda eng: run("pool", eng))
            block.sync(lambda eng: run("sp", eng))
        self._reset_phase()


def _rope_tables():
    t = np.arange(NLAT)
    row = (t // 64).astype(np.float32)
    col = (t % 64).astype(np.float32)
    inv = (10000.0 ** (-np.arange(0, 32, 2, dtype=np.float32) / 32.0)).astype(np.float32)
    ang = np.stack([row[:, None] * inv, col[:, None] * inv], axis=1).astype(np.float32)
    return np.cos(ang).astype(np.float32).reshape(NLAT, 32), np.sin(ang).astype(np.float32).reshape(NLAT, 32)


def _pool_matT(n, w):
    t = np.arange(n)
    lo = np.clip(t - w // 2, 0, n)
    hi = np.clip(t + w // 2, 0, n)
    A = np.zeros((n, n), dtype=np.float64)
    for i in range(n):
        A[i, lo[i]:hi[i]] = 1.0 / float(hi[i] - lo[i])
    A -= np.eye(n)
    return A.T.astype(np.float32)


def _band_consts():
    bl = np.zeros((4, 4, 6, 128, 512), dtype=np.float32)
    bc = np.zeros((4, 2, 128, 256), dtype=np.float32)
    for gi, w in enumerate(POOL_WINDOWS):
        MT = _pool_matT(NLAT, w)
        for j in range(4):
            for si in range(6):
                s = 4 * j - 1 + si
                if 0 <= s < 16:
                    bl[gi, j, si] = MT[s * 128:(s + 1) * 128, j * 512:(j + 1) * 512]
        MC = _pool_matT(NCTX, w)
        for s in range(2):
            bc[gi, s] = MC[s * 128:(s + 1) * 128, :]
    return bl.astype(ml_dtypes.bfloat16), bc.astype(ml_dtypes.bfloat16)


def _blocks(include_ctx=True):
    out = []
    for b in range(NB):
        for j in range(4):
            out.append((b * NBT + j * 512, 512, b, False, b, j * 512))
        if include_ctx:
            out.append((b * NBT + NLAT, NCTX, 2, True, b, 0))
    return out


def _bcast_rows(ap2d, nparts=128):
    t = ap2d.partition_broadcast(nparts)
    if len(t.shape) == 3:
        t = t[:, 0, :]
    return t


def build_program(layers, single_layer_inputs, debug=None):
    nc = bass.Bass("TRN2", target_bir_lowering=False)

    def din(name, shape, dt=F32):
        return nc.dram_tensor(name, list(shape), dt, kind="ExternalInput").ap()

    xin = din("xin", [NT, D])
    c3 = din("c3", [3, D])
    nL = len(layers)
    gath = [[nc.dram_tensor("wpk_%d_%d" % (pos, k), [CHUNK_ROWS[k], 2048], F32, kind="ExternalInput") for k in range(NCHUNK)] for pos in range(nL)]

    def layer_views(pos):
        v = {}
        for name, shape, ck, row0 in PACK_SPEC:
            off = row0 * 2048
            ap = []
            stride = 1
            for d_ in reversed(shape):
                ap.insert(0, [stride, d_])
                stride *= d_
            ap.insert(0, [0, 1])
            v[name] = bass.AP(gath[pos][ck], off, ap)
        v["w_eg"] = v["w_e_gate"]; v["w_eu"] = v["w_e_up"]; v["w_ed"] = v["w_e_down"]
        return v
    rope_cos = din("rope_cos", [NLAT, 32])
    rope_sin = din("rope_sin", [NLAT, 32])
    bandL = din("bandL", [4, 4, 6, 128, 512], BF16)
    bandC = din("bandC", [4, 2, 128, 256], BF16)
    ident_in = din("ident", [128, 128], BF16)

    xout = nc.dram_tensor("xout", [NT, D], F32, kind="ExternalOutput").ap()
    dbg_out = None

    def scratch(name, shape, dt):
        if debug and name in debug:
            return nc.dram_tensor(name, list(shape), dt, kind="ExternalOutput").ap()
        return nc.dram_tensor(name, list(shape), dt).ap()

    xs = xout
    mod_d = scratch("mod_d", [3, 6 * D], F32)
    hT_d = scratch("hT_d", [D, NT], BF16)
    qT_d = scratch("qT_d", [8, 192, NT], BF16)
    knT_d = scratch("knT_d", [8, 128, NT], BF16)
    krT_d = scratch("krT_d", [64, NT], BF16)
    v_d = scratch("v_d", [NT, 8, 128], BF16)
    r_d = scratch("r_d", [NT, 8], F32)
    bT_d = scratch("bT_d", [1024, NT], BF16)
    cpT_d = scratch("cpT_d", [1024, NT], BF16)
    aT_d = scratch("aT_d", [1024, NT], BF16)
    yT_d = scratch("yT_d", [D, NT], BF16)
    g_d = scratch("g_d", [NT, 32], F32)
    ymoe_d = scratch("ymoe_d", [NT, D], F32)

    with ExitStack() as gs:
        P = Prog(nc, gs)

        uid = [0]

        def sb(es, name, shape, dt):
            uid[0] += 1
            return es.enter_context(nc.sbuf_tensor("%s_s%d" % (name, uid[0]), list(shape), dt))

        def ps(es, name, shape, dt=F32):
            uid[0] += 1
            return es.enter_context(nc.psum_tensor("%s_p%d" % (name, uid[0]), list(shape), dt))

        for i in range(4):
            r0 = i * (NT // 4)
            P.dma("sp", xs[r0:r0 + NT // 4, :], xin[r0:r0 + NT // 4, :], w=[("xs", i)])
        P.flush()

        base_G = dict(locals())
        for li_pos, li in enumerate(layers):
            last_layer = (li == DEPTH - 1)
            ctx_out = not last_layer
            if li_pos > 0:
                P.new_epoch()
            Gl = dict(base_G)
            Gl.update(layer_views(li_pos))
            _layer(nc, P, sb, ps, 0, ctx_out, Gl, None)
    return nc


def _load_modT(nc, P, es, sb, mod_d, j, name):
    t = sb(es, name, [128, 3, 16], F32)
    for r_ in range(3):
        src = bass.AP(mod_d.tensor, r_ * 6 * D + j * D, [[1, 128], [128, 16]])
        P.dma("sp", t[:, r_, :], src, w=[name], allow_slow_non_contiguous=True)
    return t


def _norm_mod_tile(P, xt, xkey, jk, jkey, st, stkey, xnb, xnkey, psT, pskey, ident, tmpf, tmpkey,
                   scp1, shv, row, hT_dst, hkey, extra_r=()):
    P.op("act", lambda e: e.activation(out=jk[:], in_=xt[:], func=AF.Square, accum_out=st[:, 0:1]),
         r=[xkey], w=[jkey, stkey])
    P.op("act", lambda e: e.activation(out=st[:, 1:2], in_=st[:, 0:1], func=AF.Sqrt, bias=EPS, scale=1.0 / D),
         r=[stkey], w=[stkey])
    P.op("dve", lambda e: e.reciprocal(out=st[:, 2:3], in_=st[:, 1:2]), r=[stkey], w=[stkey])
    P.op("dve", lambda e: e.tensor_scalar(out=xnb[:], in0=xt[:], scalar1=st[:, 2:3], scalar2=None, op0=ALU.mult),
         r=[xkey, stkey], w=[xnkey])
    for c in range(KC):
        P.op("pe", lambda e, c=c: e.transpose(out=psT[:, c, :], in_=xnb[:, c * 128:(c + 1) * 128], identity=ident[:]),
             r=[xnkey, "ident"], w=[pskey])
    P.op("dve", lambda e: e.tensor_tensor(out=tmpf[:], in0=psT[:], in1=scp1[:, row, :].unsqueeze(2).to_broadcast([128, KC, 128]),
                                          op=ALU.mult), r=[pskey, "modsc"] + list(extra_r), w=[tmpkey])
    P.op("dve", lambda e: e.tensor_tensor(out=hT_dst, in0=tmpf[:], in1=shv[:, row, :].unsqueeze(2).to_broadcast([128, KC, 128]),
                                          op=ALU.add), r=[tmpkey, "modsh"], w=[hkey])


def _layer(nc, P, sb, ps, lw, ctx_out, G, dbg_out):
    xs = G["xs"]; c3 = G["c3"]; mod_d = G["mod_d"]
    w_ada = G["w_ada"]; b_ada = G["b_ada"]; w_in = G["w_in"]
    hT_d = G["hT_d"]; qT_d = G["qT_d"]; knT_d = G["knT_d"]; krT_d = G["krT_d"]; v_d = G["v_d"]; r_d = G["r_d"]
    bT_d = G["bT_d"]; cpT_d = G["cpT_d"]; aT_d = G["aT_d"]; yT_d = G["yT_d"]
    ident_in = G["ident_in"]
    blocks_all = _blocks(True)
    blocks_out = _blocks(ctx_out)

    with ExitStack() as es:
        c3T = sb(es, "c3T", [128, 3, KC], F32)
        scT = sb(es, "scT", [128, KC, 3], F32)
        bias3 = sb(es, "bias3", [3, 6 * D], F32)
        modsb = sb(es, "modsb", [3, 6 * D], F32)
        wts = [sb(es, "wada%d" % i, [128, KC, 512], F32) for i in range(2)]
        pms = [ps(es, "pmod%d" % i, [3, 512]) for i in range(2)]
        for r_ in range(3):
            P.dma("sp", c3T[:, r_, :], bass.AP(c3.tensor, r_ * D, [[1, 128], [128, KC]]), w=["c3T"], allow_slow_non_contiguous=True)
        P.dma("sp", bias3[:], _bcast_rows(b_ada[lw:lw + 1, :], 3), w=["bias3"])
        P.op("act", lambda e: e.activation(out=scT[:], in_=c3T[:].rearrange("p r c -> p c r"), func=AF.Silu), r=["c3T"], w=["scT"])
        for jc in range(24):
            wt = wts[jc % 2]; pm = pms[jc % 2]
            wk = ("wada", jc % 2); pk = ("pmod", jc % 2)
            P.dma("sp", wt[:], w_ada[lw, :, jc * 512:(jc + 1) * 512].rearrange("(c p) n -> p c n", p=128), w=[wk])
            for c in range(KC):
                P.op("pe", lambda e, c=c, wt=wt, pm=pm: e.matmul(pm[:], lhsT=scT[:, c, :], rhs=wt[:, c, :], start=(c == 0), stop=(c == KC - 1)),
                     r=["scT", wk], w=[pk])
            P.op("dve", lambda e, pm=pm, jc=jc: e.tensor_tensor(out=modsb[:, jc * 512:(jc + 1) * 512], in0=pm[:], in1=bias3[:, jc * 512:(jc + 1) * 512], op=ALU.add),
                 r=[pk, "bias3"], w=[("modsb", jc)])
        P.dma("sp", mod_d[:, :], modsb[:], r=[("modsb", jc) for jc in range(24)], w=["mod_d"])
        P.flush()

    if dbg_out is not None and "mod" in dbg_out:
        with ExitStack() as es:
            t = sb(es, "dbgmod", [3, 6 * D], F32)
            P.dma("sp", t[:], mod_d[:, :], w=["t"])
            P.dma("sp", dbg_out["mod"][:, :], t[:], r=["t"], w=["o"])
            P.flush()

    phases = [("m1a", _phase_m1a), ("sgu", _phase_sgu), ("pool", _phase_pool), ("attn", _phase_attn),
              ("merge", _phase_merge), ("outproj", _phase_outproj), ("moe", _phase_moe)]
    for name, fn in phases:
        if STOP_AFTER is not None and STOP_AFTER == "adaln":
            break
        fn(nc, P, sb, ps, lw, ctx_out, G)
        if STOP_AFTER is not None and STOP_AFTER == name:
            break


def _phase_m1a(nc, P, sb, ps, lw, ctx_out, G):
    xs = G["xs"]; mod_d = G["mod_d"]; w_in = G["w_in"]
    hT_d = G["hT_d"]; qT_d = G["qT_d"]; knT_d = G["knT_d"]; krT_d = G["krT_d"]; v_d = G["v_d"]; r_d = G["r_d"]
    with ExitStack() as es:
        win_a = sb(es, "win_a", [128, KC, 832], BF16)
        wuq = sb(es, "wuq", [128, 4, 1536], BF16)
        wukv = sb(es, "wukv", [128, 2, 2048], BF16)
        ident = sb(es, "ident", [128, 128], BF16)
        gT6 = sb(es, "gT6", [128, 6], F32)
        gq_bc = sb(es, "gq_bc", [128, 192], F32)
        gk_bc = sb(es, "gk_bc", [128, 192], F32)
        xt = [sb(es, "xt%d" % i, [128, D], F32) for i in range(2)]
        jk = sb(es, "jk", [128, D], BF16)
        st = [sb(es, "st%d" % i, [128, 16], F32) for i in range(2)]
        s8 = [sb(es, "s8_%d" % i, [128, 4, 8], F32) for i in range(2)]
        xnb = [sb(es, "xnb%d" % i, [128, D], BF16) for i in range(2)]
        tmpf = sb(es, "tmpf", [128, KC, 128], F32)
        hT = sb(es, "hT", [128, KC, 512], BF16)
        cn = [sb(es, "cn%d" % i, [128, 768], BF16) for i in range(2)]
        krf = sb(es, "krf", [128, 4, 64], F32)
        cT = sb(es, "cT", [128, 6, 512], BF16)
        sqf = sb(es, "sqf", [128, 1536], F32)
        qg = sb(es, "qg", [128, 8, 192], F32)
        qr = sb(es, "qr", [128, 8, 64], F32)
        rtmp = sb(es, "rtmp", [128, 4, 8, 32], F32)
        qb = sb(es, "qb", [128, 8, 192], BF16)
        qTn_blk = sb(es, "qTn_blk", [128, 8, 512], BF16)
        qTr_blk = sb(es, "qTr_blk", [64, 8, 512], BF16)
        kTn_blk = sb(es, "kTn_blk", [128, 8, 512], BF16)
        krT_blk = sb(es, "krT_blk", [64, 512], BF16)
        knb = sb(es, "knb", [128, 8, 128], BF16)
        vb = [sb(es, "vb%d" % i, [128, 8, 128], BF16) for i in range(2)]
        krg = sb(es, "krg", [128, 64], F32)
        krb = sb(es, "krb", [128, 64], BF16)
        ktmp = sb(es, "ktmp", [128, 4, 32], F32)
        rk_blk = sb(es, "rk_blk", [128, 4, 8], F32)
        cs = [sb(es, "cos%d" % i, [128, 32], F32) for i in range(2)]
        sn = [sb(es, "sin%d" % i, [128, 32], F32) for i in range(2)]
        psT = ps(es, "psT", [128, 2048], BF16)
        psA = ps(es, "psA", [128, 512])
        psB = ps(es, "psB", [128, 512])
        psC = ps(es, "psC", [128, 1024], BF16)
        psQ = ps(es, "psQ", [128, 1536])
        psT3 = psT[:].rearrange("p (c n) -> p c n", c=KC)

        P.dma("pool", win_a[:], w_in[lw, :, 0:832].rearrange("(c p) n -> p c n", p=128), w=["win_a"])
        P.dma("pool", wuq[:], G["w_uq"][lw].rearrange("(c p) n -> p c n", p=128), w=["wuq"])
        P.dma("pool", wukv[:], G["w_ukv"][lw].rearrange("(c p) n -> p c n", p=128), w=["wukv"])
        P.dma("sp", ident[:], G["ident_in"][:, :], w=["ident"])
        P.dma("sp", gT6[:, 0:4], bass.AP(G["g_cq"].tensor, G["g_cq"].offset, [[1, 128], [128, 4]]), w=["gT6a"], allow_slow_non_contiguous=True)
        P.dma("sp", gT6[:, 4:6], bass.AP(G["g_ckv"].tensor, G["g_ckv"].offset, [[1, 128], [128, 2]]), w=["gT6b"], allow_slow_non_contiguous=True)
        P.dma("sp", gq_bc[:], _bcast_rows(G["g_q"][lw:lw + 1, :]), w=["gq_bc"])
        P.dma("sp", gk_bc[:], _bcast_rows(G["g_k"][lw:lw + 1, :]), w=["gk_bc"])
        sc1 = _load_modT(nc, P, es, sb, mod_d, 1, "modsc")
        sh1 = _load_modT(nc, P, es, sb, mod_d, 0, "modsh")
        P.op("dve", lambda e: e.tensor_scalar(out=sc1[:], in0=sc1[:], scalar1=1.0, scalar2=None, op0=ALU.add), r=["modsc"], w=["modsc"])

        gt = 0
        for (tok0, ntok, row, is_ctx, b, pos0) in _blocks(True):
            need_q = ctx_out or (not is_ctx)
            ntile = ntok // 128
            for t in range(ntile):
                sl = gt % 2; gt += 1
                r0 = tok0 + t * 128
                P.dma("pool", xt[sl][:], xs[r0:r0 + 128, :], w=[("xt", sl)])
                _norm_mod_tile(P, xt[sl], ("xt", sl), jk, "jk", st[sl], ("st", sl), xnb[sl], ("xnb", sl), psT3, "psT", ident,
                               tmpf, "tmpf", sc1, sh1, row, hT[:, :, t * 128:(t + 1) * 128], ("hT", t))
            P.dma("sp", hT_d[:, tok0:tok0 + ntok].rearrange("(c p) n -> p c n", p=128), hT[:, :, 0:ntok],
                  r=[("hT", t) for t in range(ntile)], key="st_hT")
            for t in range(ntile):
                sl = t % 2
                stt = st[sl]; sk = ("st", sl)
                for c in range(KC):
                    P.op("pe", lambda e, c=c, t=t: e.matmul(psA[:], lhsT=hT[:, c, t * 128:(t + 1) * 128], rhs=win_a[:, c, 0:512], start=(c == 0), stop=(c == KC - 1)),
                         r=[("hT", t), "win_a"], w=["psA"])
                for c in range(KC):
                    P.op("pe", lambda e, c=c, t=t: e.matmul(psB[:, 0:320], lhsT=hT[:, c, t * 128:(t + 1) * 128], rhs=win_a[:, c, 512:832], start=(c == 0), stop=(c == KC - 1)),
                         r=[("hT", t), "win_a"], w=["psB"])
                P.op("act", lambda e, stt=stt: e.activation(out=jk[:, 0:512], in_=psA[:], func=AF.Square, accum_out=stt[:, 3:4]), r=["psA"], w=["jk", sk])
                P.op("act", lambda e, stt=stt: e.activation(out=jk[:, 512:768], in_=psB[:, 0:256], func=AF.Square, accum_out=stt[:, 6:7]), r=["psB"], w=["jk", sk])
                P.op("act", lambda e, stt=stt: e.activation(out=stt[:, 4:5], in_=stt[:, 3:4], func=AF.Sqrt, bias=EPS, scale=1.0 / 512), r=[sk], w=[sk])
                P.op("act", lambda e, stt=stt: e.activation(out=stt[:, 7:8], in_=stt[:, 6:7], func=AF.Sqrt, bias=EPS, scale=1.0 / 256), r=[sk], w=[sk])
                P.op("dve", lambda e, stt=stt: e.reciprocal(out=stt[:, 5:6], in_=stt[:, 4:5]), r=[sk], w=[sk])
                P.op("dve", lambda e, stt=stt: e.reciprocal(out=stt[:, 8:9], in_=stt[:, 7:8]), r=[sk], w=[sk])
                cnt = cn[sl]; ck = ("cn", sl)
                P.op("dve", lambda e, stt=stt, cnt=cnt: e.tensor_scalar(out=cnt[:, 0:512], in0=psA[:], scalar1=stt[:, 5:6], scalar2=None, op0=ALU.mult), r=["psA", sk], w=[ck])
                P.op("dve", lambda e, stt=stt, cnt=cnt: e.tensor_scalar(out=cnt[:, 512:768], in0=psB[:, 0:256], scalar1=stt[:, 8:9], scalar2=None, op0=ALU.mult), r=["psB", sk, ck], w=[ck])
                P.op("act", lambda e, t=t: e.copy(out=krf[:, t, :], in_=psB[:, 256:320]), r=["psB"], w=[("krf", t)])
                for c in range(6):
                    P.op("pe", lambda e, c=c, cnt=cnt: e.transpose(out=psC[:, c * 128:(c + 1) * 128], in_=cnt[:, c * 128:(c + 1) * 128], identity=ident[:]),
                         r=[ck, "ident"], w=["psC"])
                P.op("dve", lambda e, t=t: e.tensor_tensor(out=cT[:, :, t * 128:(t + 1) * 128], in0=psC[:, 0:768].rearrange("p (c n) -> p c n", c=6),
                                                         in1=gT6[:].unsqueeze(2).to_broadcast([128, 6, 128]), op=ALU.mult),
                     r=["psC", "gT6a", "gT6b"], w=[("cT", t)])
            for t in range(ntile):
                sl = t % 2
                r0 = tok0 + t * 128
                if not is_ctx:
                    P.dma("pool", cs[sl][:], G["rope_cos"][pos0 + t * 128:pos0 + (t + 1) * 128, :], w=[("cos", sl)])
                    P.dma("pool", sn[sl][:], G["rope_sin"][pos0 + t * 128:pos0 + (t + 1) * 128, :], w=[("sin", sl)])
                cosb = cs[sl]; sinb = sn[sl]; ckk = ("cos", sl); skk = ("sin", sl)
                s8t = s8[sl]; s8k = ("s8", sl)
                if need_q:
                    for j in range(3):
                        for c in range(4):
                            P.op("pe", lambda e, c=c, j=j, t=t: e.matmul(psQ[:, j * 512:(j + 1) * 512], lhsT=cT[:, c, t * 128:(t + 1) * 128], rhs=wuq[:, c, j * 512:(j + 1) * 512], start=(c == 0), stop=(c == 3)),
                                 r=[("cT", t), "wuq"], w=["psQ"])
                    psQ3 = psQ[:].rearrange("p (h d) -> p h d", h=8)
                    P.op("act", lambda e: e.activation(out=sqf[:], in_=psQ[:], func=AF.Square), r=["psQ"], w=["sqf"])
                    P.op("dve", lambda e, s8t=s8t: e.tensor_reduce(out=s8t[:, 0, :], in_=sqf[:].rearrange("p (h d) -> p h d", h=8), axis=AX.X, op=ALU.add), r=["sqf"], w=[s8k])
                    P.op("act", lambda e, s8t=s8t: e.activation(out=s8t[:, 1, :], in_=s8t[:, 0, :], func=AF.Sqrt, bias=EPS, scale=1.0 / 192), r=[s8k], w=[s8k])
                    P.op("dve", lambda e, s8t=s8t: e.reciprocal(out=s8t[:, 2, :], in_=s8t[:, 1, :]), r=[s8k], w=[s8k])
                    P.op("dve", lambda e, s8t=s8t, psQ3=psQ3: e.tensor_tensor(out=qg[:], in0=psQ3, in1=s8t[:, 2, :].unsqueeze(2).to_broadcast([128, 8, 192]), op=ALU.mult), r=["psQ", s8k], w=["qg"])
                    P.op("dve", lambda e: e.tensor_tensor(out=qb[:, :, 0:128], in0=qg[:, :, 0:128], in1=gq_bc[:, 0:128].unsqueeze(1).to_broadcast([128, 8, 128]), op=ALU.mult), r=["qg", "gq_bc"], w=["qbn"])
                    if is_ctx:
                        P.op("dve", lambda e: e.tensor_tensor(out=qb[:, :, 128:192], in0=qg[:, :, 128:192], in1=gq_bc[:, 128:192].unsqueeze(1).to_broadcast([128, 8, 64]), op=ALU.mult), r=["qg", "gq_bc"], w=["qbr"])
                    else:
                        P.op("dve", lambda e: e.tensor_tensor(out=qr[:], in0=qg[:, :, 128:192], in1=gq_bc[:, 128:192].unsqueeze(1).to_broadcast([128, 8, 64]), op=ALU.mult), r=["qg", "gq_bc"], w=["qr"])
                        q5 = qr[:].rearrange("p h (a s f) -> p h a s f", a=2, s=2)
                        o5 = qb[:, :, 128:192].rearrange("p h (a s f) -> p h a s f", a=2, s=2)
                        x1 = q5[:, :, :, 0, :]; x2 = q5[:, :, :, 1, :]
                        cb = cosb[:].rearrange("p (a f) -> p a f", a=2).unsqueeze(1).to_broadcast([128, 8, 2, 16])
                        sbb = sinb[:].rearrange("p (a f) -> p a f", a=2).unsqueeze(1).to_broadcast([128, 8, 2, 16])
                        tv = [rtmp[:, i].rearrange("p h (a f) -> p h a f", a=2) for i in range(4)]
                        P.op("dve", lambda e, x1=x1, cb=cb, tv=tv: e.tensor_tensor(out=tv[0], in0=x1, in1=cb, op=ALU.mult), r=["qr", ckk], w=[("rt", 0)])
                        P.op("dve", lambda e, x2=x2, sbb=sbb, tv=tv: e.tensor_tensor(out=tv[1], in0=x2, in1=sbb, op=ALU.mult), r=["qr", skk], w=[("rt", 1)])
                        P.op("dve", lambda e, x1=x1, sbb=sbb, tv=tv: e.tensor_tensor(out=tv[2], in0=x1, in1=sbb, op=ALU.mult), r=["qr", skk], w=[("rt", 2)])
                        P.op("dve", lambda e, x2=x2, cb=cb, tv=tv: e.tensor_tensor(out=tv[3], in0=x2, in1=cb, op=ALU.mult), r=["qr", ckk], w=[("rt", 3)])
                        P.op("dve", lambda e, o5=o5, tv=tv: e.tensor_tensor(out=o5[:, :, :, 0, :], in0=tv[0], in1=tv[1], op=ALU.subtract), r=[("rt", 0), ("rt", 1)], w=["qbr"])
                        P.op("dve", lambda e, o5=o5, tv=tv: e.tensor_tensor(out=o5[:, :, :, 1, :], in0=tv[2], in1=tv[3], op=ALU.add), r=[("rt", 2), ("rt", 3), "qbr"], w=["qbr"])
                    for h in range(8):
                        P.op("pe", lambda e, h=h: e.transpose(out=psT[:, h * 128:(h + 1) * 128], in_=qb[:, h, 0:128], identity=ident[:]), r=["qbn", "ident"], w=["psT"])
                    for h in range(8):
                        P.op("pe", lambda e, h=h: e.transpose(out=psT[0:64, 1024 + h * 128:1024 + (h + 1) * 128], in_=qb[:, h, 128:192], identity=ident[:]), r=["qbr", "ident"], w=["psT"])
                    P.op("act", lambda e, t=t: e.copy(out=qTn_blk[:, :, t * 128:(t + 1) * 128], in_=psT[:, 0:1024].rearrange("p (h n) -> p h n", h=8)), r=["psT"], w=[("qTn", t)])
                    P.op("act", lambda e, t=t: e.copy(out=qTr_blk[:, :, t * 128:(t + 1) * 128], in_=psT[0:64, 1024:2048].rearrange("p (h n) -> p h n", h=8)), r=["psT"], w=[("qTr", t)])
                vbt = vb[sl]; vk = ("vb", sl)
                for hh in range(2):
                    for j in range(2):
                        for c in range(2):
                            P.op("pe", lambda e, c=c, j=j, hh=hh, t=t: e.matmul(psQ[:, j * 512:(j + 1) * 512], lhsT=cT[:, 4 + c, t * 128:(t + 1) * 128],
                                                                              rhs=wukv[:, c, hh * 1024 + j * 512:hh * 1024 + (j + 1) * 512], start=(c == 0), stop=(c == 1)),
                                 r=[("cT", t), "wukv"], w=["psQ"])
                    kv4 = psQ[:, 0:1024].rearrange("p (h d) -> p h d", h=4)
                    P.op("act", lambda e, kv4=kv4: e.activation(out=sqf[:, 0:512].rearrange("p (h d) -> p h d", h=4), in_=kv4[:, :, 0:128], func=AF.Square), r=["psQ"], w=["sqf"])
                    P.op("dve", lambda e, s8t=s8t, hh=hh: e.tensor_reduce(out=s8t[:, 0, hh * 4:(hh + 1) * 4], in_=sqf[:, 0:512].rearrange("p (h d) -> p h d", h=4), axis=AX.X, op=ALU.add), r=["sqf", s8k], w=[s8k])
                    P.op("dve", lambda e, kv4=kv4, hh=hh: e.tensor_tensor(out=knb[:, hh * 4:(hh + 1) * 4, :], in0=kv4[:, :, 0:128], in1=gk_bc[:, 0:128].unsqueeze(1).to_broadcast([128, 4, 128]), op=ALU.mult),
                         r=["psQ", "gk_bc"], w=[("knb", hh)])
                    P.op("act", lambda e, kv4=kv4, hh=hh, vbt=vbt: e.copy(out=vbt[:, hh * 4:(hh + 1) * 4, :], in_=kv4[:, :, 128:256]), r=["psQ"], w=[vk])
                P.dma("sp", v_d[r0:r0 + 128, :, :], vbt[:], r=[vk], key=("st_v", sl))
                stt = st[sl]; sk = ("st", sl)
                P.op("act", lambda e, stt=stt, t=t: e.activation(out=jk[:, 0:64], in_=krf[:, t, :], func=AF.Square, accum_out=stt[:, 9:10]), r=[("krf", t)], w=["jk", sk])
                P.op("dve", lambda e, s8t=s8t, stt=stt: e.tensor_scalar(out=s8t[:, 1, :], in0=s8t[:, 0, :], scalar1=stt[:, 9:10], scalar2=None, op0=ALU.add), r=[s8k, sk], w=[s8k])
                P.op("act", lambda e, s8t=s8t: e.activation(out=s8t[:, 2, :], in_=s8t[:, 1, :], func=AF.Sqrt, bias=EPS, scale=1.0 / 192), r=[s8k], w=[s8k])
                P.op("dve", lambda e, s8t=s8t: e.reciprocal(out=s8t[:, 3, :], in_=s8t[:, 2, :]), r=[s8k], w=[s8k])
                P.op("dve", lambda e, s8t=s8t, t=t: e.tensor_scalar(out=rk_blk[:, t, :], in0=s8t[:, 3, :], scalar1=ATTN_SCALE, scalar2=None, op0=ALU.mult), r=[s8k], w=[("rk", t)])
                for h in range(8):
                    P.op("pe", lambda e, h=h: e.transpose(out=psT[:, h * 128:(h + 1) * 128], in_=knb[:, h, :], identity=ident[:]), r=[("knb", 0), ("knb", 1), "ident"], w=["psT"])
                P.op("act", lambda e, t=t: e.copy(out=kTn_blk[:, :, t * 128:(t + 1) * 128], in_=psT[:, 0:1024].rearrange("p (h n) -> p h n", h=8)), r=["psT"], w=[("kTn", t)])
                if is_ctx:
                    P.op("dve", lambda e, t=t: e.tensor_tensor(out=krb[:], in0=krf[:, t, :], in1=gk_bc[:, 128:192], op=ALU.mult), r=[("krf", t), "gk_bc"], w=["krb"])
                else:
                    P.op("dve", lambda e, t=t: e.tensor_tensor(out=krg[:], in0=krf[:, t, :], in1=gk_bc[:, 128:192], op=ALU.mult), r=[("krf", t), "gk_bc"], w=["krg"])
                    k4 = krg[:].rearrange("p (a s f) -> p a s f", a=2, s=2)
                    ko = krb[:].rearrange("p (a s f) -> p a s f", a=2, s=2)
                    c3_ = cosb[:].rearrange("p (a f) -> p a f", a=2)
                    s3_ = sinb[:].rearrange("p (a f) -> p a f", a=2)
                    kt = [ktmp[:, i, :].rearrange("p (a f) -> p a f", a=2) for i in range(4)]
                    P.op("dve", lambda e, k4=k4, c3_=c3_, kt=kt: e.tensor_tensor(out=kt[0], in0=k4[:, :, 0, :], in1=c3_, op=ALU.mult), r=["krg", ckk], w=[("kt", 0)])
                    P.op("dve", lambda e, k4=k4, s3_=s3_, kt=kt: e.tensor_tensor(out=kt[1], in0=k4[:, :, 1, :], in1=s3_, op=ALU.mult), r=["krg", skk], w=[("kt", 1)])
                    P.op("dve", lambda e, k4=k4, s3_=s3_, kt=kt: e.tensor_tensor(out=kt[2], in0=k4[:, :, 0, :], in1=s3_, op=ALU.mult), r=["krg", skk], w=[("kt", 2)])
                    P.op("dve", lambda e, k4=k4, c3_=c3_, kt=kt: e.tensor_tensor(out=kt[3], in0=k4[:, :, 1, :], in1=c3_, op=ALU.mult), r=["krg", ckk], w=[("kt", 3)])
                    P.op("dve", lambda e, ko=ko, kt=kt: e.tensor_tensor(out=ko[:, :, 0, :], in0=kt[0], in1=kt[1], op=ALU.subtract), r=[("kt", 0), ("kt", 1)], w=["krb"])
                    P.op("dve", lambda e, ko=ko, kt=kt: e.tensor_tensor(out=ko[:, :, 1, :], in0=kt[2], in1=kt[3], op=ALU.add), r=[("kt", 2), ("kt", 3), "krb"], w=["krb"])
                P.op("pe", lambda e: e.transpose(out=psT[0:64, 1024:1152], in_=krb[:], identity=ident[:]), r=["krb", "ident"], w=["psT"])
                P.op("act", lambda e, t=t: e.copy(out=krT_blk[:, t * 128:(t + 1) * 128], in_=psT[0:64, 1024:1152]), r=["psT"], w=[("krT", t)])
            tl = list(range(ntile))
            if need_q:
                P.dma("sp", qT_d[:, 0:128, tok0:tok0 + ntok].rearrange("h p n -> p h n"), qTn_blk[:, :, 0:ntok], r=[("qTn", t) for t in tl], key="st_qTn")
                P.dma("sp", qT_d[:, 128:192, tok0:tok0 + ntok].rearrange("h p n -> p h n"), qTr_blk[:, :, 0:ntok], r=[("qTr", t) for t in tl], key="st_qTr")
            P.dma("sp", knT_d[:, :, tok0:tok0 + ntok].rearrange("h p n -> p h n"), kTn_blk[:, :, 0:ntok], r=[("kTn", t) for t in tl], key="st_kTn")
            P.dma("sp", krT_d[:, tok0:tok0 + ntok], krT_blk[:, 0:ntok], r=[("krT", t) for t in tl], key="st_krT")
            P.dma("sp", r_d[tok0:tok0 + ntok, :].rearrange("(t p) e -> p t e", p=128), rk_blk[:, 0:ntile, :], r=[("rk", t) for t in tl], key="st_rk")
        P.flush()


def _phase_sgu(nc, P, sb, ps, lw, ctx_out, G):
    w_in = G["w_in"]; hT_d = G["hT_d"]; bT_d = G["bT_d"]
    with ExitStack() as es:
        win_b = sb(es, "win_b", [128, KC, 2048], BF16)
        wsT = sb(es, "wsT", [128, 8, 128], BF16)
        bs_bc = sb(es, "bs_bc", [128, 8, 128], F32)
        gs_bc = sb(es, "gs_bc", [128, 1024], F32)
        hT = [sb(es, "hT%d" % i, [128, KC, 512], BF16) for i in range(2)]
        ut = sb(es, "ut", [128, 8, 128], F32)
        gv = [sb(es, "gv%d" % i, [128, 1024], F32) for i in range(2)]
        jk = sb(es, "jk", [128, 1024], BF16)
        st = [sb(es, "st%d" % i, [128, 4], F32) for i in range(2)]
        vnb = [sb(es, "vnb%d" % i, [128, 8, 128], BF16) for i in range(2)]
        tmp = sb(es, "tmp", [128, 8, 128], F32)
        bT = [sb(es, "bT%d" % i, [128, 8, 512], BF16) for i in range(2)]
        psUt = ps(es, "psUt", [128, 8, 128])
        psV = [ps(es, "psV%d" % i, [128, 1024]) for i in range(2)]
        psS = ps(es, "psS", [128, 8, 128])

        for half in range(2):
            P.dma("pool", win_b[:, :, half * 1024:(half + 1) * 1024], w_in[lw, :, 832 + half * 1024:832 + (half + 1) * 1024].rearrange("(c p) n -> p c n", p=128), w=[("win_b", half)])
        P.dma("pool", wsT[:], G["w_sguT"][lw].rearrange("g p q -> p g q"), w=["wsT"])
        P.dma("sp", bs_bc[:].rearrange("p g q -> p (g q)"), _bcast_rows(G["b_sgu"][lw:lw + 1, :]), w=["bs_bc"])
        P.dma("sp", gs_bc[:], _bcast_rows(G["g_sgu"][lw:lw + 1, :]), w=["gs_bc"])

        gt = 0
        for bi, (tok0, ntok, row, is_ctx, b, pos0) in enumerate(_blocks(ctx_out)):
            ntile = ntok // 128
            hs = bi % 2
            hTb = hT[hs]; hk = ("hT", hs)
            bTb = bT[hs]
            P.dma("pool", hTb[:, :, 0:ntok], hT_d[:, tok0:tok0 + ntok].rearrange("(c p) n -> p c n", p=128), w=[hk])
            for t in range(ntile):
                sl = gt % 2; gt += 1
                pv = psV[sl]; pvk = ("psV", sl)
                for j in range(2):
                    for c in range(KC):
                        P.op("pe", lambda e, c=c, j=j, t=t, pv=pv, hTb=hTb: e.matmul(pv[:, j * 512:(j + 1) * 512], lhsT=hTb[:, c, t * 128:(t + 1) * 128], rhs=win_b[:, c, 1024 + j * 512:1024 + (j + 1) * 512], start=(c == 0), stop=(c == KC - 1)),
                             r=[hk, ("win_b", 1)], w=[pvk])
                for g in range(8):
                    for c in range(KC):
                        P.op("pe", lambda e, c=c, g=g, t=t, hTb=hTb: e.matmul(psUt[:, g, :], lhsT=win_b[:, c, g * 128:(g + 1) * 128], rhs=hTb[:, c, t * 128:(t + 1) * 128], start=(c == 0), stop=(c == KC - 1)),
                             r=[hk, ("win_b", 0)], w=["psUt"])
                P.op("act", lambda e: e.activation(out=ut[:], in_=psUt[:], func=AF.Gelu), r=["psUt"], w=["ut"])
                gvt = gv[sl]; gk = ("gv", sl); stt = st[sl]; sk = ("st", sl)
                P.op("act", lambda e, gvt=gvt, pv=pv: e.activation(out=gvt[:], in_=pv[:], func=AF.Gelu), r=[pvk], w=[gk])
                P.op("act", lambda e, gvt=gvt, stt=stt: e.activation(out=jk[:], in_=gvt[:], func=AF.Square, accum_out=stt[:, 0:1]), r=[gk], w=["jk", sk])
                P.op("act", lambda e, stt=stt: e.activation(out=stt[:, 1:2], in_=stt[:, 0:1], func=AF.Sqrt, bias=EPS, scale=1.0 / 1024), r=[sk], w=[sk])
                P.op("dve", lambda e, stt=stt: e.reciprocal(out=stt[:, 2:3], in_=stt[:, 1:2]), r=[sk], w=[sk])
                vt = vnb[sl]; vk = ("vnb", sl)
                P.op("dve", lambda e, gvt=gvt, stt=stt, vt=vt: e.scalar_tensor_tensor(out=vt[:].rearrange("p g c -> p (g c)"), in0=gvt[:], scalar=stt[:, 2:3], in1=gs_bc[:], op0=ALU.mult, op1=ALU.mult),
                     r=[gk, sk, "gs_bc"], w=[vk])
                for g in range(8):
                    P.op("pe", lambda e, g=g, vt=vt: e.matmul(psS[:, g, :], lhsT=vt[:, g, :], rhs=wsT[:, g, :], start=True, stop=True), r=[vk, "wsT"], w=["psS"])
                P.op("dve", lambda e: e.tensor_tensor(out=tmp[:], in0=psS[:], in1=bs_bc[:], op=ALU.add), r=["psS", "bs_bc"], w=["tmp"])
                P.op("dve", lambda e, t=t, bTb=bTb: e.tensor_tensor(out=bTb[:, :, t * 128:(t + 1) * 128], in0=tmp[:], in1=ut[:], op=ALU.mult),
                     r=["tmp", "ut"], w=[("bT", hs, t)])
            P.dma("sp", bT_d[:, tok0:tok0 + ntok].rearrange("(g p) n -> p g n", p=128), bTb[:, :, 0:ntok], r=[("bT", hs, t) for t in range(ntile)], key=("st_bT", hs))
        P.flush()


def _phase_pool(nc, P, sb, ps, lw, ctx_out, G):
    w_in = G["w_in"]; hT_d = G["hT_d"]; cpT_d = G["cpT_d"]
    with ExitStack() as es:
        win_c = sb(es, "win_c", [128, KC, 1024], BF16)
        wpool = sb(es, "wpool", [128, 4, 2, 256], BF16)
        spT = sb(es, "spT", [128, 8], F32)
        hT = [sb(es, "hT%d" % i, [128, KC, 512], BF16) for i in range(2)]
        pp = sb(es, "pp", [128, 18, 1024], BF16)
        band = [sb(es, "band%d" % i, [128, 4, 6, 512], BF16) for i in range(2)]
        mT = sb(es, "mT", [128, 8, 512], BF16)
        cpT = [sb(es, "cpT%d" % i, [128, 8, 512], BF16) for i in range(2)]
        psP = [ps(es, "psP%d" % i, [128, 1024]) for i in range(2)]
        psM = [ps(es, "psM%d" % i, [128, 512]) for i in range(2)]
        psY = [ps(es, "psY%d" % i, [128, 512]) for i in range(2)]

        P.dma("pool", win_c[:], w_in[lw, :, 2880:3904].rearrange("(c p) n -> p c n", p=128), w=["win_c"])
        for gi in range(4):
            P.dma("pool", wpool[:, gi], G["w_pool"][lw, gi].rearrange("(k p) d -> p k d", p=128), w=[("wpool", gi)], key="wpool")
        P.dma("sp", spT[:], bass.AP(G["s_pool"].tensor, G["s_pool"].offset, [[1, 128], [128, 8]]), w=["spT"], allow_slow_non_contiguous=True)

        gt = 0; gb = 0; gm = 0
        for b in range(NB):
            blks = [(b * NBT + j * 512, 512, False, j) for j in range(4)]
            if ctx_out:
                blks.append((b * NBT + NLAT, NCTX, True, 0))
            for (tok0, ntok, is_ctx, j) in blks:
                ntile = ntok // 128
                hs = gb % 2; gb += 1
                hTb = hT[hs]; hk = ("hT", hs)
                P.dma("pool", hTb[:, :, 0:ntok], hT_d[:, tok0:tok0 + ntok].rearrange("(c p) n -> p c n", p=128), w=[hk])
                for t in range(ntile):
                    ti = (16 + t) if is_ctx else (j * 4 + t)
                    sl = gt % 2; gt += 1
                    pv = psP[sl]; pvk = ("psP", sl)
                    for jj in range(2):
                        for c in range(KC):
                            P.op("pe", lambda e, c=c, jj=jj, t=t, pv=pv, hTb=hTb: e.matmul(pv[:, jj * 512:(jj + 1) * 512], lhsT=hTb[:, c, t * 128:(t + 1) * 128], rhs=win_c[:, c, jj * 512:(jj + 1) * 512], start=(c == 0), stop=(c == KC - 1)),
                                 r=[hk, "win_c"], w=[pvk])
                    P.op("act", lambda e, ti=ti, pv=pv: e.copy(out=pp[:, ti, :], in_=pv[:]), r=[pvk], w=[("pp", ti)])
            for bi, (tok0, ntok, is_ctx, j) in enumerate(blks):
                bs_ = gm % 2; gm += 1
                bd = band[bs_]; bk = ("band", bs_)
                cpb = cpT[bs_]
                if is_ctx:
                    srcs = [(16 + s, s) for s in range(2)]
                    for gi in range(4):
                        P.dma("pool", bd[:, gi, 0:2, 0:256], G["bandC"][gi].rearrange("s p n -> p s n"), w=[(bk, gi)], key=bk)
                else:
                    srcs = [(4 * j - 1 + si, si) for si in range(6) if 0 <= 4 * j - 1 + si < 16]
                    for gi in range(4):
                        P.dma("pool", bd[:, gi], G["bandL"][gi, j].rearrange("s p n -> p s n"), w=[(bk, gi)], key=bk)
                for gi in range(4):
                    for cc in range(2):
                        idx = gi * 2 + cc
                        pm = psM[idx % 2]; pmk = ("psM", idx % 2)
                        for n_, (ti, si) in enumerate(srcs):
                            P.op("pe", lambda e, gi=gi, cc=cc, ti=ti, si=si, n_=n_, pm=pm, bd=bd, ntok=ntok, ns=len(srcs): e.matmul(pm[:, 0:ntok], lhsT=pp[:, ti, gi * 256 + cc * 128:gi * 256 + (cc + 1) * 128], rhs=bd[:, gi, si, 0:ntok], start=(n_ == 0), stop=(n_ == ns - 1)),
                                 r=[("pp", ti), (bk, gi)], w=[pmk])
                        P.op("act", lambda e, idx=idx, pm=pm, ntok=ntok: e.copy(out=mT[:, idx, 0:ntok], in_=pm[:, 0:ntok]), r=[pmk], w=[("mT", idx)])
                for gi in range(4):
                    for dc in range(2):
                        idx = gi * 2 + dc
                        py = psY[idx % 2]; pyk = ("psY", idx % 2)
                        for kc in range(2):
                            P.op("pe", lambda e, gi=gi, dc=dc, kc=kc, py=py, ntok=ntok: e.matmul(py[:, 0:ntok], lhsT=wpool[:, gi, kc, dc * 128:(dc + 1) * 128], rhs=mT[:, gi * 2 + kc, 0:ntok], start=(kc == 0), stop=(kc == 1)),
                                 r=[("mT", gi * 2), ("mT", gi * 2 + 1), ("wpool", gi)], w=[pyk])
                        P.op("dve", lambda e, idx=idx, py=py, ntok=ntok, cpb=cpb: e.tensor_scalar(out=cpb[:, idx, 0:ntok], in0=py[:, 0:ntok], scalar1=spT[:, idx:idx + 1], scalar2=None, op0=ALU.mult),
                             r=[pyk, "spT"], w=[("cpT", bs_, idx)])
                P.dma("sp", cpT_d[:, tok0:tok0 + ntok].rearrange("(g p) n -> p g n", p=128), cpb[:, :, 0:ntok], r=[("cpT", bs_, i) for i in range(8)], key=("st_cpT", bs_))
        P.flush()


def _phase_attn(nc, P, sb, ps, lw, ctx_out, G):
    qT_d = G["qT_d"]; knT_d = G["knT_d"]; krT_d = G["krT_d"]; v_d = G["v_d"]; r_d = G["r_d"]; aT_d = G["aT_d"]
    NKC = NBT // 128
    with ExitStack() as es:
        ones = sb(es, "ones", [128, 128], BF16)
        krT = sb(es, "krT", [64, NBT], BF16)
        rk = sb(es, "rk", [128, NKC, 8], F32)
        kTn = [sb(es, "kTn%d" % i, [128, NBT], BF16) for i in range(2)]
        vh = [sb(es, "vh%d" % i, [128, NKC, 128], BF16) for i in range(2)]
        qn = [sb(es, "qn%d" % i, [128, 512], BF16) for i in range(2)]
        qr = [sb(es, "qr%d" % i, [64, 512], BF16) for i in range(2)]
        pT = [sb(es, "pT%d" % i, [128, 512], BF16) for i in range(3)]
        rden = sb(es, "rden", [128, 512], F32)
        aT = [sb(es, "aT%d" % i, [128, 512], BF16) for i in range(2)]
        psS = [ps(es, "psS%d" % i, [128, 512]) for i in range(3)]
        psO = [ps(es, "psO%d" % i, [128, 512]) for i in range(2)]
        psD = [ps(es, "psD%d" % i, [128, 512]) for i in range(2)]
        P.op("dve", lambda e: e.memset(ones[:], 1.0), w=["ones"])
        gq = 0; gp = 0; gh = 0
        for b in range(NB):
            k0 = b * NBT
            P.dma("pool", krT[:], krT_d[:, k0:k0 + NBT], w=["krT"])
            P.dma("pool", rk[:], r_d[k0:k0 + NBT, :].rearrange("(t p) e -> p t e", p=128), w=["rk"])
            for h in range(8):
                hs = gh % 2; gh += 1
                kt = kTn[hs]; kk = ("kTn", hs); vt = vh[hs]; vk = ("vh", hs)
                P.dma("pool", kt[:], knT_d[h, :, k0:k0 + NBT], w=[kk])
                P.dma("pool", vt[:], v_d[k0:k0 + NBT, h, :].rearrange("(t p) d -> p t d", p=128), w=[vk])
                qblocks = [(k0 + j * 512, 512, list(range(NKC))) for j in range(4)]
                if ctx_out:
                    qblocks.append((k0 + NLAT, NCTX, [16, 17]))
                for (q0, nq, kcs) in qblocks:
                    qs = gq % 2; gq += 1
                    qnt = qn[qs]; qrt = qr[qs]; qk = ("q", qs)
                    P.dma("pool", qnt[:, 0:nq], qT_d[h, 0:128, q0:q0 + nq], w=[qk], key=("ldqn", qs))
                    P.dma("pool", qrt[:, 0:nq], qT_d[h, 128:192, q0:q0 + nq], w=[("qr", qs)], key=("ldqr", qs))
                    po = psO[qs]; pd = psD[qs]; pok = ("psO", qs)
                    for n_, kc in enumerate(kcs):
                        ss = gp % 3; gp += 1
                        pss = psS[ss]; psk = ("psS", ss); ptt = pT[ss]; ptk = ("pT", ss)
                        P.op("pe", lambda e, kc=kc, pss=pss, kt=kt, qnt=qnt, nq=nq: e.matmul(pss[:, 0:nq], lhsT=kt[:, kc * 128:(kc + 1) * 128], rhs=qnt[:, 0:nq], start=True, stop=False),
                             r=[kk, qk], w=[psk])
                        P.op("pe", lambda e, kc=kc, pss=pss, qrt=qrt, nq=nq: e.matmul(pss[:, 0:nq], lhsT=krT[:, kc * 128:(kc + 1) * 128], rhs=qrt[:, 0:nq], start=False, stop=True),
                             r=["krT", ("qr", qs)], w=[psk])
                        P.op("act", lambda e, kc=kc, h=h, pss=pss, ptt=ptt, nq=nq: e.activation(out=ptt[:, 0:nq], in_=pss[:, 0:nq], func=AF.Exp, scale=rk[:, kc, h:h + 1]),
                             r=[psk, "rk"], w=[ptk])
                        first = (n_ == 0); lastk = (n_ == len(kcs) - 1)
                        P.op("pe", lambda e, kc=kc, vt=vt, ptt=ptt, po=po, nq=nq, first=first, lastk=lastk: e.matmul(po[:, 0:nq], lhsT=vt[:, kc, :], rhs=ptt[:, 0:nq], start=first, stop=lastk),
                             r=[vk, ptk], w=[pok])
                        P.op("pe", lambda e, ptt=ptt, pd=pd, nq=nq, first=first, lastk=lastk: e.matmul(pd[:, 0:nq], lhsT=ones[:], rhs=ptt[:, 0:nq], start=first, stop=lastk),
                             r=["ones", ptk], w=[pok])
                    at = aT[qs]; ak = ("aT", qs)
                    P.op("dve", lambda e, pd=pd, nq=nq: e.reciprocal(out=rden[:, 0:nq], in_=pd[:, 0:nq]), r=[pok], w=["rden"])
                    P.op("dve", lambda e, po=po, at=at, nq=nq: e.tensor_tensor(out=at[:, 0:nq], in0=po[:, 0:nq], in1=rden[:, 0:nq], op=ALU.mult), r=[pok, "rden"], w=[ak])
                    P.dma("sp", aT_d[h * 128:(h + 1) * 128, q0:q0 + nq], at[:, 0:nq], r=[ak], key=("st_aT", qs))
        P.flush()


def _phase_merge(nc, P, sb, ps, lw, ctx_out, G):
    w_in = G["w_in"]; hT_d = G["hT_d"]; yT_d = G["yT_d"]
    srcs_d = [G["aT_d"], G["bT_d"], G["cpT_d"]]
    wbr_d = [G["w_ao"], G["w_bo"], G["w_co"]]
    GATE0 = 3904
    with ExitStack() as es:
        hT = sb(es, "hT", [128, KC, 512], BF16)
        br = [sb(es, "br%d" % i, [128, 8, 512], BF16) for i in range(3)]
        wg = [sb(es, "wg%d" % i, [128, KC, 3, 256], BF16) for i in range(2)]
        wb = [sb(es, "wb%d" % i, [128, 3, 8, 256], BF16) for i in range(2)]
        sg = [sb(es, "sg%d" % i, [128, 512], F32) for i in range(2)]
        acc = sb(es, "acc", [128, 512], F32)
        t2 = sb(es, "t2", [128, 512], F32)
        yT = [sb(es, "yT%d" % i, [128, 2, 512], BF16) for i in range(2)]
        psG = [ps(es, "psG%d" % i, [128, 512]) for i in range(2)]
        psB = [ps(es, "psB%d" % i, [128, 512]) for i in range(2)]
        gw = 0; gpp = 0
        for (tok0, ntok, row, is_ctx, b, pos0) in _blocks(ctx_out):
            P.dma("sp", hT[:, :, 0:ntok], hT_d[:, tok0:tok0 + ntok].rearrange("(c p) n -> p c n", p=128), w=["hT"])
            for i in range(3):
                P.dma("sp", br[i][:, :, 0:ntok], srcs_d[i][:, tok0:tok0 + ntok].rearrange("(c p) n -> p c n", p=128), w=[("br", i)])
            for dg in range(8):
                ws = gw % 2; gw += 1
                wgt = wg[ws]; wbt = wb[ws]; wgk = ("wg", ws); wbk = ("wb", ws)
                for i in range(3):
                    P.dma("pool", wgt[:, :, i, :], w_in[lw, :, GATE0 + i * D + dg * 256:GATE0 + i * D + (dg + 1) * 256].rearrange("(c p) n -> p c n", p=128), w=[(wgk, i)], key=wgk)
                    P.dma("pool", wbt[:, i, :, :], wbr_d[i][lw, :, dg * 256:(dg + 1) * 256].rearrange("(c p) n -> p c n", p=128), w=[(wbk, i)], key=wbk)
                ytt = yT[ws]; yk = ("yT", ws)
                for dl in range(2):
                    for i in range(3):
                        s_ = gpp % 2; gpp += 1
                        pg = psG[s_]; pb = psB[s_]; pgk = ("psG", s_); pbk = ("psB", s_)
                        sgt = sg[s_]; sgk = ("sg", s_)
                        for c in range(KC):
                            P.op("pe", lambda e, c=c, i=i, dl=dl, pg=pg, wgt=wgt, ntok=ntok: e.matmul(pg[:, 0:ntok], lhsT=wgt[:, c, i, dl * 128:(dl + 1) * 128], rhs=hT[:, c, 0:ntok], start=(c == 0), stop=(c == KC - 1)),
                                 r=["hT", (wgk, i)], w=[pgk])
                        for c in range(8):
                            P.op("pe", lambda e, c=c, i=i, dl=dl, pb=pb, wbt=wbt, ntok=ntok: e.matmul(pb[:, 0:ntok], lhsT=wbt[:, i, c, dl * 128:(dl + 1) * 128], rhs=br[i][:, c, 0:ntok], start=(c == 0), stop=(c == 7)),
                                 r=[("br", i), (wbk, i)], w=[pbk])
                        P.op("act", lambda e, pg=pg, sgt=sgt, ntok=ntok: e.activation(out=sgt[:, 0:ntok], in_=pg[:, 0:ntok], func=AF.Sigmoid), r=[pgk], w=[sgk])
                        if i == 0:
                            P.op("dve", lambda e, pb=pb, sgt=sgt, ntok=ntok: e.tensor_tensor(out=acc[:, 0:ntok], in0=pb[:, 0:ntok], in1=sgt[:, 0:ntok], op=ALU.mult), r=[pbk, sgk], w=["acc"])
                        elif i == 1:
                            P.op("dve", lambda e, pb=pb, sgt=sgt, ntok=ntok: e.tensor_tensor(out=t2[:, 0:ntok], in0=pb[:, 0:ntok], in1=sgt[:, 0:ntok], op=ALU.mult), r=[pbk, sgk], w=["t2"])
                            P.op("dve", lambda e, ntok=ntok: e.tensor_tensor(out=acc[:, 0:ntok], in0=acc[:, 0:ntok], in1=t2[:, 0:ntok], op=ALU.add), r=["acc", "t2"], w=["acc"])
                        else:
                            P.op("dve", lambda e, pb=pb, sgt=sgt, ntok=ntok: e.tensor_tensor(out=t2[:, 0:ntok], in0=pb[:, 0:ntok], in1=sgt[:, 0:ntok], op=ALU.mult), r=[pbk, sgk], w=["t2"])
                            P.op("dve", lambda e, ntok=ntok, ytt=ytt, dl=dl: e.tensor_tensor(out=ytt[:, dl, 0:ntok], in0=acc[:, 0:ntok], in1=t2[:, 0:ntok], op=ALU.add), r=["acc", "t2"], w=[(yk, dl)])
                P.dma("sp", yT_d[dg * 256:(dg + 1) * 256, tok0:tok0 + ntok].rearrange("(c p) n -> p c n", p=128), ytt[:, :, 0:ntok], r=[(yk, 0), (yk, 1)], key=("st_yT", ws))
        P.flush()


def _phase_outproj(nc, P, sb, ps, lw, ctx_out, G):
    xs = G["xs"]; mod_d = G["mod_d"]; yT_d = G["yT_d"]
    with ExitStack() as es:
        wout = sb(es, "wout", [128, KC, D], BF16)
        g1 = sb(es, "g1", [128, 3, D], F32)
        yT = [sb(es, "yT%d" % i, [128, KC, 512], BF16) for i in range(2)]
        xt = [sb(es, "xt%d" % i, [128, D], F32) for i in range(2)]
        tm = [sb(es, "tm%d" % i, [128, D], F32) for i in range(2)]
        psO = [ps(es, "psO%d" % i, [128, 512]) for i in range(8)]
        for hf in range(2):
            P.dma("pool", wout[:, :, hf * 1024:(hf + 1) * 1024], G["w_out"][lw, :, hf * 1024:(hf + 1) * 1024].rearrange("(c p) n -> p c n", p=128), w=[("wout", hf)])
        for r_ in range(3):
            P.dma("sp", g1[:, r_, :], _bcast_rows(mod_d[r_:r_ + 1, 2 * D:3 * D]), w=[("g1", r_)])
        gt = 0
        for bi, (tok0, ntok, row, is_ctx, b, pos0) in enumerate(_blocks(ctx_out)):
            ys = bi % 2
            ytt = yT[ys]; yk = ("yT", ys)
            P.dma("pool", ytt[:, :, 0:ntok], yT_d[:, tok0:tok0 + ntok].rearrange("(c p) n -> p c n", p=128), w=[yk])
            for t in range(ntok // 128):
                sl = gt % 2; gt += 1
                r0 = tok0 + t * 128
                xtt = xt[sl]; xk = ("xt", sl); tmt = tm[sl]; tk = ("tm", sl)
                P.dma("pool", xtt[:], xs[r0:r0 + 128, :], w=[xk])
                for j in range(4):
                    po = psO[sl * 4 + j]; pk = ("psO", sl * 4 + j)
                    for c in range(KC):
                        P.op("pe", lambda e, c=c, j=j, t=t, po=po, ytt=ytt: e.matmul(po[:], lhsT=ytt[:, c, t * 128:(t + 1) * 128], rhs=wout[:, c, j * 512:(j + 1) * 512], start=(c == 0), stop=(c == KC - 1)),
                             r=[yk, ("wout", j // 2)], w=[pk])
                    P.op("dve", lambda e, j=j, po=po, tmt=tmt, row=row: e.tensor_tensor(out=tmt[:, j * 512:(j + 1) * 512], in0=po[:], in1=g1[:, row, j * 512:(j + 1) * 512], op=ALU.mult),
                         r=[pk, ("g1", row)], w=[(tk, j)])
                P.op("pool", lambda e, xtt=xtt, tmt=tmt: e.tensor_tensor(out=xtt[:], in0=xtt[:], in1=tmt[:], op=ALU.add), r=[xk] + [(tk, j) for j in range(4)], w=[xk])
                P.dma("sp", xs[r0:r0 + 128, :], xtt[:], r=[xk], key=("st_x", sl))
        P.flush()


def _tile_row(gti):
    b, lt = divmod(gti, NBT // 128)
    return b if lt < NLAT // 128 else 2


def _phase_moe(nc, P, sb, ps, lw, ctx_out, G):
    xs = G["xs"]; mod_d = G["mod_d"]; hT_d = G["hT_d"]
    g_d = G["g_d"]; ymoe_d = G["ymoe_d"]
    NTILE = NT // 128
    with ExitStack() as es:
        ident = sb(es, "ident", [128, 128], BF16)
        wr = sb(es, "wr", [128, KC, 36], BF16)
        br_bc = sb(es, "br_bc", [128, 36], F32)
        xt = [sb(es, "xt%d" % i, [128, D], F32) for i in range(2)]
        jk = sb(es, "jk", [128, D], BF16)
        st = [sb(es, "st%d" % i, [128, 16], F32) for i in range(2)]
        xnb = [sb(es, "xnb%d" % i, [128, D], BF16) for i in range(2)]
        tmpf = sb(es, "tmpf", [128, KC, 128], F32)
        hT = [sb(es, "hT%d" % i, [128, KC, 128], BF16) for i in range(2)]
        Lg = [sb(es, "Lg%d" % i, [128, 36], F32) for i in range(2)]
        rt = [sb(es, "rt%d" % i, [128, 96], F32) for i in range(2)]
        gat = [sb(es, "gat%d" % i, [128, 4, 8], F32) for i in range(2)]
        psT = ps(es, "psT", [128, 2048], BF16)
        psR = [ps(es, "psR%d" % i, [128, 64]) for i in range(2)]
        psT3 = psT[:].rearrange("p (c n) -> p c n", c=KC)
        P.dma("sp", ident[:], G["ident_in"][:, :], w=["ident"])
        P.dma("pool", wr[:], G["w_r"][lw].rearrange("(c p) n -> p c n", p=128), w=["wr"])
        P.dma("sp", br_bc[:], _bcast_rows(G["b_r"][lw:lw + 1, :]), w=["br_bc"])
        sc2 = _load_modT(nc, P, es, sb, mod_d, 4, "modsc")
        sh2 = _load_modT(nc, P, es, sb, mod_d, 3, "modsh")
        P.op("dve", lambda e: e.tensor_scalar(out=sc2[:], in0=sc2[:], scalar1=1.0, scalar2=None, op0=ALU.add), r=["modsc"], w=["modsc"])
        for gti in range(NTILE):
            row = _tile_row(gti)
            if row == 2 and not ctx_out:
                continue
            sl = gti % 2
            r0 = gti * 128
            P.dma("pool", xt[sl][:], xs[r0:r0 + 128, :], w=[("xt", sl)])
            hTt = hT[sl]; hk = ("hT", sl)
            _norm_mod_tile(P, xt[sl], ("xt", sl), jk, "jk", st[sl], ("st", sl), xnb[sl], ("xnb", sl), psT3, "psT", ident,
                           tmpf, "tmpf", sc2, sh2, row, hTt[:], hk)
            P.dma("sp", hT_d[:, r0:r0 + 128].rearrange("(c p) n -> p c n", p=128), hTt[:], r=[hk], key=("st_hT", sl))
            pr = psR[sl]; prk = ("psR", sl)
            for c in range(KC):
                P.op("pe", lambda e, c=c, pr=pr, hTt=hTt: e.matmul(pr[:, 0:36], lhsT=hTt[:, c, :], rhs=wr[:, c, :], start=(c == 0), stop=(c == KC - 1)), r=[hk, "wr"], w=[prk])
            L = Lg[sl]; lk = ("Lg", sl); R_ = rt[sl]; rk = ("rt", sl); gt_ = gat[sl]; gk = ("gat", sl)
            P.op("dve", lambda e, L=L, pr=pr: e.tensor_tensor(out=L[:], in0=pr[:, 0:36], in1=br_bc[:], op=ALU.add), r=[prk, "br_bc"], w=[lk])
            P.op("dve", lambda e, L=L, R_=R_: e.tensor_reduce(out=R_[:, 0:1], in_=L[:, 0:4], axis=AX.X, op=ALU.max), r=[lk], w=[rk])
            P.op("dve", lambda e, R_=R_: e.tensor_scalar(out=R_[:, 1:2], in0=R_[:, 0:1], scalar1=-1.0, scalar2=None, op0=ALU.mult), r=[rk], w=[rk])
            P.op("dve", lambda e, L=L, R_=R_: e.tensor_scalar(out=R_[:, 4:8], in0=L[:, 0:4], scalar1=R_[:, 0:1], scalar2=None, op0=ALU.is_ge), r=[lk, rk], w=[rk])
            P.op("act", lambda e, L=L, R_=R_: e.activation(out=R_[:, 8:12], in_=L[:, 0:4], func=AF.Exp, bias=R_[:, 1:2], scale=1.0, accum_out=R_[:, 2:3]), r=[lk, rk], w=[rk])
            P.op("dve", lambda e, R_=R_: e.reciprocal(out=R_[:, 3:4], in_=R_[:, 2:3]), r=[rk], w=[rk])
            P.op("dve", lambda e, L=L, R_=R_: e.tensor_tensor(out=R_[:, 12:44].rearrange("p (g x) -> p g x", g=4), in0=L[:, 4:36].rearrange("p (g x) -> p g x", g=4),
                                                             in1=R_[:, 4:8].unsqueeze(2).to_broadcast([128, 4, 8]), op=ALU.mult), r=[lk, rk], w=[rk])
            P.op("dve", lambda e, R_=R_: e.tensor_reduce(out=R_[:, 44:52], in_=R_[:, 12:44].rearrange("p (g x) -> p x g", g=4), axis=AX.X, op=ALU.add), r=[rk], w=[rk])
            P.op("dve", lambda e, R_=R_: e.tensor_reduce(out=R_[:, 52:53], in_=R_[:, 44:52], axis=AX.X, op=ALU.max), r=[rk], w=[rk])
            P.op("dve", lambda e, R_=R_: e.tensor_scalar(out=R_[:, 54:62], in0=R_[:, 44:52], scalar1=R_[:, 52:53], scalar2=None, op0=ALU.is_ge), r=[rk], w=[rk])
            P.op("dve", lambda e, R_=R_: e.scalar_tensor_tensor(out=R_[:, 62:70], in0=R_[:, 54:62], scalar=-1e30, in1=R_[:, 44:52], op0=ALU.mult, op1=ALU.add), r=[rk], w=[rk])
            P.op("dve", lambda e, R_=R_: e.tensor_reduce(out=R_[:, 53:54], in_=R_[:, 62:70], axis=AX.X, op=ALU.max), r=[rk], w=[rk])
            P.op("dve", lambda e, R_=R_: e.tensor_scalar(out=R_[:, 70:78], in0=R_[:, 62:70], scalar1=R_[:, 53:54], scalar2=None, op0=ALU.is_ge), r=[rk], w=[rk])
            P.op("dve", lambda e, R_=R_: e.tensor_tensor(out=R_[:, 78:79], in0=R_[:, 52:53], in1=R_[:, 53:54], op=ALU.subtract), r=[rk], w=[rk])
            P.op("act", lambda e, R_=R_: e.activation(out=R_[:, 79:80], in_=R_[:, 78:79], func=AF.Sigmoid), r=[rk], w=[rk])
            P.op("dve", lambda e, R_=R_: e.tensor_scalar(out=R_[:, 80:81], in0=R_[:, 79:80], scalar1=-1.0, scalar2=1.0, op0=ALU.mult, op1=ALU.add), r=[rk], w=[rk])
            P.op("dve", lambda e, R_=R_: e.tensor_scalar(out=R_[:, 81:83], in0=R_[:, 79:81], scalar1=R_[:, 3:4], scalar2=None, op0=ALU.mult), r=[rk], w=[rk])
            P.op("dve", lambda e, R_=R_: e.tensor_scalar(out=R_[:, 83:91], in0=R_[:, 54:62], scalar1=R_[:, 81:82], scalar2=None, op0=ALU.mult), r=[rk], w=[rk])
            P.op("dve", lambda e, R_=R_: e.scalar_tensor_tensor(out=R_[:, 44:52], in0=R_[:, 70:78], scalar=R_[:, 82:83], in1=R_[:, 83:91], op0=ALU.mult, op1=ALU.add), r=[rk], w=[rk])
            P.op("dve", lambda e, R_=R_, gt_=gt_: e.tensor_tensor(out=gt_[:], in0=R_[:, 4:8].unsqueeze(2).to_broadcast([128, 4, 8]), in1=R_[:, 44:52].unsqueeze(1).to_broadcast([128, 4, 8]), op=ALU.mult),
                 r=[rk], w=[gk])
            P.dma("sp", g_d[r0:r0 + 128, :], gt_[:].rearrange("p g x -> p (g x)"), r=[gk], key=("st_g", sl))
        P.flush()

    TB = 768
    with ExitStack() as es:
        hT = sb(es, "hT", [128, KC, TB], BF16)
        gates = sb(es, "gates", [128, TB // 128, 32], F32)
        yacc = sb(es, "yacc", [128, TB // 128, D], F32)
        wg = [sb(es, "wg%d" % i, [128, KC, HID], BF16) for i in range(2)]
        wu = [sb(es, "wu%d" % i, [128, KC, HID], BF16) for i in range(2)]
        wd = [sb(es, "wd%d" % i, [128, 4, D], BF16) for i in range(2)]
        hid = [sb(es, "hid%d" % i, [128, 4, TB], BF16) for i in range(2)]
        sg = [sb(es, "sg%d" % i, [128, 384], F32) for i in range(2)]
        psG = [ps(es, "psG%d" % i, [128, 512]) for i in range(2)]
        psU = [ps(es, "psU%d" % i, [128, 512]) for i in range(2)]
        psD = [ps(es, "psD%d" % i, [128, 512]) for i in range(4)]
        ge = 0; gs_ = 0
        for blk in range(NT // TB):
            tok0 = blk * TB
            tiles = [t for t in range(TB // 128) if ctx_out or _tile_row(tok0 // 128 + t) != 2]
            P.dma("sp", hT[:], hT_d[:, tok0:tok0 + TB].rearrange("(c p) n -> p c n", p=128), w=["hT"])
            P.dma("sp", gates[:], g_d[tok0:tok0 + TB, :].rearrange("(t p) e -> p t e", p=128), w=["gates"])
            P.op("pool", lambda e: e.memset(yacc[:], 0.0), w=[("yacc", t) for t in range(TB // 128)])
            for ex in range(NEXP):
                ws = ge % 2; ge += 1
                wgt = wg[ws]; wut = wu[ws]; wdt = wd[ws]; hidt = hid[ws]
                P.dma("pool", wgt[:], G["w_eg"][lw, ex].rearrange("(c p) n -> p c n", p=128), w=[("wg", ws)])
                P.dma("pool", wut[:], G["w_eu"][lw, ex].rearrange("(c p) n -> p c n", p=128), w=[("wu", ws)])
                P.dma("pool", wdt[:], G["w_ed"][lw, ex].rearrange("(c p) n -> p c n", p=128), w=[("wd", ws)])
                for hc in range(4):
                    for hf in range(2):
                        s_ = gs_ % 2; gs_ += 1
                        pg = psG[s_]; pu = psU[s_]; pgk = ("psG", s_); puk = ("psU", s_); sgt = sg[s_]; sgk = ("sg", s_)
                        for c in range(KC):
                            P.op("pe", lambda e, c=c, hc=hc, hf=hf, pg=pg, wgt=wgt: e.matmul(pg[:, 0:384], lhsT=wgt[:, c, hc * 128:(hc + 1) * 128], rhs=hT[:, c, hf * 384:(hf + 1) * 384], start=(c == 0), stop=(c == KC - 1)),
                                 r=["hT", ("wg", ws)], w=[pgk])
                        for c in range(KC):
                            P.op("pe", lambda e, c=c, hc=hc, hf=hf, pu=pu, wut=wut: e.matmul(pu[:, 0:384], lhsT=wut[:, c, hc * 128:(hc + 1) * 128], rhs=hT[:, c, hf * 384:(hf + 1) * 384], start=(c == 0), stop=(c == KC - 1)),
                                 r=["hT", ("wu", ws)], w=[puk])
                        P.op("act", lambda e, pg=pg, sgt=sgt: e.activation(out=sgt[:], in_=pg[:, 0:384], func=AF.Silu), r=[pgk], w=[sgk])
                        P.op("dve", lambda e, pu=pu, sgt=sgt, hidt=hidt, hc=hc, hf=hf: e.tensor_tensor(out=hidt[:, hc, hf * 384:(hf + 1) * 384], in0=pu[:, 0:384], in1=sgt[:], op=ALU.mult),
                             r=[puk, sgk], w=[("hid", ws, hc, hf)])
                for t in tiles:
                    hf = (t * 128) // 384
                    hf2 = (t * 128 + 127) // 384
                    for j in range(4):
                        pd = psD[j]; pdk = ("psD", j)
                        for hc in range(4):
                            P.op("pe", lambda e, hc=hc, j=j, t=t, pd=pd, hidt=hidt, wdt=wdt: e.matmul(pd[:], lhsT=hidt[:, hc, t * 128:(t + 1) * 128], rhs=wdt[:, hc, j * 512:(j + 1) * 512], start=(hc == 0), stop=(hc == 3)),
                                 r=[("hid", ws, hc, hf), ("hid", ws, hc, hf2), ("wd", ws)], w=[pdk])
                        P.op("dve", lambda e, j=j, t=t, pd=pd, ex=ex: e.scalar_tensor_tensor(out=yacc[:, t, j * 512:(j + 1) * 512], in0=pd[:], scalar=gates[:, t, ex:ex + 1], in1=yacc[:, t, j * 512:(j + 1) * 512], op0=ALU.mult, op1=ALU.add),
                             r=[pdk, "gates", ("yacc", t)], w=[("yacc", t)])
            for t in tiles:
                r0 = tok0 + t * 128
                P.dma("sp", ymoe_d[r0:r0 + 128, :], yacc[:, t, :], r=[("yacc", t)], key=("st_y", t))
        P.flush()

    with ExitStack() as es:
        g2 = sb(es, "g2", [128, 3, D], F32)
        xt = [sb(es, "xt%d" % i, [128, D], F32) for i in range(3)]
        yt = [sb(es, "yt%d" % i, [128, D], F32) for i in range(3)]
        for r_ in range(3):
            P.dma("sp", g2[:, r_, :], _bcast_rows(mod_d[r_:r_ + 1, 5 * D:6 * D]), w=[("g2", r_)])
        n = 0
        for gti in range(NTILE):
            row = _tile_row(gti)
            if row == 2 and not ctx_out:
                continue
            sl = n % 3; n += 1
            r0 = gti * 128
            xtt = xt[sl]; ytt = yt[sl]; xk = ("xt", sl); yk = ("yt", sl)
            P.dma("pool", xtt[:], xs[r0:r0 + 128, :], w=[xk])
            P.dma("pool", ytt[:], ymoe_d[r0:r0 + 128, :], w=[yk])
            P.op("dve", lambda e, ytt=ytt, row=row: e.tensor_tensor(out=ytt[:], in0=ytt[:], in1=g2[:, row, :], op=ALU.mult), r=[yk, ("g2", row)], w=[yk])
            P.op("dve", lambda e, xtt=xtt, ytt=ytt: e.tensor_tensor(out=xtt[:], in0=xtt[:], in1=ytt[:], op=ALU.add), r=[xk, yk], w=[xk])
            P.dma("sp", xs[r0:r0 + 128, :], xtt[:], r=[xk], key=("st_x", sl))
        P.flush()


_CONSTS = None


def _consts():
    global _CONSTS
    if _CONSTS is None:
        cosv, sinv = _rope_tables()
        bl, bc = _band_consts()
        _CONSTS = {"rope_cos": cosv, "rope_sin": sinv, "bandL": bl, "bandC": bc,
                   "ident": np.eye(128, dtype=np.float32).astype(ml_dtypes.bfloat16)}
    return _CONSTS


def _pack_layer(inp, li):
    w_re = inp["w_re"][li]
    src = {
        "w_sguT": np.transpose(inp["w_sgu"][li], (0, 2, 1)),
        "w_r": np.concatenate([inp["w_rg"][li], np.transpose(w_re, (1, 0, 2)).reshape(D, 32)], axis=-1),
        "b_r": np.concatenate([inp["b_rg"][li], inp["b_re"][li].reshape(32)], axis=-1),
    }
    bufs = [np.zeros((CHUNK_ROWS[k], 2048), dtype=np.float32) for k in range(NCHUNK)]
    for name, shape, ck, row0 in PACK_SPEC:
        a = src[name] if name in src else inp[name][li]
        a = np.asarray(a, dtype=np.float32).reshape(-1)
        assert a.size == int(np.prod(shape)), (name, a.size, shape)
        bufs[ck].reshape(-1)[row0 * 2048:row0 * 2048 + a.size] = a
    return bufs


_PROGS = {}


def _get_prog(layers, single):
    key = (tuple(layers), single)
    if key not in _PROGS:
        _PROGS[key] = build_program(list(layers), single)
    return _PROGS[key]


def _core_tokens(x, ctx, core):
    parts = []
    for b in range(NB):
        parts.append(x[core * NB + b])
        parts.append(ctx[core * NB + b])
    return np.ascontiguousarray(np.concatenate(parts, axis=0))


FUSED = True


def kernel(**inp):
    inp = {k: np.asarray(v) for k, v in inp.items()}
    x = inp["x"].astype(np.float32, copy=False)
    ctx = inp["ctx"].astype(np.float32, copy=False)
    c = inp["c"]; c_ctx = inp["c_ctx"]
    consts = _consts()
    xin = [_core_tokens(x, ctx, core) for core in range(NCORES)]
    c3 = [np.ascontiguousarray(np.stack([c[core * NB], c[core * NB + 1], c_ctx], axis=0)) for core in range(NCORES)]
    groups = [list(range(DEPTH))] if FUSED else [[li] for li in range(DEPTH)]
    for layers in groups:
        packed = [_pack_layer(inp, li) for li in layers]
        nc = _get_prog(layers, True)
        in_maps = []
        for core in range(NCORES):
            m = dict(consts, xin=xin[core], c3=c3[core])
            for pos in range(len(layers)):
                for k in range(NCHUNK):
                    m["wpk_%d_%d" % (pos, k)] = packed[pos][k]
            in_maps.append(m)
        del packed
        res = run_bass_kernel_spmd(nc, in_maps, core_ids=list(range(NCORES)))
        xin = [np.asarray(r["xout"]) for r in res.results]
        del res, in_maps
    out = np.empty((NCORES * NB, NLAT, D), dtype=np.float32)
    for core in range(NCORES):
        for b in range(NB):
            out[core * NB + b] = xin[core][b * NBT:b * NBT + NLAT]
    return out
```

```python
import math
from contextlib import ExitStack
import numpy as np
import ml_dtypes
import concourse.bass as bass
import concourse.mybir as mybir
from concourse.bass_utils import run_bass_kernel_spmd

F32 = mybir.dt.float32
BF16 = mybir.dt.bfloat16
AF = mybir.ActivationFunctionType
ALU = mybir.AluOpType
AX = mybir.AxisListType

D = 2048
KC = 16
DEPTH = 4
NLAT = 2048
NCTX = 256
NB = 2
NBT = NLAT + NCTX
NT = NB * NBT
IN_COLS = 10048
EPS = 1e-6
ATTN_SCALE = 1.0 / math.sqrt(192.0)
NEXP = 32
HID = 512
POOL_WINDOWS = (2, 4, 8, 16)
NCORES = 8

ENGS = ("pe", "act", "dve", "pool", "sp")

_PACK_SHAPES = [
    ("w_ada", (2048, 12288)), ("b_ada", (12288,)), ("w_in", (2048, 10048)),
    ("g_cq", (512,)), ("g_ckv", (256,)), ("w_uq", (512, 1536)), ("w_ukv", (256, 2048)),
    ("g_q", (192,)), ("g_k", (192,)), ("w_sguT", (8, 128, 128)), ("b_sgu", (1024,)), ("g_sgu", (1024,)),
    ("w_pool", (4, 256, 256)), ("s_pool", (1024,)),
    ("w_ao", (1024, 2048)), ("w_bo", (1024, 2048)), ("w_co", (1024, 2048)), ("w_out", (2048, 2048)),
    ("w_r", (2048, 36)), ("b_r", (36,)),
    ("w_e_gate", (32, 2048, 512)), ("w_e_up", (32, 2048, 512)), ("w_e_down", (32, 512, 2048)),
]
_CHUNK_OF = {"w_e_gate": 1, "w_e_up": 1, "w_e_down": 2}
CHUNK_ROWS = [32768, 32768, 16384]
NCHUNK = 3
PACK_SPEC = []
_rows = [0, 0, 0]
for _n, _s in _PACK_SHAPES:
    _c = _CHUNK_OF.get(_n, 0)
    PACK_SPEC.append((_n, _s, _c, _rows[_c]))
    _rows[_c] += -(-int(np.prod(_s)) // 2048)
assert all(_rows[i] <= CHUNK_ROWS[i] for i in range(3)), _rows
STOP_AFTER = None


class _Op:
    __slots__ = ("eng", "emit", "deps", "dma", "sig", "ord", "dsem", "dval", "idx", "dmadeps")

    def __init__(self, eng, emit, dma):
        self.eng = eng
        self.emit = emit
        self.dma = dma
        self.deps = []
        self.dmadeps = []
        self.sig = False
        self.ord = 0
        self.dsem = None
        self.dval = 0


class Prog:
    def __init__(self, nc, es, n_dma_sems=72):
        self.nc = nc
        self.es = es
        self.nepoch = 0
        self.esem = {e: es.enter_context(nc.semaphore("S_" + e)) for e in ENGS}
        self.ecount = {e: 0 for e in ENGS}
        self.bar = es.enter_context(nc.semaphore("BAR"))
        self.nbar = 0
        self.dsems = [es.enter_context(nc.semaphore("DQ%d" % i)) for i in range(n_dma_sems)]
        self.dtotal = [0] * n_dma_sems
        self.seen = {e: {} for e in ENGS}
        self._reset_phase()

    def new_epoch(self):
        self.nepoch += 1
        self.esem = {e: self.es.enter_context(self.nc.semaphore("S_%s_%d" % (e, self.nepoch))) for e in ENGS}
        self.ecount = {e: 0 for e in ENGS}
        for e in ENGS:
            for k in [k for k in self.seen[e] if isinstance(k, str)]:
                del self.seen[e][k]

    def _reset_phase(self):
        self.ops = {e: [] for e in ENGS}
        self.lastw = {}
        self.readers = {}
        self.keymap = {}

    def _track(self, o, r, w):
        deps = []
        for k in r:
            x = self.lastw.get(k)
            if x is not None:
                deps.append(x)
        for k in w:
            x = self.lastw.get(k)
            if x is not None:
                deps.append(x)
            deps.extend(self.readers.get(k, ()))
        seen = set()
        for d in deps:
            if id(d) in seen or d is o:
                continue
            seen.add(id(d))
            if d.dma:
                o.dmadeps.append((d.dsem, self.dtotal[d.dsem]))
            else:
                if d.eng == "pe" and o.eng == "pe" and not o.dma:
                    continue
                d.sig = True
                o.deps.append(d)
        for k in r:
            self.readers.setdefault(k, []).append(o)
        for k in w:
            self.lastw[k] = o
            self.readers[k] = []

    def op(self, eng, emit, r=(), w=()):
        o = _Op(eng, emit, False)
        self._track(o, r, w)
        self.ops[eng].append(o)
        return o

    def dma(self, q, out, in_, r=(), w=(), key=None, **kw):
        if key is None:
            key = w[0] if len(w) else r[0]
        if key not in self.keymap:
            self.keymap[key] = len(self.keymap)
            assert len(self.keymap) <= len(self.dsems), "too many dma keys"
        si = self.keymap[key]
        o = _Op(q, lambda e: e.dma_start(out=out, in_=in_, **kw), True)
        o.dsem = si
        self._track(o, r, w)
        self.dtotal[si] += 16
        o.dval = self.dtotal[si]
        self.ops[q].append(o)
        return o

    def flush(self, name=None):
        nc = self.nc
        last_compute = {}
        for e in ENGS:
            for o in self.ops[e]:
                if not o.dma:
                    last_compute[e] = o
        for e, o in last_compute.items():
            o.sig = True
        for e in ENGS:
            for o in self.ops[e]:
                if (not o.dma) and o.sig:
                    self.ecount[e] += 1
                    o.ord = self.ecount[e]
        self.nbar += 1
        nbar = self.nbar
        used_dsems = sorted(set(self.keymap.values()))

        def run(e, eng):
            seen = self.seen[e]

            def wait(sem, sid, val):
                if seen.get(sid, 0) >= val:
                    return
                seen[sid] = val
                eng.wait_ge(sem, val)

            my_dsems = set()
            for o in self.ops[e]:
                for d in o.deps:
                    wait(self.esem[d.eng], "E" + d.eng, d.ord)
                for (si, val) in o.dmadeps:
                    wait(self.dsems[si], si, val)
                ins = o.emit(eng)
                if o.dma:
                    ins.then_inc(self.dsems[o.dsem], 16)
                    my_dsems.add(o.dsem)
                elif o.sig:
                    ins.then_inc(self.esem[e], 1)
            if e in last_compute:
                wait(self.esem[e], "E" + e, last_compute[e].ord)
            for si in sorted(my_dsems):
                wait(self.dsems[si], si, self.dtotal[si])
            eng.sem_inc(self.bar, 1)
            eng.wait_ge(self.bar, len(ENGS) * nbar)

        with nc.Block() as block:
            block.tensor(lambda eng: run("pe", eng))
            block.scalar(lambda eng: run("act", eng))
            block.vector(lambda eng: run("dve", eng))
            block.gpsimd(lambda eng: run("pool", eng))
            block.sync(lambda eng: run("sp", eng))
        self._reset_phase()


def _rope_tables():
    t = np.arange(NLAT)
    row = (t // 64).astype(np.float32)
    col = (t % 64).astype(np.float32)
    inv = (10000.0 ** (-np.arange(0, 32, 2, dtype=np.float32) / 32.0)).astype(np.float32)
    ang = np.stack([row[:, None] * inv, col[:, None] * inv], axis=1).astype(np.float32)
    return np.cos(ang).astype(np.float32).reshape(NLAT, 32), np.sin(ang).astype(np.float32).reshape(NLAT, 32)


def _pool_matT(n, w):
    t = np.arange(n)
    lo = np.clip(t - w // 2, 0, n)
    hi = np.clip(t + w // 2, 0, n)
    A = np.zeros((n, n), dtype=np.float64)
    for i in range(n):
        A[i, lo[i]:hi[i]] = 1.0 / float(hi[i] - lo[i])
    A -= np.eye(n)
    return A.T.astype(np.float32)


def _band_consts():
    bl = np.zeros((4, 4, 6, 128, 512), dtype=np.float32)
    bc = np.zeros((4, 2, 128, 256), dtype=np.float32)
    for gi, w in enumerate(POOL_WINDOWS):
        MT = _pool_matT(NLAT, w)
        for j in range(4):
            for si in range(6):
                s = 4 * j - 1 + si
                if 0 <= s < 16:
                    bl[gi, j, si] = MT[s * 128:(s + 1) * 128, j * 512:(j + 1) * 512]
        MC = _pool_matT(NCTX, w)
        for s in range(2):
            bc[gi, s] = MC[s * 128:(s + 1) * 128, :]
    return bl.astype(ml_dtypes.bfloat16), bc.astype(ml_dtypes.bfloat16)


def _blocks(include_ctx=True):
    out = []
    for b in range(NB):
        for j in range(4):
            out.append((b * NBT + j * 512, 512, b, False, b, j * 512))
        if include_ctx:
            out.append((b * NBT + NLAT, NCTX, 2, True, b, 0))
    return out


def _bcast_rows(ap2d, nparts=128):
    t = ap2d.partition_broadcast(nparts)
    if len(t.shape) == 3:
        t = t[:, 0, :]
    return t


def build_program(layers, single_layer_inputs, debug=None):
    nc = bass.Bass("TRN2", target_bir_lowering=False)

    def din(name, shape, dt=F32):
        return nc.dram_tensor(name, list(shape), dt, kind="ExternalInput").ap()

    xin = din("xin", [NT, D])
    c3 = din("c3", [3, D])
    nL = len(layers)
    gath = [[nc.dram_tensor("wpk_%d_%d" % (pos, k), [CHUNK_ROWS[k], 2048], F32, kind="ExternalInput") for k in range(NCHUNK)] for pos in range(nL)]

    def layer_views(pos):
        v = {}
        for name, shape, ck, row0 in PACK_SPEC:
            off = row0 * 2048
            ap = []
            stride = 1
            for d_ in reversed(shape):
                ap.insert(0, [stride, d_])
                stride *= d_
            ap.insert(0, [0, 1])
            v[name] = bass.AP(gath[pos][ck], off, ap)
        v["w_eg"] = v["w_e_gate"]; v["w_eu"] = v["w_e_up"]; v["w_ed"] = v["w_e_down"]
        return v
    rope_cos = din("rope_cos", [NLAT, 32])
    rope_sin = din("rope_sin", [NLAT, 32])
    bandL = din("bandL", [4, 4, 6, 128, 512], BF16)
    bandC = din("bandC", [4, 2, 128, 256], BF16)
    ident_in = din("ident", [128, 128], BF16)

    xout = nc.dram_tensor("xout", [NT, D], F32, kind="ExternalOutput").ap()
    dbg_out = None

    def scratch(name, shape, dt):
        if debug and name in debug:
            return nc.dram_tensor(name, list(shape), dt, kind="ExternalOutput").ap()
        return nc.dram_tensor(name, list(shape), dt).ap()

    xs = xout
    mod_d = scratch("mod_d", [3, 6 * D], F32)
    hT_d = scratch("hT_d", [D, NT], BF16)
    qT_d = scratch("qT_d", [8, 192, NT], BF16)
    knT_d = scratch("knT_d", [8, 128, NT], BF16)
    krT_d = scratch("krT_d", [64, NT], BF16)
    v_d = scratch("v_d", [NT, 8, 128], BF16)
    r_d = scratch("r_d", [NT, 8], F32)
    bT_d = scratch("bT_d", [1024, NT], BF16)
    cpT_d = scratch("cpT_d", [1024, NT], BF16)
    aT_d = scratch("aT_d", [1024, NT], BF16)
    yT_d = scratch("yT_d", [D, NT], BF16)
    g_d = scratch("g_d", [NT, 32], F32)
    ymoe_d = scratch("ymoe_d", [NT, D], F32)

    with ExitStack() as gs:
        P = Prog(nc, gs)

        uid = [0]

        def sb(es, name, shape, dt):
            uid[0] += 1
            return es.enter_context(nc.sbuf_tensor("%s_s%d" % (name, uid[0]), list(shape), dt))

        def ps(es, name, shape, dt=F32):
            uid[0] += 1
            return es.enter_context(nc.psum_tensor("%s_p%d" % (name, uid[0]), list(shape), dt))

        for i in range(4):
            r0 = i * (NT // 4)
            P.dma("sp", xs[r0:r0 + NT // 4, :], xin[r0:r0 + NT // 4, :], w=[("xs", i)])
        P.flush()

        base_G = dict(locals())
        for li_pos, li in enumerate(layers):
            last_layer = (li == DEPTH - 1)
            ctx_out = not last_layer
            if li_pos > 0:
                P.new_epoch()
            Gl = dict(base_G)
            Gl.update(layer_views(li_pos))
            _layer(nc, P, sb, ps, 0, ctx_out, Gl, None)
    return nc


def _load_modT(nc, P, es, sb, mod_d, j, name):
    t = sb(es, name, [128, 3, 16], F32)
    for r_ in range(3):
        src = bass.AP(mod_d.tensor, r_ * 6 * D + j * D, [[1, 128], [128, 16]])
        P.dma("sp", t[:, r_, :], src, w=[name], allow_slow_non_contiguous=True)
    return t


def _norm_mod_tile(P, xt, xkey, jk, jkey, st, stkey, xnb, xnkey, psT, pskey, ident, tmpf, tmpkey,
                   scp1, shv, row, hT_dst, hkey, extra_r=()):
    P.op("act", lambda e: e.activation(out=jk[:], in_=xt[:], func=AF.Square, accum_out=st[:, 0:1]),
         r=[xkey], w=[jkey, stkey])
    P.op("act", lambda e: e.activation(out=st[:, 1:2], in_=st[:, 0:1], func=AF.Sqrt, bias=EPS, scale=1.0 / D),
         r=[stkey], w=[stkey])
    P.op("dve", lambda e: e.reciprocal(out=st[:, 2:3], in_=st[:, 1:2]), r=[stkey], w=[stkey])
    P.op("dve", lambda e: e.tensor_scalar(out=xnb[:], in0=xt[:], scalar1=st[:, 2:3], scalar2=None, op0=ALU.mult),
         r=[xkey, stkey], w=[xnkey])
    for c in range(KC):
        P.op("pe", lambda e, c=c: e.transpose(out=psT[:, c, :], in_=xnb[:, c * 128:(c + 1) * 128], identity=ident[:]),
             r=[xnkey, "ident"], w=[pskey])
    P.op("dve", lambda e: e.tensor_tensor(out=tmpf[:], in0=psT[:], in1=scp1[:, row, :].unsqueeze(2).to_broadcast([128, KC, 128]),
                                          op=ALU.mult), r=[pskey, "modsc"] + list(extra_r), w=[tmpkey])
    P.op("dve", lambda e: e.tensor_tensor(out=hT_dst, in0=tmpf[:], in1=shv[:, row, :].unsqueeze(2).to_broadcast([128, KC, 128]),
                                          op=ALU.add), r=[tmpkey, "modsh"], w=[hkey])


def _layer(nc, P, sb, ps, lw, ctx_out, G, dbg_out):
    xs = G["xs"]; c3 = G["c3"]; mod_d = G["mod_d"]
    w_ada = G["w_ada"]; b_ada = G["b_ada"]; w_in = G["w_in"]
    hT_d = G["hT_d"]; qT_d = G["qT_d"]; knT_d = G["knT_d"]; krT_d = G["krT_d"]; v_d = G["v_d"]; r_d = G["r_d"]
    bT_d = G["bT_d"]; cpT_d = G["cpT_d"]; aT_d = G["aT_d"]; yT_d = G["yT_d"]
    ident_in = G["ident_in"]
    blocks_all = _blocks(True)
    blocks_out = _blocks(ctx_out)

    with ExitStack() as es:
        c3T = sb(es, "c3T", [128, 3, KC], F32)
        scT = sb(es, "scT", [128, KC, 3], F32)
        bias3 = sb(es, "bias3", [3, 6 * D], F32)
        modsb = sb(es, "modsb", [3, 6 * D], F32)
        wts = [sb(es, "wada%d" % i, [128, KC, 512], F32) for i in range(2)]
        pms = [ps(es, "pmod%d" % i, [3, 512]) for i in range(2)]
        for r_ in range(3):
            P.dma("sp", c3T[:, r_, :], bass.AP(c3.tensor, r_ * D, [[1, 128], [128, KC]]), w=["c3T"], allow_slow_non_contiguous=True)
        P.dma("sp", bias3[:], _bcast_rows(b_ada[lw:lw + 1, :], 3), w=["bias3"])
        P.op("act", lambda e: e.activation(out=scT[:], in_=c3T[:].rearrange("p r c -> p c r"), func=AF.Silu), r=["c3T"], w=["scT"])
        for jc in range(24):
            wt = wts[jc % 2]; pm = pms[jc % 2]
            wk = ("wada", jc % 2); pk = ("pmod", jc % 2)
            P.dma("sp", wt[:], w_ada[lw, :, jc * 512:(jc + 1) * 512].rearrange("(c p) n -> p c n", p=128), w=[wk])
            for c in range(KC):
                P.op("pe", lambda e, c=c, wt=wt, pm=pm: e.matmul(pm[:], lhsT=scT[:, c, :], rhs=wt[:, c, :], start=(c == 0), stop=(c == KC - 1)),
                     r=["scT", wk], w=[pk])
            P.op("dve", lambda e, pm=pm, jc=jc: e.tensor_tensor(out=modsb[:, jc * 512:(jc + 1) * 512], in0=pm[:], in1=bias3[:, jc * 512:(jc + 1) * 512], op=ALU.add),
                 r=[pk, "bias3"], w=[("modsb", jc)])
        P.dma("sp", mod_d[:, :], modsb[:], r=[("modsb", jc) for jc in range(24)], w=["mod_d"])
        P.flush()

    if dbg_out is not None and "mod" in dbg_out:
        with ExitStack() as es:
            t = sb(es, "dbgmod", [3, 6 * D], F32)
            P.dma("sp", t[:], mod_d[:, :], w=["t"])
            P.dma("sp", dbg_out["mod"][:, :], t[:], r=["t"], w=["o"])
            P.flush()

    phases = [("m1a", _phase_m1a), ("sgu", _phase_sgu), ("pool", _phase_pool), ("attn", _phase_attn),
              ("merge", _phase_merge), ("outproj", _phase_outproj), ("moe", _phase_moe)]
    for name, fn in phases:
        if STOP_AFTER is not None and STOP_AFTER == "adaln":
            break
        fn(nc, P, sb, ps, lw, ctx_out, G)
        if STOP_AFTER is not None and STOP_AFTER == name:
            break


def _phase_m1a(nc, P, sb, ps, lw, ctx_out, G):
    xs = G["xs"]; mod_d = G["mod_d"]; w_in = G["w_in"]
    hT_d = G["hT_d"]; qT_d = G["qT_d"]; knT_d = G["knT_d"]; krT_d = G["krT_d"]; v_d = G["v_d"]; r_d = G["r_d"]
    with ExitStack() as es:
        win_a = sb(es, "win_a", [128, KC, 832], BF16)
        wuq = sb(es, "wuq", [128, 4, 1536], BF16)
        wukv = sb(es, "wukv", [128, 2, 2048], BF16)
        ident = sb(es, "ident", [128, 128], BF16)
        gT6 = sb(es, "gT6", [128, 6], F32)
        gq_bc = sb(es, "gq_bc", [128, 192], F32)
        gk_bc = sb(es, "gk_bc", [128, 192], F32)
        xt = [sb(es, "xt%d" % i, [128, D], F32) for i in range(2)]
        jk = sb(es, "jk", [128, D], BF16)
        st = [sb(es, "st%d" % i, [128, 16], F32) for i in range(2)]
        s8 = [sb(es, "s8_%d" % i, [128, 4, 8], F32) for i in range(2)]
        xnb = [sb(es, "xnb%d" % i, [128, D], BF16) for i in range(2)]
        tmpf = sb(es, "tmpf", [128, KC, 128], F32)
        hT = sb(es, "hT", [128, KC, 512], BF16)
        cn = [sb(es, "cn%d" % i, [128, 768], BF16) for i in range(2)]
        krf = sb(es, "krf", [128, 4, 64], F32)
        cT = sb(es, "cT", [128, 6, 512], BF16)
        sqf = sb(es, "sqf", [128, 1536], F32)
        qg = sb(es, "qg", [128, 8, 192], F32)
        qr = sb(es, "qr", [128, 8, 64], F32)
        rtmp = sb(es, "rtmp", [128, 4, 8, 32], F32)
        qb = sb(es, "qb", [128, 8, 192], BF16)
        qTn_blk = sb(es, "qTn_blk", [128, 8, 512], BF16)
        qTr_blk = sb(es, "qTr_blk", [64, 8, 512], BF16)
        kTn_blk = sb(es, "kTn_blk", [128, 8, 512], BF16)
        krT_blk = sb(es, "krT_blk", [64, 512], BF16)
        knb = sb(es, "knb", [128, 8, 128], BF16)
        vb = [sb(es, "vb%d" % i, [128, 8, 128], BF16) for i in range(2)]
        krg = sb(es, "krg", [128, 64], F32)
        krb = sb(es, "krb", [128, 64], BF16)
        ktmp = sb(es, "ktmp", [128, 4, 32], F32)
        rk_blk = sb(es, "rk_blk", [128, 4, 8], F32)
        cs = [sb(es, "cos%d" % i, [128, 32], F32) for i in range(2)]
        sn = [sb(es, "sin%d" % i, [128, 32], F32) for i in range(2)]
        psT = ps(es, "psT", [128, 2048], BF16)
        psA = ps(es, "psA", [128, 512])
        psB = ps(es, "psB", [128, 512])
        psC = ps(es, "psC", [128, 1024], BF16)
        psQ = ps(es, "psQ", [128, 1536])
        psT3 = psT[:].rearrange("p (c n) -> p c n", c=KC)

        P.dma("pool", win_a[:], w_in[lw, :, 0:832].rearrange("(c p) n -> p c n", p=128), w=["win_a"])
        P.dma("pool", wuq[:], G["w_uq"][lw].rearrange("(c p) n -> p c n", p=128), w=["wuq"])
        P.dma("pool", wukv[:], G["w_ukv"][lw].rearrange("(c p) n -> p c n", p=128), w=["wukv"])
        P.dma("sp", ident[:], G["ident_in"][:, :], w=["ident"])
        P.dma("sp", gT6[:, 0:4], bass.AP(G["g_cq"].tensor, G["g_cq"].offset, [[1, 128], [128, 4]]), w=["gT6a"], allow_slow_non_contiguous=True)
        P.dma("sp", gT6[:, 4:6], bass.AP(G["g_ckv"].tensor, G["g_ckv"].offset, [[1, 128], [128, 2]]), w=["gT6b"], allow_slow_non_contiguous=True)
        P.dma("sp", gq_bc[:], _bcast_rows(G["g_q"][lw:lw + 1, :]), w=["gq_bc"])
        P.dma("sp", gk_bc[:], _bcast_rows(G["g_k"][lw:lw + 1, :]), w=["gk_bc"])
        sc1 = _load_modT(nc, P, es, sb, mod_d, 1, "modsc")
        sh1 = _load_modT(nc, P, es, sb, mod_d, 0, "modsh")
        P.op("dve", lambda e: e.tensor_scalar(out=sc1[:], in0=sc1[:], scalar1=1.0, scalar2=None, op0=ALU.add), r=["modsc"], w=["modsc"])

        gt = 0
        for (tok0, ntok, row, is_ctx, b, pos0) in _blocks(True):
            need_q = ctx_out or (not is_ctx)
            ntile = ntok // 128
            for t in range(ntile):
                sl = gt % 2; gt += 1
                r0 = tok0 + t * 128
                P.dma("pool", xt[sl][:], xs[r0:r0 + 128, :], w=[("xt", sl)])
                _norm_mod_tile(P, xt[sl], ("xt", sl), jk, "jk", st[sl], ("st", sl), xnb[sl], ("xnb", sl), psT3, "psT", ident,
                               tmpf, "tmpf", sc1, sh1, row, hT[:, :, t * 128:(t + 1) * 128], ("hT", t))
            P.dma("sp", hT_d[:, tok0:tok0 + ntok].rearrange("(c p) n -> p c n", p=128), hT[:, :, 0:ntok],
                  r=[("hT", t) for t in range(ntile)], key="st_hT")
            for t in range(ntile):
                sl = t % 2
                stt = st[sl]; sk = ("st", sl)
                for c in range(KC):
                    P.op("pe", lambda e, c=c, t=t: e.matmul(psA[:], lhsT=hT[:, c, t * 128:(t + 1) * 128], rhs=win_a[:, c, 0:512], start=(c == 0), stop=(c == KC - 1)),
                         r=[("hT", t), "win_a"], w=["psA"])
                for c in range(KC):
                    P.op("pe", lambda e, c=c, t=t: e.matmul(psB[:, 0:320], lhsT=hT[:, c, t * 128:(t + 1) * 128], rhs=win_a[:, c, 512:832], start=(c == 0), stop=(c == KC - 1)),
                         r=[("hT", t), "win_a"], w=["psB"])
                P.op("act", lambda e, stt=stt: e.activation(out=jk[:, 0:512], in_=psA[:], func=AF.Square, accum_out=stt[:, 3:4]), r=["psA"], w=["jk", sk])
                P.op("act", lambda e, stt=stt: e.activation(out=jk[:, 512:768], in_=psB[:, 0:256], func=AF.Square, accum_out=stt[:, 6:7]), r=["psB"], w=["jk", sk])
                P.op("act", lambda e, stt=stt: e.activation(out=stt[:, 4:5], in_=stt[:, 3:4], func=AF.Sqrt, bias=EPS, scale=1.0 / 512), r=[sk], w=[sk])
                P.op("act", lambda e, stt=stt: e.activation(out=stt[:, 7:8], in_=stt[:, 6:7], func=AF.Sqrt, bias=EPS, scale=1.0 / 256), r=[sk], w=[sk])
                P.op("dve", lambda e, stt=stt: e.reciprocal(out=stt[:, 5:6], in_=stt[:, 4:5]), r=[sk], w=[sk])
                P.op("dve", lambda e, stt=stt: e.reciprocal(out=stt[:, 8:9], in_=stt[:, 7:8]), r=[sk], w=[sk])
                cnt = cn[sl]; ck = ("cn", sl)
                P.op("dve", lambda e, stt=stt, cnt=cnt: e.tensor_scalar(out=cnt[:, 0:512], in0=psA[:], scalar1=stt[:, 5:6], scalar2=None, op0=ALU.mult), r=["psA", sk], w=[ck])
                P.op("dve", lambda e, stt=stt, cnt=cnt: e.tensor_scalar(out=cnt[:, 512:768], in0=psB[:, 0:256], scalar1=stt[:, 8:9], scalar2=None, op0=ALU.mult), r=["psB", sk, ck], w=[ck])
                P.op("act", lambda e, t=t: e.copy(out=krf[:, t, :], in_=psB[:, 256:320]), r=["psB"], w=[("krf", t)])
                for c in range(6):
                    P.op("pe", lambda e, c=c, cnt=cnt: e.transpose(out=psC[:, c * 128:(c + 1) * 128], in_=cnt[:, c * 128:(c + 1) * 128], identity=ident[:]),
                         r=[ck, "ident"], w=["psC"])
                P.op("dve", lambda e, t=t: e.tensor_tensor(out=cT[:, :, t * 128:(t + 1) * 128], in0=psC[:, 0:768].rearrange("p (c n) -> p c n", c=6),
                                                         in1=gT6[:].unsqueeze(2).to_broadcast([128, 6, 128]), op=ALU.mult),
                     r=["psC", "gT6a", "gT6b"], w=[("cT", t)])
            for t in range(ntile):
                sl = t % 2
                r0 = tok0 + t * 128
                if not is_ctx:
                    P.dma("pool", cs[sl][:], G["rope_cos"][pos0 + t * 128:pos0 + (t + 1) * 128, :], w=[("cos", sl)])
                    P.dma("pool", sn[sl][:], G["rope_sin"][pos0 + t * 128:pos0 + (t + 1) * 128, :], w=[("sin", sl)])
                cosb = cs[sl]; sinb = sn[sl]; ckk = ("cos", sl); skk = ("sin", sl)
                s8t = s8[sl]; s8k = ("s8", sl)
                if need_q:
                    for j in range(3):
                        for c in range(4):
                            P.op("pe", lambda e, c=c, j=j, t=t: e.matmul(psQ[:, j * 512:(j + 1) * 512], lhsT=cT[:, c, t * 128:(t + 1) * 128], rhs=wuq[:, c, j * 512:(j + 1) * 512], start=(c == 0), stop=(c == 3)),
                                 r=[("cT", t), "wuq"], w=["psQ"])
                    psQ3 = psQ[:].rearrange("p (h d) -> p h d", h=8)
                    P.op("act", lambda e: e.activation(out=sqf[:], in_=psQ[:], func=AF.Square), r=["psQ"], w=["sqf"])
                    P.op("dve", lambda e, s8t=s8t: e.tensor_reduce(out=s8t[:, 0, :], in_=sqf[:].rearrange("p (h d) -> p h d", h=8), axis=AX.X, op=ALU.add), r=["sqf"], w=[s8k])
                    P.op("act", lambda e, s8t=s8t: e.activation(out=s8t[:, 1, :], in_=s8t[:, 0, :], func=AF.Sqrt, bias=EPS, scale=1.0 / 192), r=[s8k], w=[s8k])
                    P.op("dve", lambda e, s8t=s8t: e.reciprocal(out=s8t[:, 2, :], in_=s8t[:, 1, :]), r=[s8k], w=[s8k])
                    P.op("dve", lambda e, s8t=s8t, psQ3=psQ3: e.tensor_tensor(out=qg[:], in0=psQ3, in1=s8t[:, 2, :].unsqueeze(2).to_broadcast([128, 8, 192]), op=ALU.mult), r=["psQ", s8k], w=["qg"])
                    P.op("dve", lambda e: e.tensor_tensor(out=qb[:, :, 0:128], in0=qg[:, :, 0:128], in1=gq_bc[:, 0:128].unsqueeze(1).to_broadcast([128, 8, 128]), op=ALU.mult), r=["qg", "gq_bc"], w=["qbn"])
                    if is_ctx:
                        P.op("dve", lambda e: e.tensor_tensor(out=qb[:, :, 128:192], in0=qg[:, :, 128:192], in1=gq_bc[:, 128:192].unsqueeze(1).to_broadcast([128, 8, 64]), op=ALU.mult), r=["qg", "gq_bc"], w=["qbr"])
                    else:
                        P.op("dve", lambda e: e.tensor_tensor(out=qr[:], in0=qg[:, :, 128:192], in1=gq_bc[:, 128:192].unsqueeze(1).to_broadcast([128, 8, 64]), op=ALU.mult), r=["qg", "gq_bc"], w=["qr"])
                        q5 = qr[:].rearrange("p h (a s f) -> p h a s f", a=2, s=2)
                        o5 = qb[:, :, 128:192].rearrange("p h (a s f) -> p h a s f", a=2, s=2)
                        x1 = q5[:, :, :, 0, :]; x2 = q5[:, :, :, 1, :]
                        cb = cosb[:].rearrange("p (a f) -> p a f", a=2).unsqueeze(1).to_broadcast([128, 8, 2, 16])
                        sbb = sinb[:].rearrange("p (a f) -> p a f", a=2).unsqueeze(1).to_broadcast([128, 8, 2, 16])
                        tv = [rtmp[:, i].rearrange("p h (a f) -> p h a f", a=2) for i in range(4)]
                        P.op("dve", lambda e, x1=x1, cb=cb, tv=tv: e.tensor_tensor(out=tv[0], in0=x1, in1=cb, op=ALU.mult), r=["qr", ckk], w=[("rt", 0)])
                        P.op("dve", lambda e, x2=x2, sbb=sbb, tv=tv: e.tensor_tensor(out=tv[1], in0=x2, in1=sbb, op=ALU.mult), r=["qr", skk], w=[("rt", 1)])
                        P.op("dve", lambda e, x1=x1, sbb=sbb, tv=tv: e.tensor_tensor(out=tv[2], in0=x1, in1=sbb, op=ALU.mult), r=["qr", skk], w=[("rt", 2)])
                        P.op("dve", lambda e, x2=x2, cb=cb, tv=tv: e.tensor_tensor(out=tv[3], in0=x2, in1=cb, op=ALU.mult), r=["qr", ckk], w=[("rt", 3)])
                        P.op("dve", lambda e, o5=o5, tv=tv: e.tensor_tensor(out=o5[:, :, :, 0, :], in0=tv[0], in1=tv[1], op=ALU.subtract), r=[("rt", 0), ("rt", 1)], w=["qbr"])
                        P.op("dve", lambda e, o5=o5, tv=tv: e.tensor_tensor(out=o5[:, :, :, 1, :], in0=tv[2], in1=tv[3], op=ALU.add), r=[("rt", 2), ("rt", 3), "qbr"], w=["qbr"])
                    for h in range(8):
                        P.op("pe", lambda e, h=h: e.transpose(out=psT[:, h * 128:(h + 1) * 128], in_=qb[:, h, 0:128], identity=ident[:]), r=["qbn", "ident"], w=["psT"])
                    for h in range(8):
                        P.op("pe", lambda e, h=h: e.transpose(out=psT[0:64, 1024 + h * 128:1024 + (h + 1) * 128], in_=qb[:, h, 128:192], identity=ident[:]), r=["qbr", "ident"], w=["psT"])
                    P.op("act", lambda e, t=t: e.copy(out=qTn_blk[:, :, t * 128:(t + 1) * 128], in_=psT[:, 0:1024].rearrange("p (h n) -> p h n", h=8)), r=["psT"], w=[("qTn", t)])
                    P.op("act", lambda e, t=t: e.copy(out=qTr_blk[:, :, t * 128:(t + 1) * 128], in_=psT[0:64, 1024:2048].rearrange("p (h n) -> p h n", h=8)), r=["psT"], w=[("qTr", t)])
                vbt = vb[sl]; vk = ("vb", sl)
                for hh in range(2):
                    for j in range(2):
                        for c in range(2):
                            P.op("pe", lambda e, c=c, j=j, hh=hh, t=t: e.matmul(psQ[:, j * 512:(j + 1) * 512], lhsT=cT[:, 4 + c, t * 128:(t + 1) * 128],
                                                                              rhs=wukv[:, c, hh * 1024 + j * 512:hh * 1024 + (j + 1) * 512], start=(c == 0), stop=(c == 1)),
                                 r=[("cT", t), "wukv"], w=["psQ"])
                    kv4 = psQ[:, 0:1024].rearrange("p (h d) -> p h d", h=4)
                    P.op("act", lambda e, kv4=kv4: e.activation(out=sqf[:, 0:512].rearrange("p (h d) -> p h d", h=4), in_=kv4[:, :, 0:128], func=AF.Square), r=["psQ"], w=["sqf"])
                    P.op("dve", lambda e, s8t=s8t, hh=hh: e.tensor_reduce(out=s8t[:, 0, hh * 4:(hh + 1) * 4], in_=sqf[:, 0:512].rearrange("p (h d) -> p h d", h=4), axis=AX.X, op=ALU.add), r=["sqf", s8k], w=[s8k])
                    P.op("dve", lambda e, kv4=kv4, hh=hh: e.tensor_tensor(out=knb[:, hh * 4:(hh + 1) * 4, :], in0=kv4[:, :, 0:128], in1=gk_bc[:, 0:128].unsqueeze(1).to_broadcast([128, 4, 128]), op=ALU.mult),
                         r=["psQ", "gk_bc"], w=[("knb", hh)])
                    P.op("act", lambda e, kv4=kv4, hh=hh, vbt=vbt: e.copy(out=vbt[:, hh * 4:(hh + 1) * 4, :], in_=kv4[:, :, 128:256]), r=["psQ"], w=[vk])
                P.dma("sp", v_d[r0:r0 + 128, :, :], vbt[:], r=[vk], key=("st_v", sl))
                stt = st[sl]; sk = ("st", sl)
                P.op("act", lambda e, stt=stt, t=t: e.activation(out=jk[:, 0:64], in_=krf[:, t, :], func=AF.Square, accum_out=stt[:, 9:10]), r=[("krf", t)], w=["jk", sk])
                P.op("dve", lambda e, s8t=s8t, stt=stt: e.tensor_scalar(out=s8t[:, 1, :], in0=s8t[:, 0, :], scalar1=stt[:, 9:10], scalar2=None, op0=ALU.add), r=[s8k, sk], w=[s8k])
                P.op("act", lambda e, s8t=s8t: e.activation(out=s8t[:, 2, :], in_=s8t[:, 1, :], func=AF.Sqrt, bias=EPS, scale=1.0 / 192), r=[s8k], w=[s8k])
                P.op("dve", lambda e, s8t=s8t: e.reciprocal(out=s8t[:, 3, :], in_=s8t[:, 2, :]), r=[s8k], w=[s8k])
                P.op("dve", lambda e, s8t=s8t, t=t: e.tensor_scalar(out=rk_blk[:, t, :], in0=s8t[:, 3, :], scalar1=ATTN_SCALE, scalar2=None, op0=ALU.mult), r=[s8k], w=[("rk", t)])
                for h in range(8):
                    P.op("pe", lambda e, h=h: e.transpose(out=psT[:, h * 128:(h + 1) * 128], in_=knb[:, h, :], identity=ident[:]), r=[("knb", 0), ("knb", 1), "ident"], w=["psT"])
                P.op("act", lambda e, t=t: e.copy(out=kTn_blk[:, :, t * 128:(t + 1) * 128], in_=psT[:, 0:1024].rearrange("p (h n) -> p h n", h=8)), r=["psT"], w=[("kTn", t)])
                if is_ctx:
                    P.op("dve", lambda e, t=t: e.tensor_tensor(out=krb[:], in0=krf[:, t, :], in1=gk_bc[:, 128:192], op=ALU.mult), r=[("krf", t), "gk_bc"], w=["krb"])
                else:
                    P.op("dve", lambda e, t=t: e.tensor_tensor(out=krg[:], in0=krf[:, t, :], in1=gk_bc[:, 128:192], op=ALU.mult), r=[("krf", t), "gk_bc"], w=["krg"])
                    k4 = krg[:].rearrange("p (a s f) -> p a s f", a=2, s=2)
                    ko = krb[:].rearrange("p (a s f) -> p a s f", a=2, s=2)
                    c3_ = cosb[:].rearrange("p (a f) -> p a f", a=2)
                    s3_ = sinb[:].rearrange("p (a f) -> p a f", a=2)
                    kt = [ktmp[:, i, :].rearrange("p (a f) -> p a f", a=2) for i in range(4)]
                    P.op("dve", lambda e, k4=k4, c3_=c3_, kt=kt: e.tensor_tensor(out=kt[0], in0=k4[:, :, 0, :], in1=c3_, op=ALU.mult), r=["krg", ckk], w=[("kt", 0)])
                    P.op("dve", lambda e, k4=k4, s3_=s3_, kt=kt: e.tensor_tensor(out=kt[1], in0=k4[:, :, 1, :], in1=s3_, op=ALU.mult), r=["krg", skk], w=[("kt", 1)])
                    P.op("dve", lambda e, k4=k4, s3_=s3_, kt=kt: e.tensor_tensor(out=kt[2], in0=k4[:, :, 0, :], in1=s3_, op=ALU.mult), r=["krg", skk], w=[("kt", 2)])
                    P.op("dve", lambda e, k4=k4, c3_=c3_, kt=kt: e.tensor_tensor(out=kt[3], in0=k4[:, :, 1, :], in1=c3_, op=ALU.mult), r=["krg", ckk], w=[("kt", 3)])
                    P.op("dve", lambda e, ko=ko, kt=kt: e.tensor_tensor(out=ko[:, :, 0, :], in0=kt[0], in1=kt[1], op=ALU.subtract), r=[("kt", 0), ("kt", 1)], w=["krb"])
                    P.op("dve", lambda e, ko=ko, kt=kt: e.tensor_tensor(out=ko[:, :, 1, :], in0=kt[2], in1=kt[3], op=ALU.add), r=[("kt", 2), ("kt", 3), "krb"], w=["krb"])
                P.op("pe", lambda e: e.transpose(out=psT[0:64, 1024:1152], in_=krb[:], identity=ident[:]), r=["krb", "ident"], w=["psT"])
                P.op("act", lambda e, t=t: e.copy(out=krT_blk[:, t * 128:(t + 1) * 128], in_=psT[0:64, 1024:1152]), r=["psT"], w=[("krT", t)])
            tl = list(range(ntile))
            if need_q:
                P.dma("sp", qT_d[:, 0:128, tok0:tok0 + ntok].rearrange("h p n -> p h n"), qTn_blk[:, :, 0:ntok], r=[("qTn", t) for t in tl], key="st_qTn")
                P.dma("sp", qT_d[:, 128:192, tok0:tok0 + ntok].rearrange("h p n -> p h n"), qTr_blk[:, :, 0:ntok], r=[("qTr", t) for t in tl], key="st_qTr")
            P.dma("sp", knT_d[:, :, tok0:tok0 + ntok].rearrange("h p n -> p h n"), kTn_blk[:, :, 0:ntok], r=[("kTn", t) for t in tl], key="st_kTn")
            P.dma("sp", krT_d[:, tok0:tok0 + ntok], krT_blk[:, 0:ntok], r=[("krT", t) for t in tl], key="st_krT")
            P.dma("sp", r_d[tok0:tok0 + ntok, :].rearrange("(t p) e -> p t e", p=128), rk_blk[:, 0:ntile, :], r=[("rk", t) for t in tl], key="st_rk")
        P.flush()


def _phase_sgu(nc, P, sb, ps, lw, ctx_out, G):
    w_in = G["w_in"]; hT_d = G["hT_d"]; bT_d = G["bT_d"]
    with ExitStack() as es:
        win_b = sb(es, "win_b", [128, KC, 2048], BF16)
        wsT = sb(es, "wsT", [128, 8, 128], BF16)
        bs_bc = sb(es, "bs_bc", [128, 8, 128], F32)
        gs_bc = sb(es, "gs_bc", [128, 1024], F32)
        hT = [sb(es, "hT%d" % i, [128, KC, 512], BF16) for i in range(2)]
        ut = sb(es, "ut", [128, 8, 128], F32)
        gv = [sb(es, "gv%d" % i, [128, 1024], F32) for i in range(2)]
        jk = sb(es, "jk", [128, 1024], BF16)
        st = [sb(es, "st%d" % i, [128, 4], F32) for i in range(2)]
        vnb = [sb(es, "vnb%d" % i, [128, 8, 128], BF16) for i in range(2)]
        tmp = sb(es, "tmp", [128, 8, 128], F32)
        bT = [sb(es, "bT%d" % i, [128, 8, 512], BF16) for i in range(2)]
        psUt = ps(es, "psUt", [128, 8, 128])
        psV = [ps(es, "psV%d" % i, [128, 1024]) for i in range(2)]
        psS = ps(es, "psS", [128, 8, 128])

        for half in range(2):
            P.dma("pool", win_b[:, :, half * 1024:(half + 1) * 1024], w_in[lw, :, 832 + half * 1024:832 + (half + 1) * 1024].rearrange("(c p) n -> p c n", p=128), w=[("win_b", half)])
        P.dma("pool", wsT[:], G["w_sguT"][lw].rearrange("g p q -> p g q"), w=["wsT"])
        P.dma("sp", bs_bc[:].rearrange("p g q -> p (g q)"), _bcast_rows(G["b_sgu"][lw:lw + 1, :]), w=["bs_bc"])
        P.dma("sp", gs_bc[:], _bcast_rows(G["g_sgu"][lw:lw + 1, :]), w=["gs_bc"])

        gt = 0
        for bi, (tok0, ntok, row, is_ctx, b, pos0) in enumerate(_blocks(ctx_out)):
            ntile = ntok // 128
            hs = bi % 2
            hTb = hT[hs]; hk = ("hT", hs)
            bTb = bT[hs]
            P.dma("pool", hTb[:, :, 0:ntok], hT_d[:, tok0:tok0 + ntok].rearrange("(c p) n -> p c n", p=128), w=[hk])
            for t in range(ntile):
                sl = gt % 2; gt += 1
                pv = psV[sl]; pvk = ("psV", sl)
                for j in range(2):
                    for c in range(KC):
                        P.op("pe", lambda e, c=c, j=j, t=t, pv=pv, hTb=hTb: e.matmul(pv[:, j * 512:(j + 1) * 512], lhsT=hTb[:, c, t * 128:(t + 1) * 128], rhs=win_b[:, c, 1024 + j * 512:1024 + (j + 1) * 512], start=(c == 0), stop=(c == KC - 1)),
                             r=[hk, ("win_b", 1)], w=[pvk])
                for g in range(8):
                    for c in range(KC):
                        P.op("pe", lambda e, c=c, g=g, t=t, hTb=hTb: e.matmul(psUt[:, g, :], lhsT=win_b[:, c, g * 128:(g + 1) * 128], rhs=hTb[:, c, t * 128:(t + 1) * 128], start=(c == 0), stop=(c == KC - 1)),
                             r=[hk, ("win_b", 0)], w=["psUt"])
                P.op("act", lambda e: e.activation(out=ut[:], in_=psUt[:], func=AF.Gelu), r=["psUt"], w=["ut"])
                gvt = gv[sl]; gk = ("gv", sl); stt = st[sl]; sk = ("st", sl)
                P.op("act", lambda e, gvt=gvt, pv=pv: e.activation(out=gvt[:], in_=pv[:], func=AF.Gelu), r=[pvk], w=[gk])
                P.op("act", lambda e, gvt=gvt, stt=stt: e.activation(out=jk[:], in_=gvt[:], func=AF.Square, accum_out=stt[:, 0:1]), r=[gk], w=["jk", sk])
                P.op("act", lambda e, stt=stt: e.activation(out=stt[:, 1:2], in_=stt[:, 0:1], func=AF.Sqrt, bias=EPS, scale=1.0 / 1024), r=[sk], w=[sk])
                P.op("dve", lambda e, stt=stt: e.reciprocal(out=stt[:, 2:3], in_=stt[:, 1:2]), r=[sk], w=[sk])
                vt = vnb[sl]; vk = ("vnb", sl)
                P.op("dve", lambda e, gvt=gvt, stt=stt, vt=vt: e.scalar_tensor_tensor(out=vt[:].rearrange("p g c -> p (g c)"), in0=gvt[:], scalar=stt[:, 2:3], in1=gs_bc[:], op0=ALU.mult, op1=ALU.mult),
                     r=[gk, sk, "gs_bc"], w=[vk])
                for g in range(8):
                    P.op("pe", lambda e, g=g, vt=vt: e.matmul(psS[:, g, :], lhsT=vt[:, g, :], rhs=wsT[:, g, :], start=True, stop=True), r=[vk, "wsT"], w=["psS"])
                P.op("dve", lambda e: e.tensor_tensor(out=tmp[:], in0=psS[:], in1=bs_bc[:], op=ALU.add), r=["psS", "bs_bc"], w=["tmp"])
                P.op("dve", lambda e, t=t, bTb=bTb: e.tensor_tensor(out=bTb[:, :, t * 128:(t + 1) * 128], in0=tmp[:], in1=ut[:], op=ALU.mult),
                     r=["tmp", "ut"], w=[("bT", hs, t)])
            P.dma("sp", bT_d[:, tok0:tok0 + ntok].rearrange("(g p) n -> p g n", p=128), bTb[:, :, 0:ntok], r=[("bT", hs, t) for t in range(ntile)], key=("st_bT", hs))
        P.flush()


def _phase_pool(nc, P, sb, ps, lw, ctx_out, G):
    w_in = G["w_in"]; hT_d = G["hT_d"]; cpT_d = G["cpT_d"]
    with ExitStack() as es:
        win_c = sb(es, "win_c", [128, KC, 1024], BF16)
        wpool = sb(es, "wpool", [128, 4, 2, 256], BF16)
        spT = sb(es, "spT", [128, 8], F32)
        hT = [sb(es, "hT%d" % i, [128, KC, 512], BF16) for i in range(2)]
        pp = sb(es, "pp", [128, 18, 1024], BF16)
        band = [sb(es, "band%d" % i, [128, 4, 6, 512], BF16) for i in range(2)]
        mT = sb(es, "mT", [128, 8, 512], BF16)
        cpT = [sb(es, "cpT%d" % i, [128, 8, 512], BF16) for i in range(2)]
        psP = [ps(es, "psP%d" % i, [128, 1024]) for i in range(2)]
        psM = [ps(es, "psM%d" % i, [128, 512]) for i in range(2)]
        psY = [ps(es, "psY%d" % i, [128, 512]) for i in range(2)]

        P.dma("pool", win_c[:], w_in[lw, :, 2880:3904].rearrange("(c p) n -> p c n", p=128), w=["win_c"])
        for gi in range(4):
            P.dma("pool", wpool[:, gi], G["w_pool"][lw, gi].rearrange("(k p) d -> p k d", p=128), w=[("wpool", gi)], key="wpool")
        P.dma("sp", spT[:], bass.AP(G["s_pool"].tensor, G["s_pool"].offset, [[1, 128], [128, 8]]), w=["spT"], allow_slow_non_contiguous=True)

        gt = 0; gb = 0; gm = 0
        for b in range(NB):
            blks = [(b * NBT + j * 512, 512, False, j) for j in range(4)]
            if ctx_out:
                blks.append((b * NBT + NLAT, NCTX, True, 0))
            for (tok0, ntok, is_ctx, j) in blks:
                ntile = ntok // 128
                hs = gb % 2; gb += 1
                hTb = hT[hs]; hk = ("hT", hs)
                P.dma("pool", hTb[:, :, 0:ntok], hT_d[:, tok0:tok0 + ntok].rearrange("(c p) n -> p c n", p=128), w=[hk])
                for t in range(ntile):
                    ti = (16 + t) if is_ctx else (j * 4 + t)
                    sl = gt % 2; gt += 1
                    pv = psP[sl]; pvk = ("psP", sl)
                    for jj in range(2):
                        for c in range(KC):
                            P.op("pe", lambda e, c=c, jj=jj, t=t, pv=pv, hTb=hTb: e.matmul(pv[:, jj * 512:(jj + 1) * 512], lhsT=hTb[:, c, t * 128:(t + 1) * 128], rhs=win_c[:, c, jj * 512:(jj + 1) * 512], start=(c == 0), stop=(c == KC - 1)),
                                 r=[hk, "win_c"], w=[pvk])
                    P.op("act", lambda e, ti=ti, pv=pv: e.copy(out=pp[:, ti, :], in_=pv[:]), r=[pvk], w=[("pp", ti)])
            for bi, (tok0, ntok, is_ctx, j) in enumerate(blks):
                bs_ = gm % 2; gm += 1
                bd = band[bs_]; bk = ("band", bs_)
                cpb = cpT[bs_]
                if is_ctx:
                    srcs = [(16 + s, s) for s in range(2)]
                    for gi in range(4):
                        P.dma("pool", bd[:, gi, 0:2, 0:256], G["bandC"][gi].rearrange("s p n -> p s n"), w=[(bk, gi)], key=bk)
                else:
                    srcs = [(4 * j - 1 + si, si) for si in range(6) if 0 <= 4 * j - 1 + si < 16]
                    for gi in range(4):
                        P.dma("pool", bd[:, gi], G["bandL"][gi, j].rearrange("s p n -> p s n"), w=[(bk, gi)], key=bk)
                for gi in range(4):
                    for cc in range(2):
                        idx = gi * 2 + cc
                        pm = psM[idx % 2]; pmk = ("psM", idx % 2)
                        for n_, (ti, si) in enumerate(srcs):
                            P.op("pe", lambda e, gi=gi, cc=cc, ti=ti, si=si, n_=n_, pm=pm, bd=bd, ntok=ntok, ns=len(srcs): e.matmul(pm[:, 0:ntok], lhsT=pp[:, ti, gi * 256 + cc * 128:gi * 256 + (cc + 1) * 128], rhs=bd[:, gi, si, 0:ntok], start=(n_ == 0), stop=(n_ == ns - 1)),
                                 r=[("pp", ti), (bk, gi)], w=[pmk])
                        P.op("act", lambda e, idx=idx, pm=pm, ntok=ntok: e.copy(out=mT[:, idx, 0:ntok], in_=pm[:, 0:ntok]), r=[pmk], w=[("mT", idx)])
                for gi in range(4):
                    for dc in range(2):
                        idx = gi * 2 + dc
                        py = psY[idx % 2]; pyk = ("psY", idx % 2)
                        for kc in range(2):
                            P.op("pe", lambda e, gi=gi, dc=dc, kc=kc, py=py, ntok=ntok: e.matmul(py[:, 0:ntok], lhsT=wpool[:, gi, kc, dc * 128:(dc + 1) * 128], rhs=mT[:, gi * 2 + kc, 0:ntok], start=(kc == 0), stop=(kc == 1)),
                                 r=[("mT", gi * 2), ("mT", gi * 2 + 1), ("wpool", gi)], w=[pyk])
                        P.op("dve", lambda e, idx=idx, py=py, ntok=ntok, cpb=cpb: e.tensor_scalar(out=cpb[:, idx, 0:ntok], in0=py[:, 0:ntok], scalar1=spT[:, idx:idx + 1], scalar2=None, op0=ALU.mult),
                             r=[pyk, "spT"], w=[("cpT", bs_, idx)])
                P.dma("sp", cpT_d[:, tok0:tok0 + ntok].rearrange("(g p) n -> p g n", p=128), cpb[:, :, 0:ntok], r=[("cpT", bs_, i) for i in range(8)], key=("st_cpT", bs_))
        P.flush()


def _phase_attn(nc, P, sb, ps, lw, ctx_out, G):
    qT_d = G["qT_d"]; knT_d = G["knT_d"]; krT_d = G["krT_d"]; v_d = G["v_d"]; r_d = G["r_d"]; aT_d = G["aT_d"]
    NKC = NBT // 128
    with ExitStack() as es:
        ones = sb(es, "ones", [128, 128], BF16)
        krT = sb(es, "krT", [64, NBT], BF16)
        rk = sb(es, "rk", [128, NKC, 8], F32)
        kTn = [sb(es, "kTn%d" % i, [128, NBT], BF16) for i in range(2)]
        vh = [sb(es, "vh%d" % i, [128, NKC, 128], BF16) for i in range(2)]
        qn = [sb(es, "qn%d" % i, [128, 512], BF16) for i in range(2)]
        qr = [sb(es, "qr%d" % i, [64, 512], BF16) for i in range(2)]
        pT = [sb(es, "pT%d" % i, [128, 512], BF16) for i in range(3)]
        rden = sb(es, "rden", [128, 512], F32)
        aT = [sb(es, "aT%d" % i, [128, 512], BF16) for i in range(2)]
        psS = [ps(es, "psS%d" % i, [128, 512]) for i in range(3)]
        psO = [ps(es, "psO%d" % i, [128, 512]) for i in range(2)]
        psD = [ps(es, "psD%d" % i, [128, 512]) for i in range(2)]
        P.op("dve", lambda e: e.memset(ones[:], 1.0), w=["ones"])
        gq = 0; gp = 0; gh = 0
        for b in range(NB):
            k0 = b * NBT
            P.dma("pool", krT[:], krT_d[:, k0:k0 + NBT], w=["krT"])
            P.dma("pool", rk[:], r_d[k0:k0 + NBT, :].rearrange("(t p) e -> p t e", p=128), w=["rk"])
            for h in range(8):
                hs = gh % 2; gh += 1
                kt = kTn[hs]; kk = ("kTn", hs); vt = vh[hs]; vk = ("vh", hs)
                P.dma("pool", kt[:], knT_d[h, :, k0:k0 + NBT], w=[kk])
                P.dma("pool", vt[:], v_d[k0:k0 + NBT, h, :].rearrange("(t p) d -> p t d", p=128), w=[vk])
                qblocks = [(k0 + j * 512, 512, list(range(NKC))) for j in range(4)]
                if ctx_out:
                    qblocks.append((k0 + NLAT, NCTX, [16, 17]))
                for (q0, nq, kcs) in qblocks:
                    qs = gq % 2; gq += 1
                    qnt = qn[qs]; qrt = qr[qs]; qk = ("q", qs)
                    P.dma("pool", qnt[:, 0:nq], qT_d[h, 0:128, q0:q0 + nq], w=[qk], key=("ldqn", qs))
                    P.dma("pool", qrt[:, 0:nq], qT_d[h, 128:192, q0:q0 + nq], w=[("qr", qs)], key=("ldqr", qs))
                    po = psO[qs]; pd = psD[qs]; pok = ("psO", qs)
                    for n_, kc in enumerate(kcs):
                        ss = gp % 3; gp += 1
                        pss = psS[ss]; psk = ("psS", ss); ptt = pT[ss]; ptk = ("pT", ss)
                        P.op("pe", lambda e, kc=kc, pss=pss, kt=kt, qnt=qnt, nq=nq: e.matmul(pss[:, 0:nq], lhsT=kt[:, kc * 128:(kc + 1) * 128], rhs=qnt[:, 0:nq], start=True, stop=False),
                             r=[kk, qk], w=[psk])
                        P.op("pe", lambda e, kc=kc, pss=pss, qrt=qrt, nq=nq: e.matmul(pss[:, 0:nq], lhsT=krT[:, kc * 128:(kc + 1) * 128], rhs=qrt[:, 0:nq], start=False, stop=True),
                             r=["krT", ("qr", qs)], w=[psk])
                        P.op("act", lambda e, kc=kc, h=h, pss=pss, ptt=ptt, nq=nq: e.activation(out=ptt[:, 0:nq], in_=pss[:, 0:nq], func=AF.Exp, scale=rk[:, kc, h:h + 1]),
                             r=[psk, "rk"], w=[ptk])
                        first = (n_ == 0); lastk = (n_ == len(kcs) - 1)
                        P.op("pe", lambda e, kc=kc, vt=vt, ptt=ptt, po=po, nq=nq, first=first, lastk=lastk: e.matmul(po[:, 0:nq], lhsT=vt[:, kc, :], rhs=ptt[:, 0:nq], start=first, stop=lastk),
                             r=[vk, ptk], w=[pok])
                        P.op("pe", lambda e, ptt=ptt, pd=pd, nq=nq, first=first, lastk=lastk: e.matmul(pd[:, 0:nq], lhsT=ones[:], rhs=ptt[:, 0:nq], start=first, stop=lastk),
                             r=["ones", ptk], w=[pok])
                    at = aT[qs]; ak = ("aT", qs)
                    P.op("dve", lambda e, pd=pd, nq=nq: e.reciprocal(out=rden[:, 0:nq], in_=pd[:, 0:nq]), r=[pok], w=["rden"])
                    P.op("dve", lambda e, po=po, at=at, nq=nq: e.tensor_tensor(out=at[:, 0:nq], in0=po[:, 0:nq], in1=rden[:, 0:nq], op=ALU.mult), r=[pok, "rden"], w=[ak])
                    P.dma("sp", aT_d[h * 128:(h + 1) * 128, q0:q0 + nq], at[:, 0:nq], r=[ak], key=("st_aT", qs))
        P.flush()


def _phase_merge(nc, P, sb, ps, lw, ctx_out, G):
    w_in = G["w_in"]; hT_d = G["hT_d"]; yT_d = G["yT_d"]
    srcs_d = [G["aT_d"], G["bT_d"], G["cpT_d"]]
    wbr_d = [G["w_ao"], G["w_bo"], G["w_co"]]
    GATE0 = 3904
    with ExitStack() as es:
        hT = [sb(es, "hT%d" % q, [128, KC, 512], BF16) for q in range(2)]
        br = [[sb(es, "br%d_%d" % (q, i), [128, 8, 512], BF16) for i in range(3)] for q in range(2)]
        wg = [sb(es, "wg%d" % i, [128, KC, 3, 256], BF16) for i in range(2)]
        wb = [sb(es, "wb%d" % i, [128, 3, 8, 256], BF16) for i in range(2)]
        sg = [sb(es, "sg%d" % i, [128, 512], F32) for i in range(2)]
        acc = sb(es, "acc", [128, 512], F32)
        t2 = sb(es, "t2", [128, 512], F32)
        yT = [sb(es, "yT%d" % i, [128, 2, 512], BF16) for i in range(4)]
        psG = [ps(es, "psG%d" % i, [128, 512]) for i in range(2)]
        psB = [ps(es, "psB%d" % i, [128, 512]) for i in range(2)]
        gw = 0; gpp = 0
        blks = _blocks(ctx_out)
        pairs = [blks[i:i + 2] for i in range(0, len(blks), 2)]
        for pair in pairs:
            for q, (tok0, ntok, row, is_ctx, b, pos0) in enumerate(pair):
                P.dma("sp", hT[q][:, :, 0:ntok], hT_d[:, tok0:tok0 + ntok].rearrange("(c p) n -> p c n", p=128), w=[("hT", q)])
                for i in range(3):
                    P.dma("sp", br[q][i][:, :, 0:ntok], srcs_d[i][:, tok0:tok0 + ntok].rearrange("(c p) n -> p c n", p=128), w=[("br", q, i)])
            for dg in range(8):
                ws = gw % 2; gw += 1
                wgt = wg[ws]; wbt = wb[ws]; wgk = ("wg", ws); wbk = ("wb", ws)
                for i in range(3):
                    P.dma("pool", wgt[:, :, i, :], w_in[lw, :, GATE0 + i * D + dg * 256:GATE0 + i * D + (dg + 1) * 256].rearrange("(c p) n -> p c n", p=128), w=[(wgk, i)], key=wgk)
                    P.dma("pool", wbt[:, i, :, :], wbr_d[i][lw, :, dg * 256:(dg + 1) * 256].rearrange("(c p) n -> p c n", p=128), w=[(wbk, i)], key=wbk)
                for q, (tok0, ntok, row, is_ctx, b, pos0) in enumerate(pair):
                    hTq = hT[q]; brq = br[q]
                    ys = ws * 2 + q
                    ytt = yT[ys]; yk = ("yT", ys)
                    for dl in range(2):
                        for i in range(3):
                            s_ = gpp % 2; gpp += 1
                            pg = psG[s_]; pb = psB[s_]; pgk = ("psG", s_); pbk = ("psB", s_)
                            sgt = sg[s_]; sgk = ("sg", s_)
                            for c in range(KC):
                                P.op("pe", lambda e, c=c, i=i, dl=dl, pg=pg, wgt=wgt, ntok=ntok, hTq=hTq: e.matmul(pg[:, 0:ntok], lhsT=wgt[:, c, i, dl * 128:(dl + 1) * 128], rhs=hTq[:, c, 0:ntok], start=(c == 0), stop=(c == KC - 1)),
                                     r=[("hT", q), (wgk, i)], w=[pgk])
                            for c in range(8):
                                P.op("pe", lambda e, c=c, i=i, dl=dl, pb=pb, wbt=wbt, ntok=ntok, brq=brq: e.matmul(pb[:, 0:ntok], lhsT=wbt[:, i, c, dl * 128:(dl + 1) * 128], rhs=brq[i][:, c, 0:ntok], start=(c == 0), stop=(c == 7)),
                                     r=[("br", q, i), (wbk, i)], w=[pbk])
                            P.op("act", lambda e, pg=pg, sgt=sgt, ntok=ntok: e.activation(out=sgt[:, 0:ntok], in_=pg[:, 0:ntok], func=AF.Sigmoid), r=[pgk], w=[sgk])
                            if i == 0:
                                P.op("dve", lambda e, pb=pb, sgt=sgt, ntok=ntok: e.tensor_tensor(out=acc[:, 0:ntok], in0=pb[:, 0:ntok], in1=sgt[:, 0:ntok], op=ALU.mult), r=[pbk, sgk], w=["acc"])
                            elif i == 1:
                                P.op("dve", lambda e, pb=pb, sgt=sgt, ntok=ntok: e.tensor_tensor(out=t2[:, 0:ntok], in0=pb[:, 0:ntok], in1=sgt[:, 0:ntok], op=ALU.mult), r=[pbk, sgk], w=["t2"])
                                P.op("dve", lambda e, ntok=ntok: e.tensor_tensor(out=acc[:, 0:ntok], in0=acc[:, 0:ntok], in1=t2[:, 0:ntok], op=ALU.add), r=["acc", "t2"], w=["acc"])
                            else:
                                P.op("dve", lambda e, pb=pb, sgt=sgt, ntok=ntok: e.tensor_tensor(out=t2[:, 0:ntok], in0=pb[:, 0:ntok], in1=sgt[:, 0:ntok], op=ALU.mult), r=[pbk, sgk], w=["t2"])
                                P.op("dve", lambda e, ntok=ntok, ytt=ytt, dl=dl: e.tensor_tensor(out=ytt[:, dl, 0:ntok], in0=acc[:, 0:ntok], in1=t2[:, 0:ntok], op=ALU.add), r=["acc", "t2"], w=[(yk, dl)])
                    P.dma("sp", yT_d[dg * 256:(dg + 1) * 256, tok0:tok0 + ntok].rearrange("(c p) n -> p c n", p=128), ytt[:, :, 0:ntok], r=[(yk, 0), (yk, 1)], key=("st_yT", ys))
        P.flush()


def _phase_outproj(nc, P, sb, ps, lw, ctx_out, G):
    xs = G["xs"]; mod_d = G["mod_d"]; yT_d = G["yT_d"]
    with ExitStack() as es:
        wout = sb(es, "wout", [128, KC, D], BF16)
        g1 = sb(es, "g1", [128, 3, D], F32)
        yT = [sb(es, "yT%d" % i, [128, KC, 512], BF16) for i in range(2)]
        xt = [sb(es, "xt%d" % i, [128, D], F32) for i in range(2)]
        tm = [sb(es, "tm%d" % i, [128, D], F32) for i in range(2)]
        psO = [ps(es, "psO%d" % i, [128, 512]) for i in range(8)]
        for hf in range(2):
            P.dma("pool", wout[:, :, hf * 1024:(hf + 1) * 1024], G["w_out"][lw, :, hf * 1024:(hf + 1) * 1024].rearrange("(c p) n -> p c n", p=128), w=[("wout", hf)])
        for r_ in range(3):
            P.dma("sp", g1[:, r_, :], _bcast_rows(mod_d[r_:r_ + 1, 2 * D:3 * D]), w=[("g1", r_)])
        gt = 0
        for bi, (tok0, ntok, row, is_ctx, b, pos0) in enumerate(_blocks(ctx_out)):
            ys = bi % 2
            ytt = yT[ys]; yk = ("yT", ys)
            P.dma("pool", ytt[:, :, 0:ntok], yT_d[:, tok0:tok0 + ntok].rearrange("(c p) n -> p c n", p=128), w=[yk])
            for t in range(ntok // 128):
                sl = gt % 2; gt += 1
                r0 = tok0 + t * 128
                xtt = xt[sl]; xk = ("xt", sl); tmt = tm[sl]; tk = ("tm", sl)
                P.dma("pool", xtt[:], xs[r0:r0 + 128, :], w=[xk])
                for j in range(4):
                    po = psO[sl * 4 + j]; pk = ("psO", sl * 4 + j)
                    for c in range(KC):
                        P.op("pe", lambda e, c=c, j=j, t=t, po=po, ytt=ytt: e.matmul(po[:], lhsT=ytt[:, c, t * 128:(t + 1) * 128], rhs=wout[:, c, j * 512:(j + 1) * 512], start=(c == 0), stop=(c == KC - 1)),
                             r=[yk, ("wout", j // 2)], w=[pk])
                    P.op("dve", lambda e, j=j, po=po, tmt=tmt, row=row: e.tensor_tensor(out=tmt[:, j * 512:(j + 1) * 512], in0=po[:], in1=g1[:, row, j * 512:(j + 1) * 512], op=ALU.mult),
                         r=[pk, ("g1", row)], w=[(tk, j)])
                P.op("pool", lambda e, xtt=xtt, tmt=tmt: e.tensor_tensor(out=xtt[:], in0=xtt[:], in1=tmt[:], op=ALU.add), r=[xk] + [(tk, j) for j in range(4)], w=[xk])
                P.dma("sp", xs[r0:r0 + 128, :], xtt[:], r=[xk], key=("st_x", sl))
        P.flush()


def _tile_row(gti):
    b, lt = divmod(gti, NBT // 128)
    return b if lt < NLAT // 128 else 2


def _phase_moe(nc, P, sb, ps, lw, ctx_out, G):
    xs = G["xs"]; mod_d = G["mod_d"]; hT_d = G["hT_d"]
    g_d = G["g_d"]; ymoe_d = G["ymoe_d"]
    NTILE = NT // 128
    with ExitStack() as es:
        ident = sb(es, "ident", [128, 128], BF16)
        wr = sb(es, "wr", [128, KC, 36], BF16)
        br_bc = sb(es, "br_bc", [128, 36], F32)
        xt = [sb(es, "xt%d" % i, [128, D], F32) for i in range(2)]
        jk = sb(es, "jk", [128, D], BF16)
        st = [sb(es, "st%d" % i, [128, 16], F32) for i in range(2)]
        xnb = [sb(es, "xnb%d" % i, [128, D], BF16) for i in range(2)]
        tmpf = sb(es, "tmpf", [128, KC, 128], F32)
        hT = [sb(es, "hT%d" % i, [128, KC, 128], BF16) for i in range(2)]
        Lg = [sb(es, "Lg%d" % i, [128, 36], F32) for i in range(2)]
        rt = [sb(es, "rt%d" % i, [128, 96], F32) for i in range(2)]
        gat = [sb(es, "gat%d" % i, [128, 4, 8], F32) for i in range(2)]
        psT = ps(es, "psT", [128, 2048], BF16)
        psR = [ps(es, "psR%d" % i, [128, 64]) for i in range(2)]
        psT3 = psT[:].rearrange("p (c n) -> p c n", c=KC)
        P.dma("sp", ident[:], G["ident_in"][:, :], w=["ident"])
        P.dma("pool", wr[:], G["w_r"][lw].rearrange("(c p) n -> p c n", p=128), w=["wr"])
        P.dma("sp", br_bc[:], _bcast_rows(G["b_r"][lw:lw + 1, :]), w=["br_bc"])
        sc2 = _load_modT(nc, P, es, sb, mod_d, 4, "modsc")
        sh2 = _load_modT(nc, P, es, sb, mod_d, 3, "modsh")
        P.op("dve", lambda e: e.tensor_scalar(out=sc2[:], in0=sc2[:], scalar1=1.0, scalar2=None, op0=ALU.add), r=["modsc"], w=["modsc"])
        for gti in range(NTILE):
            row = _tile_row(gti)
            if row == 2 and not ctx_out:
                continue
            sl = gti % 2
            r0 = gti * 128
            P.dma("pool", xt[sl][:], xs[r0:r0 + 128, :], w=[("xt", sl)])
            hTt = hT[sl]; hk = ("hT", sl)
            _norm_mod_tile(P, xt[sl], ("xt", sl), jk, "jk", st[sl], ("st", sl), xnb[sl], ("xnb", sl), psT3, "psT", ident,
                           tmpf, "tmpf", sc2, sh2, row, hTt[:], hk)
            P.dma("sp", hT_d[:, r0:r0 + 128].rearrange("(c p) n -> p c n", p=128), hTt[:], r=[hk], key=("st_hT", sl))
            pr = psR[sl]; prk = ("psR", sl)
            for c in range(KC):
                P.op("pe", lambda e, c=c, pr=pr, hTt=hTt: e.matmul(pr[:, 0:36], lhsT=hTt[:, c, :], rhs=wr[:, c, :], start=(c == 0), stop=(c == KC - 1)), r=[hk, "wr"], w=[prk])
            L = Lg[sl]; lk = ("Lg", sl); R_ = rt[sl]; rk = ("rt", sl); gt_ = gat[sl]; gk = ("gat", sl)
            P.op("dve", lambda e, L=L, pr=pr: e.tensor_tensor(out=L[:], in0=pr[:, 0:36], in1=br_bc[:], op=ALU.add), r=[prk, "br_bc"], w=[lk])
            P.op("dve", lambda e, L=L, R_=R_: e.tensor_reduce(out=R_[:, 0:1], in_=L[:, 0:4], axis=AX.X, op=ALU.max), r=[lk], w=[rk])
            P.op("dve", lambda e, R_=R_: e.tensor_scalar(out=R_[:, 1:2], in0=R_[:, 0:1], scalar1=-1.0, scalar2=None, op0=ALU.mult), r=[rk], w=[rk])
            P.op("dve", lambda e, L=L, R_=R_: e.tensor_scalar(out=R_[:, 4:8], in0=L[:, 0:4], scalar1=R_[:, 0:1], scalar2=None, op0=ALU.is_ge), r=[lk, rk], w=[rk])
            P.op("act", lambda e, L=L, R_=R_: e.activation(out=R_[:, 8:12], in_=L[:, 0:4], func=AF.Exp, bias=R_[:, 1:2], scale=1.0, accum_out=R_[:, 2:3]), r=[lk, rk], w=[rk])
            P.op("dve", lambda e, R_=R_: e.reciprocal(out=R_[:, 3:4], in_=R_[:, 2:3]), r=[rk], w=[rk])
            P.op("dve", lambda e, L=L, R_=R_: e.tensor_tensor(out=R_[:, 12:44].rearrange("p (g x) -> p g x", g=4), in0=L[:, 4:36].rearrange("p (g x) -> p g x", g=4),
                                                             in1=R_[:, 4:8].unsqueeze(2).to_broadcast([128, 4, 8]), op=ALU.mult), r=[lk, rk], w=[rk])
            P.op("dve", lambda e, R_=R_: e.tensor_reduce(out=R_[:, 44:52], in_=R_[:, 12:44].rearrange("p (g x) -> p x g", g=4), axis=AX.X, op=ALU.add), r=[rk], w=[rk])
            P.op("dve", lambda e, R_=R_: e.tensor_reduce(out=R_[:, 52:53], in_=R_[:, 44:52], axis=AX.X, op=ALU.max), r=[rk], w=[rk])
            P.op("dve", lambda e, R_=R_: e.tensor_scalar(out=R_[:, 54:62], in0=R_[:, 44:52], scalar1=R_[:, 52:53], scalar2=None, op0=ALU.is_ge), r=[rk], w=[rk])
            P.op("dve", lambda e, R_=R_: e.scalar_tensor_tensor(out=R_[:, 62:70], in0=R_[:, 54:62], scalar=-1e30, in1=R_[:, 44:52], op0=ALU.mult, op1=ALU.add), r=[rk], w=[rk])
            P.op("dve", lambda e, R_=R_: e.tensor_reduce(out=R_[:, 53:54], in_=R_[:, 62:70], axis=AX.X, op=ALU.max), r=[rk], w=[rk])
            P.op("dve", lambda e, R_=R_: e.tensor_scalar(out=R_[:, 70:78], in0=R_[:, 62:70], scalar1=R_[:, 53:54], scalar2=None, op0=ALU.is_ge), r=[rk], w=[rk])
            P.op("dve", lambda e, R_=R_: e.tensor_tensor(out=R_[:, 78:79], in0=R_[:, 52:53], in1=R_[:, 53:54], op=ALU.subtract), r=[rk], w=[rk])
            P.op("act", lambda e, R_=R_: e.activation(out=R_[:, 79:80], in_=R_[:, 78:79], func=AF.Sigmoid), r=[rk], w=[rk])
            P.op("dve", lambda e, R_=R_: e.tensor_scalar(out=R_[:, 80:81], in0=R_[:, 79:80], scalar1=-1.0, scalar2=1.0, op0=ALU.mult, op1=ALU.add), r=[rk], w=[rk])
            P.op("dve", lambda e, R_=R_: e.tensor_scalar(out=R_[:, 81:83], in0=R_[:, 79:81], scalar1=R_[:, 3:4], scalar2=None, op0=ALU.mult), r=[rk], w=[rk])
            P.op("dve", lambda e, R_=R_: e.tensor_scalar(out=R_[:, 83:91], in0=R_[:, 54:62], scalar1=R_[:, 81:82], scalar2=None, op0=ALU.mult), r=[rk], w=[rk])
            P.op("dve", lambda e, R_=R_: e.scalar_tensor_tensor(out=R_[:, 44:52], in0=R_[:, 70:78], scalar=R_[:, 82:83], in1=R_[:, 83:91], op0=ALU.mult, op1=ALU.add), r=[rk], w=[rk])
            P.op("dve", lambda e, R_=R_, gt_=gt_: e.tensor_tensor(out=gt_[:], in0=R_[:, 4:8].unsqueeze(2).to_broadcast([128, 4, 8]), in1=R_[:, 44:52].unsqueeze(1).to_broadcast([128, 4, 8]), op=ALU.mult),
                 r=[rk], w=[gk])
            P.dma("sp", g_d[r0:r0 + 128, :], gt_[:].rearrange("p g x -> p (g x)"), r=[gk], key=("st_g", sl))
        P.flush()

    TB = 768
    with ExitStack() as es:
        hT = sb(es, "hT", [128, KC, TB], BF16)
        gates = sb(es, "gates", [128, TB // 128, 32], F32)
        yacc = sb(es, "yacc", [128, TB // 128, D], F32)
        wg = [sb(es, "wg%d" % i, [128, KC, HID], BF16) for i in range(2)]
        wu = [sb(es, "wu%d" % i, [128, KC, HID], BF16) for i in range(2)]
        wd = [sb(es, "wd%d" % i, [128, 4, D], BF16) for i in range(2)]
        hid = [sb(es, "hid%d" % i, [128, 4, TB], BF16) for i in range(2)]
        sg = [sb(es, "sg%d" % i, [128, 384], F32) for i in range(2)]
        psG = [ps(es, "psG%d" % i, [128, 512]) for i in range(2)]
        psU = [ps(es, "psU%d" % i, [128, 512]) for i in range(2)]
        psD = [ps(es, "psD%d" % i, [128, 512]) for i in range(4)]
        ge = 0; gs_ = 0
        for blk in range(NT // TB):
            tok0 = blk * TB
            tiles = [t for t in range(TB // 128) if ctx_out or _tile_row(tok0 // 128 + t) != 2]
            P.dma("sp", hT[:], hT_d[:, tok0:tok0 + TB].rearrange("(c p) n -> p c n", p=128), w=["hT"])
            P.dma("sp", gates[:], g_d[tok0:tok0 + TB, :].rearrange("(t p) e -> p t e", p=128), w=["gates"])
            P.op("pool", lambda e: e.memset(yacc[:], 0.0), w=[("yacc", t) for t in range(TB // 128)])
            for ex in range(NEXP):
                ws = ge % 2; ge += 1
                wgt = wg[ws]; wut = wu[ws]; wdt = wd[ws]; hidt = hid[ws]
                P.dma("pool", wgt[:], G["w_eg"][lw, ex].rearrange("(c p) n -> p c n", p=128), w=[("wg", ws)])
                P.dma("pool", wut[:], G["w_eu"][lw, ex].rearrange("(c p) n -> p c n", p=128), w=[("wu", ws)])
                P.dma("pool", wdt[:], G["w_ed"][lw, ex].rearrange("(c p) n -> p c n", p=128), w=[("wd", ws)])
                for hc in range(4):
                    for hf in range(2):
                        s_ = gs_ % 2; gs_ += 1
                        pg = psG[s_]; pu = psU[s_]; pgk = ("psG", s_); puk = ("psU", s_); sgt = sg[s_]; sgk = ("sg", s_)
                        for c in range(KC):
                            P.op("pe", lambda e, c=c, hc=hc, hf=hf, pg=pg, wgt=wgt: e.matmul(pg[:, 0:384], lhsT=wgt[:, c, hc * 128:(hc + 1) * 128], rhs=hT[:, c, hf * 384:(hf + 1) * 384], start=(c == 0), stop=(c == KC - 1)),
                                 r=["hT", ("wg", ws)], w=[pgk])
                        for c in range(KC):
                            P.op("pe", lambda e, c=c, hc=hc, hf=hf, pu=pu, wut=wut: e.matmul(pu[:, 0:384], lhsT=wut[:, c, hc * 128:(hc + 1) * 128], rhs=hT[:, c, hf * 384:(hf + 1) * 384], start=(c == 0), stop=(c == KC - 1)),
                                 r=["hT", ("wu", ws)], w=[puk])
                        P.op("act", lambda e, pg=pg, sgt=sgt: e.activation(out=sgt[:], in_=pg[:, 0:384], func=AF.Silu), r=[pgk], w=[sgk])
                        P.op("dve", lambda e, pu=pu, sgt=sgt, hidt=hidt, hc=hc, hf=hf: e.tensor_tensor(out=hidt[:, hc, hf * 384:(hf + 1) * 384], in0=pu[:, 0:384], in1=sgt[:], op=ALU.mult),
                             r=[puk, sgk], w=[("hid", ws, hc, hf)])
                for t in tiles:
                    hf = (t * 128) // 384
                    hf2 = (t * 128 + 127) // 384
                    for j in range(4):
                        pd = psD[j]; pdk = ("psD", j)
                        for hc in range(4):
                            P.op("pe", lambda e, hc=hc, j=j, t=t, pd=pd, hidt=hidt, wdt=wdt: e.matmul(pd[:], lhsT=hidt[:, hc, t * 128:(t + 1) * 128], rhs=wdt[:, hc, j * 512:(j + 1) * 512], start=(hc == 0), stop=(hc == 3)),
                                 r=[("hid", ws, hc, hf), ("hid", ws, hc, hf2), ("wd", ws)], w=[pdk])
                        P.op("dve", lambda e, j=j, t=t, pd=pd, ex=ex: e.scalar_tensor_tensor(out=yacc[:, t, j * 512:(j + 1) * 512], in0=pd[:], scalar=gates[:, t, ex:ex + 1], in1=yacc[:, t, j * 512:(j + 1) * 512], op0=ALU.mult, op1=ALU.add),
                             r=[pdk, "gates", ("yacc", t)], w=[("yacc", t)])
            for t in tiles:
                r0 = tok0 + t * 128
                P.dma("sp", ymoe_d[r0:r0 + 128, :], yacc[:, t, :], r=[("yacc", t)], key=("st_y", t))
        P.flush()

    with ExitStack() as es:
        g2 = sb(es, "g2", [128, 3, D], F32)
        xt = [sb(es, "xt%d" % i, [128, D], F32) for i in range(3)]
        yt = [sb(es, "yt%d" % i, [128, D], F32) for i in range(3)]
        for r_ in range(3):
            P.dma("sp", g2[:, r_, :], _bcast_rows(mod_d[r_:r_ + 1, 5 * D:6 * D]), w=[("g2", r_)])
        n = 0
        for gti in range(NTILE):
            row = _tile_row(gti)
            if row == 2 and not ctx_out:
                continue
            sl = n % 3; n += 1
            r0 = gti * 128
            xtt = xt[sl]; ytt = yt[sl]; xk = ("xt", sl); yk = ("yt", sl)
            P.dma("pool", xtt[:], xs[r0:r0 + 128, :], w=[xk])
            P.dma("pool", ytt[:], ymoe_d[r0:r0 + 128, :], w=[yk])
            P.op("dve", lambda e, ytt=ytt, row=row: e.tensor_tensor(out=ytt[:], in0=ytt[:], in1=g2[:, row, :], op=ALU.mult), r=[yk, ("g2", row)], w=[yk])
            P.op("dve", lambda e, xtt=xtt, ytt=ytt: e.tensor_tensor(out=xtt[:], in0=xtt[:], in1=ytt[:], op=ALU.add), r=[xk, yk], w=[xk])
            P.dma("sp", xs[r0:r0 + 128, :], xtt[:], r=[xk], key=("st_x", sl))
        P.flush()


_CONSTS = None


def _consts():
    global _CONSTS
    if _CONSTS is None:
        cosv, sinv = _rope_tables()
        bl, bc = _band_consts()
        _CONSTS = {"rope_cos": cosv, "rope_sin": sinv, "bandL": bl, "bandC": bc,
                   "ident": np.eye(128, dtype=np.float32).astype(ml_dtypes.bfloat16)}
    return _CONSTS


def _pack_layer(inp, li):
    w_re = inp["w_re"][li]
    src = {
        "w_sguT": np.transpose(inp["w_sgu"][li], (0, 2, 1)),
        "w_r": np.concatenate([inp["w_rg"][li], np.transpose(w_re, (1, 0, 2)).reshape(D, 32)], axis=-1),
        "b_r": np.concatenate([inp["b_rg"][li], inp["b_re"][li].reshape(32)], axis=-1),
    }
    bufs = [np.zeros((CHUNK_ROWS[k], 2048), dtype=np.float32) for k in range(NCHUNK)]
    for name, shape, ck, row0 in PACK_SPEC:
        a = src[name] if name in src else inp[name][li]
        a = np.asarray(a, dtype=np.float32).reshape(-1)
        assert a.size == int(np.prod(shape)), (name, a.size, shape)
        bufs[ck].reshape(-1)[row0 * 2048:row0 * 2048 + a.size] = a
    return bufs


_PROGS = {}


def _get_prog(layers, single):
    key = (tuple(layers), single)
    if key not in _PROGS:
        _PROGS[key] = build_program(list(layers), single)
    return _PROGS[key]


def _core_tokens(x, ctx, core):
    parts = []
    for b in range(NB):
        parts.append(x[core * NB + b])
        parts.append(ctx[core * NB + b])
    return np.ascontiguousarray(np.concatenate(parts, axis=0))


FUSED = True


def kernel(**inp):
    inp = {k: np.asarray(v) for k, v in inp.items()}
    x = inp["x"].astype(np.float32, copy=False)
    ctx = inp["ctx"].astype(np.float32, copy=False)
    c = inp["c"]; c_ctx = inp["c_ctx"]
    consts = _consts()
    xin = [_core_tokens(x, ctx, core) for core in range(NCORES)]
    c3 = [np.ascontiguousarray(np.stack([c[core * NB], c[core * NB + 1], c_ctx], axis=0)) for core in range(NCORES)]
    groups = [list(range(DEPTH))] if FUSED else [[li] for li in range(DEPTH)]
    for layers in groups:
        packed = [_pack_layer(inp, li) for li in layers]
        nc = _get_prog(layers, True)
        in_maps = []
        for core in range(NCORES):
            m = dict(consts, xin=xin[core], c3=c3[core])
            for pos in range(len(layers)):
                for k in range(NCHUNK):
                    m["wpk_%d_%d" % (pos, k)] = packed[pos][k]
            in_maps.append(m)
        del packed
        res = run_bass_kernel_spmd(nc, in_maps, core_ids=list(range(NCORES)))
        xin = [np.asarray(r["xout"]) for r in res.results]
        del res, in_maps
    out = np.empty((NCORES * NB, NLAT, D), dtype=np.float32)
    for core in range(NCORES):
        for b in range(NB):
            out[core * NB + b] = xin[core][b * NBT:b * NBT + NLAT]
    return out
```
